# Optimizing a Trainium2 kernel written in Bass

```python
import math
import numpy as np
import jax
import jax.numpy as jnp
from jax import lax

D_MODEL = 2048
BATCH = 4
SEQ = 4096
DEPTH = 2

F32 = jnp.float32
N_EVEN = (DEPTH + 1) // 2
N_ODD = DEPTH // 2
NORM_EPS = 1e-6
NEG_BIG = -1e30

SSM_HEADS = 32
SSM_HEAD_DIM = 64
D_SSM = SSM_HEADS * SSM_HEAD_DIM
SSM_GROUPS = 4
D_STATE = 128
CONV_WIDTH = 4
SSD_CHUNK = 128
D_CONV = D_SSM + 2 * SSM_GROUPS * D_STATE

NSA_HEADS = 16
NSA_KV_HEADS = 4
NSA_HEAD_DIM = 128
NSA_Q_PER_KV = NSA_HEADS // NSA_KV_HEADS
D_NSA = NSA_HEADS * NSA_HEAD_DIM
D_NSA_KV = NSA_KV_HEADS * NSA_HEAD_DIM
CMP_BLOCK = 32
CMP_STRIDE = 16
CMP_HIDDEN = 256
SEL_BLOCK = 64
N_SELECT = 16
WINDOW = 512
NSA_QUERY_BLOCK = 32
ROPE_DIM = NSA_HEAD_DIM // 4
ROPE_THETA = 500000.0

EVEN_SPLITS = (D_SSM, D_CONV, SSM_HEADS, D_NSA, D_NSA_KV, D_NSA_KV, D_NSA_KV, D_NSA_KV, D_NSA_KV, D_NSA_KV, 3 * NSA_HEADS)
D_IN_EVEN = sum(EVEN_SPLITS)
D_MIX_EVEN = D_SSM + D_NSA

RWKV_HEAD_DIM = 64
RWKV_HEADS = D_MODEL // RWKV_HEAD_DIM
DECAY_LORA = max(32, int(round(1.8 * D_MODEL ** 0.5 / 32)) * 32)
AAA_LORA = DECAY_LORA
GATE_LORA = max(32, int(round(0.6 * D_MODEL ** 0.8 / 32)) * 32)
RWKV_GN_EPS = 1e-5 * RWKV_HEAD_DIM
N_TOKEN_MIX = 6

MEM_LEN = 256
XATTN_HEADS = 4
XATTN_HEAD_DIM = 128
D_XATTN = XATTN_HEADS * XATTN_HEAD_DIM

D_FF_DENSE = 5632
N_EXPERTS = 8
TOP_K = 2
D_FF_EXPERT = 7168
MOE_BLOCK = 256

kernel_name = 'hybrid_ssd_nsa_rwkv7_moe_trunk'


def rms_norm(x, gain, eps=NORM_EPS):
    xf = x.astype(F32)
    y = xf * lax.rsqrt(jnp.mean(xf * xf, axis=-1, keepdims=True) + eps)
    return (y * gain.astype(F32)).astype(x.dtype)


def masked_softmax(scores, mask):
    s = jnp.where(mask, scores.astype(F32), NEG_BIG)
    return jax.nn.softmax(s, axis=-1) * mask


def partial_rope(x, positions):
    half = ROPE_DIM // 2
    inv_freq = jnp.exp(-math.log(ROPE_THETA) * jnp.arange(0, ROPE_DIM, 2, dtype=F32) / ROPE_DIM)
    ang = positions.astype(F32)[..., None] * inv_freq
    cos = jnp.cos(ang)[:, :, None, :]
    sin = jnp.sin(ang)[:, :, None, :]
    xf = x.astype(F32)
    x1 = xf[..., :half]
    x2 = xf[..., half:ROPE_DIM]
    out = jnp.concatenate([x1 * cos - x2 * sin, x2 * cos + x1 * sin, xf[..., ROPE_DIM:]], axis=-1)
    return out.astype(x.dtype)


def causal_depthwise_conv(x, w, b):
    rhs = jnp.transpose(w)[:, None, :].astype(x.dtype)
    y = lax.conv_general_dilated(x, rhs, window_strides=(1,), padding=[(CONV_WIDTH - 1, 0)],
                                 dimension_numbers=('NWC', 'WIO', 'NWC'),
                                 feature_group_count=x.shape[-1])
    return y + b.astype(x.dtype)


def ssd_chunked_scan(xs, dt, a, bm, cm):
    bsz, seq = xs.shape[0], xs.shape[1]
    G, R, P, N, L = SSM_GROUPS, SSM_HEADS // SSM_GROUPS, SSM_HEAD_DIM, D_STATE, SSD_CHUNK
    nc = seq // L
    xdt = (xs.astype(F32) * dt[..., None]).reshape(bsz, nc, L, G, R, P)
    adt = (dt * a).reshape(bsz, nc, L, G, R).transpose(0, 1, 3, 4, 2)
    bc = bm.astype(F32).reshape(bsz, nc, L, G, N)
    cc = cm.astype(F32).reshape(bsz, nc, L, G, N)
    a_cum = jnp.cumsum(adt, axis=-1)
    causal = jnp.tril(jnp.ones((L, L), dtype=bool))
    decay = jnp.exp(jnp.where(causal, a_cum[..., :, None] - a_cum[..., None, :], NEG_BIG))
    cb = jnp.einsum('bclgn,bcsgn->bcgls', cc, bc)
    y_diag = jnp.einsum('bcgls,bcgrls,bcsgrp->bclgrp', cb, decay, xdt)
    decay_to_end = jnp.exp(a_cum[..., -1:] - a_cum)
    chunk_states = jnp.einsum('bclgn,bcgrl,bclgrp->bcgrpn', bc, decay_to_end, xdt)
    chunk_decay = jnp.exp(a_cum[..., -1])

    def carry_state(h, inp):
        st, dec = inp
        return h * dec[..., None, None] + st, h

    h0 = jnp.zeros((bsz, G, R, P, N), F32)
    _, h_in = lax.scan(carry_state, h0, (jnp.moveaxis(chunk_states, 1, 0), jnp.moveaxis(chunk_decay, 1, 0)))
    h_in = jnp.moveaxis(h_in, 0, 1)
    y_off = jnp.einsum('bclgn,bcgrpn,bcgrl->bclgrp', cc, h_in, jnp.exp(a_cum))
    return (y_diag + y_off).reshape(bsz, seq, SSM_HEADS, P)


def mamba2_group(z, xbc, dt_raw, conv_w, conv_b, dt_bias, a_log, d_skip, norm_w):
    bsz, seq, _ = z.shape
    xbc = jax.nn.silu(causal_depthwise_conv(xbc, conv_w, conv_b))
    xs, bm, cm = jnp.split(xbc, [D_SSM, D_SSM + SSM_GROUPS * D_STATE], axis=-1)
    xs = xs.reshape(bsz, seq, SSM_HEADS, SSM_HEAD_DIM)
    bm = bm.reshape(bsz, seq, SSM_GROUPS, D_STATE)
    cm = cm.reshape(bsz, seq, SSM_GROUPS, D_STATE)
    dt = jax.nn.softplus(dt_raw.astype(F32) + dt_bias.astype(F32))
    a = -jnp.exp(a_log.astype(F32))
    y = ssd_chunked_scan(xs, dt, a, bm, cm) + xs.astype(F32) * d_skip.astype(F32)[:, None]
    y = y.reshape(bsz, seq, D_SSM) * jax.nn.silu(z.astype(F32))
    y = y.reshape(bsz, seq, SSM_GROUPS, D_SSM // SSM_GROUPS)
    y = y * lax.rsqrt(jnp.mean(y * y, axis=-1, keepdims=True) + NORM_EPS)
    return (y.reshape(bsz, seq, D_SSM) * norm_w.astype(F32)).astype(z.dtype)


def compress_blocks(u, pe, w1, w2):
    bsz, seq, nkv, hd = u.shape
    n_cmp = (seq - CMP_BLOCK) // CMP_STRIDE + 1
    idx = jnp.arange(n_cmp)[:, None] * CMP_STRIDE + jnp.arange(CMP_BLOCK)[None, :]
    blocks = u[:, idx] + pe[:, None, :].astype(u.dtype)
    blocks = blocks.transpose(0, 1, 3, 2, 4).reshape(bsz, n_cmp, nkv, CMP_BLOCK * hd)
    return jax.nn.silu(blocks @ w1) @ w2


def selection_overlap(n_cmp, n_blk):
    c0 = np.arange(n_cmp)[:, None] * CMP_STRIDE
    s0 = np.arange(n_blk)[None, :] * SEL_BLOCK
    ov = np.minimum(c0 + CMP_BLOCK, s0 + SEL_BLOCK) - np.maximum(c0, s0)
    return jnp.asarray(np.clip(ov, 0, None) / CMP_STRIDE, dtype=F32)


def nsa_group(q, k_cmp, v_cmp, k_sel, v_sel, k_win, v_win, gate_logits, kc_gain, pe_k, pe_v, wk1, wk2, wv1, wv2):
    bsz, seq = q.shape[0], q.shape[1]
    G, R, hd = NSA_KV_HEADS, NSA_Q_PER_KV, NSA_HEAD_DIM
    scale = hd ** -0.5
    t = jnp.arange(seq)
    qg = q.reshape(bsz, seq, G, R, hd)
    kc = rms_norm(compress_blocks(k_cmp, pe_k, wk1, wk2), kc_gain)
    vc = compress_blocks(v_cmp, pe_v, wv1, wv2)
    n_cmp = kc.shape[1]
    cmp_visible = (jnp.arange(n_cmp) * CMP_STRIDE + CMP_BLOCK - 1)[None, :] <= t[:, None]
    p_cmp = masked_softmax(jnp.einsum('bsgrd,bcgd->bgrsc', qg, kc) * scale, cmp_visible)
    o_cmp = jnp.einsum('bgrsc,bcgd->bsgrd', p_cmp.astype(vc.dtype), vc)
    n_blk = seq // SEL_BLOCK
    n_sel = min(N_SELECT, n_blk)
    imp = jnp.einsum('bgrsc,cj->bsgj', p_cmp, selection_overlap(n_cmp, n_blk))
    cur = (t // SEL_BLOCK)[:, None]
    blk = jnp.arange(n_blk)[None, :]
    causal_blk = (blk <= cur)[:, None, :]
    forced = ((blk == 0) | (blk >= cur - 1))[:, None, :] & causal_blk
    score = jnp.where(forced, jnp.inf, jnp.where(causal_blk, imp, -jnp.inf))
    top_score, sel_idx = lax.top_k(score, n_sel)
    sel_valid = top_score > -jnp.inf
    ks_blocks = k_sel.reshape(bsz, n_blk, SEL_BLOCK, G, hd).transpose(0, 3, 1, 2, 4)
    vs_blocks = v_sel.reshape(bsz, n_blk, SEL_BLOCK, G, hd).transpose(0, 3, 1, 2, 4)
    pad = ((0, 0), (WINDOW, 0), (0, 0), (0, 0))
    kw_pad = jnp.pad(k_win, pad)
    vw_pad = jnp.pad(v_win, pad)
    b_ix = jnp.arange(bsz)[:, None, None, None]
    g_ix = jnp.arange(G)[None, None, :, None]
    qb = NSA_QUERY_BLOCK

    def query_block(i):
        start = i * qb
        qi = lax.dynamic_slice_in_dim(qg, start, qb, axis=1)
        ti = start + jnp.arange(qb)
        idx = lax.dynamic_slice_in_dim(sel_idx, start, qb, axis=1)
        valid = lax.dynamic_slice_in_dim(sel_valid, start, qb, axis=1)
        kg = ks_blocks[b_ix, g_ix, idx]
        vg = vs_blocks[b_ix, g_ix, idx]
        tok = idx[..., None] * SEL_BLOCK + jnp.arange(SEL_BLOCK)
        m_sel = (valid[..., None] & (tok <= ti[None, :, None, None, None])).reshape(bsz, qb, G, 1, n_sel * SEL_BLOCK)
        s_sel = (jnp.einsum('bqgrd,bqgnkd->bqgrnk', qi, kg) * scale).reshape(bsz, qb, G, R, n_sel * SEL_BLOCK)
        p_sel = masked_softmax(s_sel, m_sel).reshape(bsz, qb, G, R, n_sel, SEL_BLOCK)
        o_sel = jnp.einsum('bqgrnk,bqgnkd->bqgrd', p_sel.astype(vg.dtype), vg)
        kw = lax.dynamic_slice_in_dim(kw_pad, start, qb + WINDOW, axis=1)
        vw = lax.dynamic_slice_in_dim(vw_pad, start, qb + WINDOW, axis=1)
        kp = start - WINDOW + jnp.arange(qb + WINDOW)
        m_win = (kp[None, :] <= ti[:, None]) & (kp[None, :] > ti[:, None] - WINDOW) & (kp[None, :] >= 0)
        s_win = jnp.einsum('bqgrd,bkgd->bqgrk', qi, kw) * scale
        p_win = masked_softmax(s_win, m_win[None, :, None, None, :])
        o_win = jnp.einsum('bqgrk,bkgd->bqgrd', p_win.astype(vw.dtype), vw)
        return o_sel, o_win

    o_sel, o_win = lax.map(query_block, jnp.arange(seq // qb))

    def unblock(o):
        return jnp.moveaxis(o, 0, 1).reshape(bsz, seq, G, R, hd)

    gate = jax.nn.sigmoid(gate_logits).reshape(bsz, seq, G, R, 3)
    out = gate[..., 0:1] * o_cmp + gate[..., 1:2] * unblock(o_sel) + gate[..., 2:3] * unblock(o_win)
    return out.reshape(bsz, seq, D_NSA)


def even_mixer(h, positions, w_in, conv_w, conv_b, dt_bias, a_log, d_skip, ssm_norm, q_gain, kc_gain, ks_gain, kw_gain, pe_k, pe_v, wk1, wk2, wv1, wv2, w_out):
    bsz, seq, _ = h.shape
    cuts = [int(c) for c in np.cumsum(EVEN_SPLITS)[:-1]]
    z, xbc, dt_raw, q, kc, vc, ks, vs, kw, vw, gl = jnp.split(h @ w_in, cuts, axis=-1)
    y_ssm = mamba2_group(z, xbc, dt_raw, conv_w, conv_b, dt_bias, a_log, d_skip, ssm_norm)

    def heads(u, n):
        return u.reshape(bsz, seq, n, NSA_HEAD_DIM)

    q = partial_rope(rms_norm(heads(q, NSA_HEADS), q_gain), positions)
    ks = partial_rope(rms_norm(heads(ks, NSA_KV_HEADS), ks_gain), positions)
    kw = partial_rope(rms_norm(heads(kw, NSA_KV_HEADS), kw_gain), positions)
    y_nsa = nsa_group(q, heads(kc, NSA_KV_HEADS), heads(vc, NSA_KV_HEADS), ks, heads(vs, NSA_KV_HEADS),
                      kw, heads(vw, NSA_KV_HEADS), gl, kc_gain, pe_k, pe_v, wk1, wk2, wv1, wv2)
    return jnp.concatenate([y_ssm, y_nsa], axis=-1) @ w_out


def wkv7_scan(r, w, k, v, a, b):
    def step(state, inp):
        r_t, w_t, k_t, v_t, a_t, b_t = inp
        sa = jnp.einsum('bhij,bhj->bhi', state, a_t)
        state = state * w_t[:, :, None, :] + sa[..., None] * b_t[:, :, None, :] + v_t[..., None] * k_t[:, :, None, :]
        return state, jnp.einsum('bhij,bhj->bhi', state, r_t)

    bsz, _, nh, n = r.shape
    xs = (jnp.moveaxis(r, 1, 0), jnp.moveaxis(w, 1, 0), jnp.moveaxis(k, 1, 0),
          jnp.moveaxis(v, 1, 0), jnp.moveaxis(a, 1, 0), jnp.moveaxis(b, 1, 0))
    _, y = lax.scan(step, jnp.zeros((bsz, nh, n, n), F32), xs)
    return jnp.moveaxis(y, 0, 1)


def rwkv7_time_mix(h, mu, w_r, w_k, w_v, w_o, w0, w1, w2, a0, a1, a2, g1, g2, k_k, k_a, r_k, ln_w, ln_b):
    bsz, seq, _ = h.shape
    xx = jnp.pad(h, ((0, 0), (1, 0), (0, 0)))[:, :-1] - h

    def mix(j):
        return h + xx * mu[j]

    def heads(u):
        return u.astype(F32).reshape(bsz, seq, RWKV_HEADS, RWKV_HEAD_DIM)

    r = mix(0) @ w_r
    w = -jax.nn.softplus(-(w0 + jnp.tanh(mix(1) @ w1) @ w2)) - 0.5
    k = mix(2) @ w_k
    v = mix(3) @ w_v
    a = jax.nn.sigmoid(a0 + (mix(4) @ a1) @ a2)
    g = jax.nn.sigmoid(mix(5) @ g1) @ g2
    kk = heads(k * k_k)
    kk = kk / jnp.maximum(jnp.sqrt(jnp.sum(kk * kk, axis=-1, keepdims=True)), 1e-12)
    kh = heads(k * (1 + (a - 1) * k_a))
    ah = heads(a)
    rh = heads(r)
    vh = heads(v)
    decay = jnp.exp(-jnp.exp(heads(w)))
    y = wkv7_scan(rh, decay, kh, vh, -kk, kk * ah)
    mean = jnp.mean(y, axis=-1, keepdims=True)
    var = jnp.mean(jnp.square(y - mean), axis=-1, keepdims=True)
    y = ((y - mean) * lax.rsqrt(var + RWKV_GN_EPS)).reshape(bsz, seq, D_MODEL) * ln_w.astype(F32) + ln_b.astype(F32)
    bonus = jnp.sum(rh * kh * r_k.astype(F32), axis=-1, keepdims=True) * vh
    y = (y + bonus.reshape(bsz, seq, D_MODEL)) * g.astype(F32)
    return y.astype(h.dtype) @ w_o


def memory_cross_attention(h, mem_n, w_q, w_kv, w_o, q_gain, k_gain):
    bsz, seq, _ = h.shape
    n_mem = mem_n.shape[1]
    q = rms_norm((h @ w_q).reshape(bsz, seq, XATTN_HEADS, XATTN_HEAD_DIM), q_gain)
    k, v = jnp.split(mem_n @ w_kv, 2, axis=-1)
    k = rms_norm(k.reshape(bsz, n_mem, XATTN_HEADS, XATTN_HEAD_DIM), k_gain)
    v = v.reshape(bsz, n_mem, XATTN_HEADS, XATTN_HEAD_DIM)
    s = jnp.einsum('bshd,bmhd->bhsm', q, k).astype(F32) * XATTN_HEAD_DIM ** -0.5
    p = jax.nn.softmax(s, axis=-1).astype(v.dtype)
    o = jnp.einsum('bhsm,bmhd->bshd', p, v).reshape(bsz, seq, D_XATTN)
    return o @ w_o


def swiglu(h, w1, w3, w2):
    return (jax.nn.silu(h @ w1) * (h @ w3)) @ w2


def moe_swiglu(h, w_router, w1, w3, w2):
    bsz, seq, d = h.shape
    hf = h.reshape(-1, d)
    n_tok = hf.shape[0]
    logits = (hf @ w_router).astype(F32)
    top_logit, top_e = lax.top_k(logits, TOP_K)
    gate = jax.nn.softmax(top_logit, axis=-1)
    n_assign = n_tok * TOP_K
    flat_e = top_e.reshape(n_assign)
    flat_tok = jnp.arange(n_assign, dtype=jnp.int32) // TOP_K
    flat_g = gate.reshape(n_assign)
    order = jnp.argsort(flat_e)
    e_sorted = flat_e[order]
    counts = jnp.bincount(flat_e, length=N_EXPERTS)
    padded = (counts + MOE_BLOCK - 1) // MOE_BLOCK * MOE_BLOCK
    start = jnp.cumsum(counts) - counts
    pstart = jnp.cumsum(padded) - padded
    dest = pstart[e_sorted] + jnp.arange(n_assign, dtype=jnp.int32) - start[e_sorted]
    n_blocks = -(-n_assign // MOE_BLOCK) + N_EXPERTS
    n_slots = n_blocks * MOE_BLOCK
    slot_tok = jnp.zeros((n_slots,), jnp.int32).at[dest].set(flat_tok[order])
    slot_gate = jnp.zeros((n_slots,), F32).at[dest].set(flat_g[order])
    block_e = jnp.minimum(jnp.sum(jnp.arange(n_blocks)[:, None] * MOE_BLOCK >= jnp.cumsum(padded)[None, :], axis=1), N_EXPERTS - 1)
    xb = hf[slot_tok].reshape(n_blocks, MOE_BLOCK, d)

    def expert_block(args):
        xblk, e = args
        return (jax.nn.silu(xblk @ w1[e]) * (xblk @ w3[e])) @ w2[e]

    yb = lax.map(expert_block, (xb, block_e)).reshape(n_slots, d)
    out = jnp.zeros_like(hf).at[slot_tok].add(yb * slot_gate[:, None].astype(yb.dtype))
    return out.reshape(bsz, seq, d)


def setup_inputs(seed: int = 0) -> dict:
    key = jax.random.key(seed)
    keys = iter(jax.random.split(key, 80))
    D, NE, NO, E = D_MODEL, N_EVEN, N_ODD, N_EXPERTS

    def nrm(shape, scale):
        return jax.random.normal(next(keys), shape, F32) * scale

    def gain(shape):
        return 1.0 + 0.02 * jax.random.normal(next(keys), shape, F32)

    def unif(shape, lo, hi):
        return jax.random.uniform(next(keys), shape, F32, lo, hi)

    dt_init = jnp.exp(unif((NE, SSM_HEADS), math.log(1e-3), math.log(1e-1)))
    return {
        'x': nrm((BATCH, SEQ, D), 1.0),
        'mem': nrm((BATCH, MEM_LEN, D), 1.0),
        'positions': jnp.arange(SEQ, dtype=jnp.int32)[None, :] + jax.random.randint(next(keys), (BATCH, 1), 0, 1024, dtype=jnp.int32),
        'norm_mix': gain((DEPTH, D)),
        'norm_xattn': gain((DEPTH, D)),
        'norm_mem': gain((DEPTH, D)),
        'norm_ffn': gain((DEPTH, D)),
        'xattn_wq': nrm((DEPTH, D, D_XATTN), D ** -0.5),
        'xattn_wkv': nrm((DEPTH, D, 2 * D_XATTN), D ** -0.5),
        'xattn_wo': nrm((DEPTH, D_XATTN, D), D_XATTN ** -0.5),
        'xattn_q_gain': gain((DEPTH, XATTN_HEAD_DIM)),
        'xattn_k_gain': gain((DEPTH, XATTN_HEAD_DIM)),
        'ev_w_in': nrm((NE, D, D_IN_EVEN), D ** -0.5),
        'ev_conv_w': nrm((NE, D_CONV, CONV_WIDTH), CONV_WIDTH ** -0.5),
        'ev_conv_b': nrm((NE, D_CONV), 0.02),
        'ev_dt_bias': dt_init + jnp.log(-jnp.expm1(-dt_init)),
        'ev_a_log': jnp.log(unif((NE, SSM_HEADS), 1.0, 16.0)),
        'ev_d_skip': gain((NE, SSM_HEADS)),
        'ev_ssm_norm': gain((NE, D_SSM)),
        'ev_q_gain': gain((NE, NSA_HEAD_DIM)),
        'ev_kc_gain': gain((NE, NSA_HEAD_DIM)),
        'ev_ks_gain': gain((NE, NSA_HEAD_DIM)),
        'ev_kw_gain': gain((NE, NSA_HEAD_DIM)),
        'ev_pe_k': nrm((NE, CMP_BLOCK, NSA_HEAD_DIM), 0.02),
        'ev_pe_v': nrm((NE, CMP_BLOCK, NSA_HEAD_DIM), 0.02),
        'ev_cmp_wk1': nrm((NE, CMP_BLOCK * NSA_HEAD_DIM, CMP_HIDDEN), (CMP_BLOCK * NSA_HEAD_DIM) ** -0.5),
        'ev_cmp_wk2': nrm((NE, CMP_HIDDEN, NSA_HEAD_DIM), CMP_HIDDEN ** -0.5),
        'ev_cmp_wv1': nrm((NE, CMP_BLOCK * NSA_HEAD_DIM, CMP_HIDDEN), (CMP_BLOCK * NSA_HEAD_DIM) ** -0.5),
        'ev_cmp_wv2': nrm((NE, CMP_HIDDEN, NSA_HEAD_DIM), CMP_HIDDEN ** -0.5),
        'ev_w_out': nrm((NE, D_MIX_EVEN, D), D_MIX_EVEN ** -0.5),
        'ev_ffn_w1': nrm((NE, D, D_FF_DENSE), D ** -0.5),
        'ev_ffn_w3': nrm((NE, D, D_FF_DENSE), D ** -0.5),
        'ev_ffn_w2': nrm((NE, D_FF_DENSE, D), D_FF_DENSE ** -0.5),
        'od_mu': unif((NO, N_TOKEN_MIX, D), 0.0, 1.0),
        'od_w_r': nrm((NO, D, D), D ** -0.5),
        'od_w_k': nrm((NO, D, D), D ** -0.5),
        'od_w_v': nrm((NO, D, D), D ** -0.5),
        'od_w_o': nrm((NO, D, D), D ** -0.5),
        'od_w0': unif((NO, D), -5.5, -0.5),
        'od_w1': nrm((NO, D, DECAY_LORA), D ** -0.5),
        'od_w2': nrm((NO, DECAY_LORA, D), 0.5 * DECAY_LORA ** -0.5),
        'od_a0': nrm((NO, D), 0.1),
        'od_a1': nrm((NO, D, AAA_LORA), D ** -0.5),
        'od_a2': nrm((NO, AAA_LORA, D), 0.5 * AAA_LORA ** -0.5),
        'od_g1': nrm((NO, D, GATE_LORA), D ** -0.5),
        'od_g2': nrm((NO, GATE_LORA, D), GATE_LORA ** -0.5),
        'od_k_k': 0.85 * gain((NO, D)),
        'od_k_a': gain((NO, D)),
        'od_r_k': nrm((NO, RWKV_HEADS, RWKV_HEAD_DIM), 0.1),
        'od_ln_w': gain((NO, D)),
        'od_ln_b': nrm((NO, D), 0.02),
        'od_router': nrm((NO, D, E), D ** -0.5),
        'od_moe_w1': nrm((NO, E, D, D_FF_EXPERT), D ** -0.5),
        'od_moe_w3': nrm((NO, E, D, D_FF_EXPERT), D ** -0.5),
        'od_moe_w2': nrm((NO, E, D_FF_EXPERT, D), D_FF_EXPERT ** -0.5),
    }


def reference(x, mem, positions, norm_mix, norm_xattn, norm_mem, norm_ffn,
              xattn_wq, xattn_wkv, xattn_wo, xattn_q_gain, xattn_k_gain,
              ev_w_in, ev_conv_w, ev_conv_b, ev_dt_bias, ev_a_log, ev_d_skip, ev_ssm_norm,
              ev_q_gain, ev_kc_gain, ev_ks_gain, ev_kw_gain,
              ev_pe_k, ev_pe_v, ev_cmp_wk1, ev_cmp_wk2, ev_cmp_wv1, ev_cmp_wv2,
              ev_w_out, ev_ffn_w1, ev_ffn_w3, ev_ffn_w2,
              od_mu, od_w_r, od_w_k, od_w_v, od_w_o, od_w0, od_w1, od_w2,
              od_a0, od_a1, od_a2, od_g1, od_g2, od_k_k, od_k_a, od_r_k, od_ln_w, od_ln_b,
              od_router, od_moe_w1, od_moe_w3, od_moe_w2):
    h = x
    for layer in range(DEPTH):
        i = layer // 2
        hn = rms_norm(h, norm_mix[layer])
        if layer % 2 == 0:
            h = h + even_mixer(hn, positions, ev_w_in[i], ev_conv_w[i], ev_conv_b[i], ev_dt_bias[i],
                               ev_a_log[i], ev_d_skip[i], ev_ssm_norm[i], ev_q_gain[i], ev_kc_gain[i],
                               ev_ks_gain[i], ev_kw_gain[i], ev_pe_k[i], ev_pe_v[i], ev_cmp_wk1[i],
                               ev_cmp_wk2[i], ev_cmp_wv1[i], ev_cmp_wv2[i], ev_w_out[i])
        else:
            h = h + rwkv7_time_mix(hn, od_mu[i], od_w_r[i], od_w_k[i], od_w_v[i], od_w_o[i], od_w0[i],
                                   od_w1[i], od_w2[i], od_a0[i], od_a1[i], od_a2[i], od_g1[i], od_g2[i],
                                   od_k_k[i], od_k_a[i], od_r_k[i], od_ln_w[i], od_ln_b[i])
        h = h + memory_cross_attention(rms_norm(h, norm_xattn[layer]), rms_norm(mem, norm_mem[layer]),
                                       xattn_wq[layer], xattn_wkv[layer], xattn_wo[layer],
                                       xattn_q_gain[layer], xattn_k_gain[layer])
        hn = rms_norm(h, norm_ffn[layer])
        if layer % 2 == 0:
            h = h + swiglu(hn, ev_ffn_w1[i], ev_ffn_w3[i], ev_ffn_w2[i])
        else:
            h = h + moe_swiglu(hn, od_router[i], od_moe_w1[i], od_moe_w3[i], od_moe_w2[i])
    return h
```

```python
import math
import numpy as np
import ml_dtypes
import concourse.bass as bass
import concourse.mybir as mybir
from concourse.bass_utils import run_bass_kernel_spmd
from contextlib import ExitStack

F32 = mybir.dt.float32
BF16 = mybir.dt.bfloat16
I32 = mybir.dt.int32
ALU = mybir.AluOpType
AF = mybir.ActivationFunctionType
AX = mybir.AxisListType

D = 2048
KC = D // 128
NORM_EPS = 1e-6


class Buf:
    __slots__ = ("name", "t", "last_w", "readers")

    def __init__(self, name, t):
        self.name = name
        self.t = t
        self.last_w = None
        self.readers = []

    def __getitem__(self, k):
        return self.t[k]


class Sched:
    def __init__(self, nc):
        self.nc = nc
        self.ops = []
        self.es = ExitStack()
        self.uid = 0

    def sb(self, name, shape, dt):
        self.uid += 1
        nm = f"{name}_{self.uid}"
        es = self.phase_es if getattr(self, "phase_es", None) is not None else self.es
        return Buf(nm, es.enter_context(self.nc.sbuf_tensor(nm, list(shape), dt)))

    def phase_begin(self):
        self.phase_es = ExitStack()

    def phase_end(self):
        self.ops.append(dict(barrier=True))
        self.phase_es.close()
        self.phase_es = None

    def ps(self, name, shape, dt=F32):
        self.uid += 1
        nm = f"{name}_{self.uid}"
        return Buf(nm, self.es.enter_context(self.nc.psum_tensor(nm, list(shape), dt)))

    def dram(self, name, shape, dt, kind="Internal"):
        return Buf(name, self.nc.dram_tensor(name, list(shape), dt, kind=kind).ap())

    def op(self, eng, fn, reads=(), writes=(), dma=False, owner=None):
        self.ops.append(dict(eng=eng, fn=fn, reads=list(reads), writes=list(writes), dma=dma, owner=owner))

    def dma(self, q, out_ap, in_ap, reads, writes, owner, **kw):
        self.op(q, lambda e: e.dma_start(out=out_ap, in_=in_ap, **kw), reads, writes, dma=True, owner=owner)

    def emit(self):
        nc = self.nc
        ops = self.ops
        n = len(ops)
        deps = [None] * n
        needed = [False] * n
        last_on = {}
        bar_deps = []
        pending = set()
        for i, o in enumerate(ops):
            if o.get("barrier"):
                bar_deps = list(last_on.values())
                pending = set(["pe", "act", "dve", "pool", "sp"])
                deps[i] = []
                o.update(eng=None, dma=False, reads=[], writes=[])
                continue
            d = set()
            if bar_deps and o["eng"] in pending:
                d.update(bar_deps)
                pending.discard(o["eng"])
            last_on[("dma", o["owner"].name) if o["dma"] else (o["eng"],)] = i
            for r in o["reads"]:
                if r.last_w is not None:
                    d.add(r.last_w)
            for w in o["writes"]:
                if w.last_w is not None:
                    d.add(w.last_w)
                d.update(w.readers)
            d.discard(i)
            dd = []
            for j in d:
                oj = ops[j]
                if (not oj["dma"]) and (not o["dma"]) and oj["eng"] == o["eng"] == "pe":
                    continue
                dd.append(j)
            deps[i] = dd
            for j in dd:
                needed[j] = True
            for r in o["reads"]:
                r.readers.append(i)
            for w in o["writes"]:
                w.last_w = i
                w.readers = []
        engs = ["pe", "act", "dve", "pool", "sp"]
        esem = {e: self.es.enter_context(nc.semaphore("s_" + e)) for e in engs}
        ecount = {e: 0 for e in engs}
        dsem = {}
        dcount = {}
        tok = [None] * n
        for i, o in enumerate(ops):
            if o.get("barrier"):
                continue
            if o["dma"]:
                ow = o["owner"]
                if ow not in dsem:
                    dsem[ow] = self.es.enter_context(nc.semaphore("d_" + ow.name))
                    dcount[ow] = 0
                dcount[ow] += 16
                tok[i] = (dsem[ow], dcount[ow], 16)
            elif needed[i]:
                ecount[o["eng"]] += 1
                tok[i] = (esem[o["eng"]], ecount[o["eng"]], 1)
        streams = {e: [] for e in engs}
        seen = {e: {} for e in engs}
        for i, o in enumerate(ops):
            if o.get("barrier"):
                continue
            e = o["eng"]
            waits = {}
            for j in deps[i]:
                s, v, _ = tok[j]
                if seen[e].get(id(s), 0) >= v:
                    continue
                if waits.get(id(s), (None, 0))[1] < v:
                    waits[id(s)] = (s, v)
            for k, (s, v) in waits.items():
                seen[e][k] = v
            streams[e].append((list(waits.values()), o["fn"], tok[i]))

        def run_stream(engine, lst):
            for waits, fn, t in lst:
                for s, v in waits:
                    engine.wait_ge(s, v)
                ins = fn(engine)
                if t is not None:
                    ins.then_inc(t[0], t[2])

        with nc.Block() as block:
            @block.tensor
            def _(eng):
                run_stream(eng, streams["pe"])

            @block.scalar
            def _(eng):
                run_stream(eng, streams["act"])

            @block.vector
            def _(eng):
                run_stream(eng, streams["dve"])

            @block.gpsimd
            def _(eng):
                run_stream(eng, streams["pool"])

            @block.sync
            def _(eng):
                run_stream(eng, streams["sp"])
                for ow, s in dsem.items():
                    eng.wait_ge(s, dcount[ow])
        self.es.close()


class Rot:
    def __init__(self, bufs):
        self.bufs = bufs
        self.i = 0

    def next(self):
        b = self.bufs[self.i % len(self.bufs)]
        self.i += 1
        return b


class K:
    def __init__(self, nc, n_tp=2, n_acc=4):
        self.nc = nc
        self.S = Sched(nc)
        S = self.S
        self.ident_d = S.dram("c_ident", [128, 128], F32, kind="ExternalInput")
        self.ident = S.sb("ident", [128, 128], BF16)
        S.dma("pool", self.ident[:], self.ident_d[:], [self.ident_d], [self.ident], self.ident)
        self.tp = Rot([S.ps("tp", [128, 1024], BF16) for _ in range(n_tp)])
        self.acc = Rot([S.ps("acc", [128, 512], F32) for _ in range(n_acc)])
        self.junk = S.sb("junk", [128, 2048], BF16)
        self.stat = Rot([S.sb("stat", [128, 8], F32) for _ in range(6)])
        self.wq = 0

    def dq(self):
        return "sp"

    def dump(self, name, buf, ap, shape, dt=F32):
        if not getattr(self, "debug", False):
            return
        S = self.S
        d = S.dram("dbg_" + name, list(shape), dt, kind="ExternalOutput")
        S.dma("sp", d[:], ap, [buf], [d], d)

    def gain_bc(self, name, ap_row, n):
        S = self.S
        b = S.sb(name, [128, n], F32)
        src = ap_row.t if isinstance(ap_row, Buf) else ap_row
        S.dma("sp", b[:], src.partition_broadcast(128), [], [b], b)
        return b

    def rms_rstd(self, x, xap, n, eps=NORM_EPS):
        S = self.S
        st = self.stat.next()
        junk = self.junk
        S.op("act", lambda e: e.activation(out=junk[:, 0:n], in_=xap, func=AF.Square, accum_out=st[:, 0:1]), [x], [junk, st])
        S.op("dve", lambda e: e.tensor_scalar(out=st[:, 1:2], in0=st[:, 0:1], scalar1=1.0 / n, scalar2=eps, op0=ALU.mult, op1=ALU.add), [st], [st])
        S.op("act", lambda e: e.activation(out=st[:, 3:4], in_=st[:, 1:2], func=AF.Sqrt), [st], [st])
        S.op("dve", lambda e: e.reciprocal(out=st[:, 2:3], in_=st[:, 3:4]), [st], [st])
        return st

    def rmsnorm(self, x, xap, gain, gap, out, oap, n, eps=NORM_EPS):
        S = self.S
        st = self.rms_rstd(x, xap, n, eps)
        S.op("dve", lambda e: e.scalar_tensor_tensor(out=oap, in0=xap, scalar=st[:, 2:3], in1=gap, op0=ALU.mult, op1=ALU.mult), [x, st, gain], [out])

    def transpose_to(self, src, src_ap_fn, nchunks, dst, dst_ap_fn, eng_rot=("dve", "act")):
        S = self.S
        for c0 in range(0, nchunks, 8):
            c1 = min(nchunks, c0 + 8)
            tp = self.tp.next()
            for c in range(c0, c1):
                S.op("pe", lambda e, c=c, tp=tp, c0=c0: e.transpose(out=tp[:, (c - c0) * 128:(c - c0 + 1) * 128], in_=src_ap_fn(c), identity=self.ident[:]), [src, self.ident], [tp])
            eng = eng_rot[(c0 // 8) % len(eng_rot)]
            if eng == "dve":
                S.op("dve", lambda e, tp=tp, c0=c0, c1=c1: e.tensor_copy(out=dst_ap_fn(c0, c1), in_=tp[:, 0:(c1 - c0) * 128]), [tp], [dst])
            else:
                S.op("act", lambda e, tp=tp, c0=c0, c1=c1: e.activation(out=dst_ap_fn(c0, c1), in_=tp[:, 0:(c1 - c0) * 128], func=AF.Copy), [tp], [dst])


def _ident():
    return np.eye(128, dtype=np.float32)


BLK = 256
W2W = 128


class Swiglu:
    def __init__(self, k, H):
        S = k.S
        self.k = k
        self.H = H
        self.HC = H // 128
        self.w1g = Rot([S.sb("w1g", [128, KC, 256], BF16) for _ in range(2)])
        self.w3g = Rot([S.sb("w3g", [128, KC, 256], BF16) for _ in range(2)])
        self.w2n = Rot([S.sb("w2n", [128, self.HC, W2W], BF16) for _ in range(1)])
        self.actT = S.sb("actT", [128, self.HC, BLK], BF16)
        self.sg = Rot([S.sb("sg", [128, BLK], F32) for _ in range(2)])

    def run(self, xT, w1d, w3d, w2d, out_cb, gate=None):
        k, S, HC = self.k, self.k.S, self.HC
        w1v = w1d.t.rearrange("(kc p) n -> p kc n", p=128)
        w3v = w3d.t.rearrange("(kc p) n -> p kc n", p=128)
        w2v = w2d.t.rearrange("(c p) n -> p c n", p=128)
        actT = self.actT
        for hg in range(self.H // 256):
            w1g = self.w1g.next()
            w3g = self.w3g.next()
            S.dma("pool", w1g[:], w1v[:, :, hg * 256:(hg + 1) * 256], [w1d], [w1g], w1g)
            S.dma("pool", w3g[:], w3v[:, :, hg * 256:(hg + 1) * 256], [w3d], [w3g], w3g)
            for c in range(2):
                ga = k.acc.next()
                ua = k.acc.next()
                for kc in range(KC):
                    S.op("pe", lambda e, ga=ga, w1g=w1g, kc=kc, c=c: e.matmul(ga[:, 0:BLK], lhsT=w1g[:, kc, c * 128:(c + 1) * 128], rhs=xT[:, kc, :], start=(kc == 0), stop=(kc == KC - 1)), [w1g, xT], [ga])
                for kc in range(KC):
                    S.op("pe", lambda e, ua=ua, w3g=w3g, kc=kc, c=c: e.matmul(ua[:, 0:BLK], lhsT=w3g[:, kc, c * 128:(c + 1) * 128], rhs=xT[:, kc, :], start=(kc == 0), stop=(kc == KC - 1)), [w3g, xT], [ua])
                sg = self.sg.next()
                S.op("act", lambda e, sg=sg, ga=ga: e.activation(out=sg[:], in_=ga[:, 0:BLK], func=AF.Silu), [ga], [sg])
                hc = hg * 2 + c
                S.op("dve", lambda e, sg=sg, ua=ua, hc=hc: e.tensor_tensor(out=actT[:, hc, :], in0=sg[:], in1=ua[:, 0:BLK], op=ALU.mult), [sg, ua], [actT])
        for nn in range(D // W2W):
            w2n = self.w2n.next()
            S.dma("pool", w2n[:], w2v[:, :, nn * W2W:(nn + 1) * W2W], [w2d], [w2n], w2n)
            for tt in range(BLK // 128):
                acc = k.acc.next()
                for c in range(HC):
                    S.op("pe", lambda e, acc=acc, w2n=w2n, c=c, tt=tt: e.matmul(acc[:, 0:W2W], lhsT=actT[:, c, tt * 128:(tt + 1) * 128], rhs=w2n[:, c, :], start=(c == 0), stop=(c == HC - 1)), [actT, w2n], [acc])
                out_cb(tt, nn * W2W, (nn + 1) * W2W, acc)


XH, XD, MEM = 4, 128, 256


def build_mid(T, n_part, H, moe, debug=False):
    nc = bass.Bass("TRN2", target_bir_lowering=False)
    k = K(nc)
    k.debug = debug
    S = k.S
    NT = T // 128
    NB = BLK // 128
    xres = S.dram("xres", [T, D], F32, kind="ExternalInput")
    parts = S.dram("parts", [n_part, T, D], BF16, kind="ExternalInput") if n_part else None
    mem = S.dram("mem", [MEM, D], F32, kind="ExternalInput")
    g_x = S.dram("g_x", [D], F32, kind="ExternalInput")
    g_m = S.dram("g_m", [D], F32, kind="ExternalInput")
    g_f = S.dram("g_f", [D], F32, kind="ExternalInput")
    wq = S.dram("wq", [D, 512], F32, kind="ExternalInput")
    wkv = S.dram("wkv", [D, 1024], F32, kind="ExternalInput")
    wo = S.dram("wo", [512, D], F32, kind="ExternalInput")
    qg = S.dram("qg", [128], F32, kind="ExternalInput")
    kg = S.dram("kg", [128], F32, kind="ExternalInput")
    h_out = S.dram("h_out", [T, D], F32, kind="ExternalOutput")
    if moe:
        router = S.dram("router", [D, 8], F32, kind="ExternalInput")
        hn_out = S.dram("hn_out", [T, D], BF16, kind="ExternalOutput")
        gate_out = S.dram("gate_out", [T, 8], F32, kind="ExternalOutput")
    else:
        w1 = S.dram("w1", [D, H], F32, kind="ExternalInput")
        w3 = S.dram("w3", [D, H], F32, kind="ExternalInput")
        w2 = S.dram("w2", [H, D], F32, kind="ExternalInput")

    gx_bc = k.gain_bc("gx", g_x, D)
    gf_bc = k.gain_bc("gf", g_f, D)
    qg_bc = k.gain_bc("qg", qg, 128)
    kg_bc = k.gain_bc("kg", kg, 128)
    hkeep = S.sb("hkeep", [128, NB, D], F32)
    gm_ap = hkeep[:, 0, :]
    S.dma("sp", gm_ap, g_m.t.partition_broadcast(128), [], [hkeep], hkeep)
    wq_s = S.sb("wq_s", [128, KC, 512], BF16)
    S.dma("pool", wq_s[:], wq.t.rearrange("(kc p) n -> p kc n", p=128), [wq], [wq_s], wq_s)
    wo_s = S.sb("wo_s", [128, 4, D], BF16)
    S.dma("pool", wo_s[:], wo.t.rearrange("(kc p) n -> p kc n", p=128), [wo], [wo_s], wo_s)
    if moe:
        wkvbuf = Rot([S.sb("wkvg", [128, KC, 256], BF16) for _ in range(2)])
    else:
        sw = Swiglu(k, H)
        wkvbuf = sw.w1g
    hbuf = Rot([S.sb("h_f", [128, D], F32) for _ in range(2)])
    pbuf = Rot([S.sb("p_b", [128, D], BF16) for _ in range(2)]) if n_part else None
    hxbuf = Rot([S.sb("hx", [128, D], BF16) for _ in range(2)])
    hxTbuf = Rot([S.sb("hxT", [128, KC, 128], BF16) for _ in range(2)])
    hfT = S.sb("hfT", [128, KC, BLK], BF16)
    kv = S.sb("kv_f", [128, 1024], F32)
    kn = S.sb("k_n", [128, 512], BF16)
    KT = S.sb("KT", [128, XH, MEM], BF16)
    Vx = S.sb("Vx", [128, 2, XH, 132], BF16)
    qf = S.sb("q_f", [128, 512], F32)
    qn = S.sb("q_n", [128, 512], BF16)
    qT = S.sb("qT", [128, XH, 128], BF16)
    pT = S.sb("pT", [128, XH, 2, 128], BF16)
    on = S.sb("o_n", [128, 512], BF16)
    onT = S.sb("o_nT", [128, 4, 128], BF16)
    rs = S.sb("rs", [128, XH], F32)

    S.op("pool", lambda e: e.memset(Vx[:], 1.0), [], [Vx])
    mTs = []
    for mt in range(2):
        mt_f = hbuf.next()
        S.dma("sp", mt_f[:], mem[mt * 128:(mt + 1) * 128, :], [mem], [mt_f], mt_f)
        mn = hxbuf.next()
        k.rmsnorm(mt_f, mt_f[:], hkeep, gm_ap, mn, mn[:], D)
        mT = hxTbuf.next()
        k.transpose_to(mn, lambda c, mn=mn: mn[:, c * 128:(c + 1) * 128], KC, mT, lambda c0, c1, mT=mT: mT[:, c0:c1, :])
        mTs.append(mT)
    wkv_r = wkv.t.rearrange("(kc p) n -> p kc n", p=128)
    kvs = [kv, S.sb("kv_f2", [128, 1024], F32)]
    for nn in range(4):
        wg = wkvbuf.next()
        S.dma("pool", wg[:], wkv_r[:, :, nn * 256:(nn + 1) * 256], [wkv], [wg], wg)
        for mt in range(2):
            acc = k.acc.next()
            for kc in range(KC):
                S.op("pe", lambda e, acc=acc, kc=kc, mt=mt, wg=wg: e.matmul(acc[:, 0:256], lhsT=mTs[mt][:, kc, :], rhs=wg[:, kc, :], start=(kc == 0), stop=(kc == KC - 1)), [mTs[mt], wg], [acc])
            S.op("act", lambda e, acc=acc, nn=nn, mt=mt: e.activation(out=kvs[mt][:, nn * 256:(nn + 1) * 256], in_=acc[:, 0:256], func=AF.Copy), [acc], [kvs[mt]])
    for mt in range(2):
        kvm = kvs[mt]
        for h in range(XH):
            k.rmsnorm(kvm, kvm[:, h * 128:(h + 1) * 128], kg_bc, kg_bc[:], kn, kn[:, h * 128:(h + 1) * 128], 128)
        k.transpose_to(kn, lambda c: kn[:, c * 128:(c + 1) * 128], XH, KT, lambda c0, c1, mt=mt: KT[:, c0:c1, mt * 128:(mt + 1) * 128])
        S.op("dve", lambda e, mt=mt, kvm=kvm: e.tensor_copy(out=Vx[:, mt, :, 0:128], in_=kvm[:, 512:1024].rearrange("p (h d) -> p h d", h=XH)), [kvm], [Vx])

    if moe:
        hf32 = S.sb("hf32", [128, D], F32)
        rt_s = S.sb("rt_s", [128, KC, 8], F32)
        S.dma("sp", rt_s[:], router.t.rearrange("(kc p) e -> p kc e", p=128), [router], [rt_s], rt_s)
        ident_f = S.sb("ident_f", [128, 128], F32)
        S.dma("sp", ident_f[:], k.ident_d[:], [k.ident_d], [ident_f], ident_f)
        hf32T = S.sb("hf32T", [128, KC, 128], F32)
        lg = S.sb("lg", [128, 8], F32)
        m8 = S.sb("m8", [128, 8], F32)
        gt = S.sb("gt", [128, 8], F32)

    scale = XD ** -0.5
    for tt in range(NT):
        bt = tt % NB
        h = hbuf.next()
        S.dma("sp", h[:], xres[tt * 128:(tt + 1) * 128, :], [xres], [h], h)
        for p in range(n_part):
            pb = pbuf.next()
            S.dma("sp", pb[:], parts[p, tt * 128:(tt + 1) * 128, :], [parts], [pb], pb)
            S.op("dve", lambda e, h=h, pb=pb: e.tensor_tensor(out=h[:], in0=h[:], in1=pb[:], op=ALU.add), [h, pb], [h])
        if tt == 0:
            k.dump("h0", h, h[:], [128, D])
        hx = hxbuf.next()
        k.rmsnorm(h, h[:], gx_bc, gx_bc[:], hx, hx[:], D)
        if tt == 0:
            k.dump("hx", hx, hx[:], [128, D], BF16)
        hxT = hxTbuf.next()
        k.transpose_to(hx, lambda c, hx=hx: hx[:, c * 128:(c + 1) * 128], KC, hxT, lambda c0, c1, hxT=hxT: hxT[:, c0:c1, :])
        acc = k.acc.next()
        for kc in range(KC):
            S.op("pe", lambda e, acc=acc, kc=kc, hxT=hxT: e.matmul(acc[:], lhsT=hxT[:, kc, :], rhs=wq_s[:, kc, :], start=(kc == 0), stop=(kc == KC - 1)), [hxT, wq_s], [acc])
        S.op("act", lambda e, acc=acc: e.activation(out=qf[:], in_=acc[:], func=AF.Copy), [acc], [qf])
        if tt == 0:
            k.dump("qf", qf, qf[:], [128, 512])
        for hh in range(XH):
            k.rmsnorm(qf, qf[:, hh * 128:(hh + 1) * 128], qg_bc, qg_bc[:], qn, qn[:, hh * 128:(hh + 1) * 128], 128)
        if tt == 0:
            k.dump("qn", qn, qn[:], [128, 512], BF16)
            k.dump("KT", KT, KT[:], [128, XH, MEM], BF16)
            k.dump("Vx", Vx, Vx[:], [128, 2, XH, 132], BF16)
        k.transpose_to(qn, lambda c: qn[:, c * 128:(c + 1) * 128], XH, qT, lambda c0, c1: qT[:, c0:c1, :])
        for half in range(2):
            sacc = k.acc.next()
            for j in range(4):
                hh, mt = (half * 4 + j) // 2, (half * 4 + j) % 2
                S.op("pe", lambda e, sacc=sacc, j=j, hh=hh, mt=mt: e.matmul(sacc[:, j * 128:(j + 1) * 128], lhsT=KT[:, hh, mt * 128:(mt + 1) * 128], rhs=qT[:, hh, :], start=True, stop=True), [KT, qT], [sacc])
            S.op("act", lambda e, sacc=sacc, half=half: e.activation(out=pT[:, half * 2:(half + 1) * 2, :, :].rearrange("p a b q -> p (a b q)"), in_=sacc[:], func=AF.Exp, scale=scale), [sacc], [pT])
        oacc = k.acc.next()
        for hh in range(XH):
            for mt in range(2):
                S.op("pe", lambda e, oacc=oacc, hh=hh, mt=mt: e.matmul(oacc[:, hh * 128:hh * 128 + 128], lhsT=pT[:, hh, mt, :], rhs=Vx[:, mt, hh, 0:128], start=(mt == 0), stop=(mt == 1)), [pT, Vx], [oacc])
        racc = k.acc.next()
        for hh in range(XH):
            for mt in range(2):
                S.op("pe", lambda e, racc=racc, hh=hh, mt=mt: e.matmul(racc[:, hh:hh + 1], lhsT=pT[:, hh, mt, :], rhs=Vx[:, mt, hh, 128:129], start=(mt == 0), stop=(mt == 1)), [pT, Vx], [racc])
        S.op("dve", lambda e, racc=racc: e.reciprocal(out=rs[:], in_=racc[:, 0:XH]), [racc], [rs])
        for hh in range(XH):
            S.op("dve", lambda e, oacc=oacc, hh=hh: e.tensor_scalar(out=on[:, hh * 128:(hh + 1) * 128], in0=oacc[:, hh * 128:(hh + 1) * 128], scalar1=rs[:, hh:hh + 1], scalar2=None, op0=ALU.mult), [oacc, rs], [on])
        if tt == 0:
            k.dump("pT", pT, pT[:], [128, XH, 2, 128], BF16)
            k.dump("on", on, on[:], [128, 512], BF16)
        k.transpose_to(on, lambda c: on[:, c * 128:(c + 1) * 128], 4, onT, lambda c0, c1: onT[:, c0:c1, :])
        for nn in range(4):
            acc = k.acc.next()
            for kc in range(4):
                S.op("pe", lambda e, acc=acc, kc=kc, nn=nn: e.matmul(acc[:], lhsT=onT[:, kc, :], rhs=wo_s[:, kc, nn * 512:(nn + 1) * 512], start=(kc == 0), stop=(kc == 3)), [onT, wo_s], [acc])
            S.op("dve", lambda e, acc=acc, nn=nn, h=h: e.tensor_tensor(out=h[:, nn * 512:(nn + 1) * 512], in0=h[:, nn * 512:(nn + 1) * 512], in1=acc[:], op=ALU.add), [acc, h], [h])
        hf = hx
        k.rmsnorm(h, h[:], gf_bc, gf_bc[:], hf, hf[:], D)
        if moe:
            S.dma("sp", h_out[tt * 128:(tt + 1) * 128, :], h[:], [h], [h_out], h)
            S.dma("sp", hn_out[tt * 128:(tt + 1) * 128, :], hf[:], [hf], [hn_out], hf)
            k.rmsnorm(h, h[:], gf_bc, gf_bc[:], hf32, hf32[:], D)
            for c0 in range(0, KC, 4):
                tacc = k.acc.next()
                for c in range(c0, c0 + 4):
                    S.op("pe", lambda e, tacc=tacc, c=c, c0=c0: e.transpose(out=tacc[:, (c - c0) * 128:(c - c0 + 1) * 128], in_=hf32[:, c * 128:(c + 1) * 128], identity=ident_f[:]), [hf32, ident_f], [tacc])
                S.op("dve", lambda e, tacc=tacc, c0=c0: e.tensor_copy(out=hf32T[:, c0:c0 + 4, :].rearrange("p a b -> p (a b)"), in_=tacc[:]), [tacc], [hf32T])
            lacc = k.acc.next()
            for kc in range(KC):
                S.op("pe", lambda e, lacc=lacc, kc=kc: e.matmul(lacc[:, 0:8], lhsT=hf32T[:, kc, :], rhs=rt_s[:, kc, :], start=(kc == 0), stop=(kc == KC - 1)), [hf32T, rt_s], [lacc])
            S.op("dve", lambda e, lacc=lacc: e.tensor_copy(out=lg[:], in_=lacc[:, 0:8]), [lacc], [lg])
            S.op("dve", lambda e: e.max(out=m8[:], in_=lg[:]), [lg], [m8])
            S.op("dve", lambda e: e.tensor_scalar(out=gt[:], in0=lg[:], scalar1=m8[:, 1:2], scalar2=None, op0=ALU.is_ge), [lg, m8], [gt])
            S.op("dve", lambda e: e.tensor_scalar(out=lg[:], in0=lg[:], scalar1=m8[:, 0:1], scalar2=None, op0=ALU.subtract), [lg, m8], [lg])
            S.op("act", lambda e: e.activation(out=lg[:], in_=lg[:], func=AF.Exp), [lg], [lg])
            S.op("dve", lambda e: e.tensor_tensor(out=gt[:], in0=gt[:], in1=lg[:], op=ALU.mult), [gt, lg], [gt])
            S.op("dve", lambda e: e.reduce_sum(out=m8[:, 2:3], in_=gt[:], axis=AX.X), [gt], [m8])
            S.op("dve", lambda e: e.reciprocal(out=m8[:, 3:4], in_=m8[:, 2:3]), [m8], [m8])
            S.op("dve", lambda e: e.tensor_scalar(out=gt[:], in0=gt[:], scalar1=m8[:, 3:4], scalar2=None, op0=ALU.mult), [gt, m8], [gt])
            S.dma("sp", gate_out[tt * 128:(tt + 1) * 128, :], gt[:], [gt], [gate_out], gt)
        else:
            k.transpose_to(hf, lambda c, hf=hf: hf[:, c * 128:(c + 1) * 128], KC, hfT, lambda c0, c1, bt=bt: hfT[:, c0:c1, bt * 128:(bt + 1) * 128])
            S.op("pool", lambda e, h=h, bt=bt: e.tensor_copy(out=hkeep[:, bt, :], in_=h[:]), [h], [hkeep])
            if bt == NB - 1:
                t0 = tt - bt

                def out_cb(tq, n0, n1, acc, t0=t0):
                    S.op("dve", lambda e: e.tensor_tensor(out=hkeep[:, tq, n0:n1], in0=hkeep[:, tq, n0:n1], in1=acc[:, 0:n1 - n0], op=ALU.add), [acc, hkeep], [hkeep])
                    if n1 == D:
                        S.dma("sp", h_out[(t0 + tq) * 128:(t0 + tq + 1) * 128, :], hkeep[:, tq, :], [hkeep], [h_out], hkeep)
                sw.run(hfT, w1, w3, w2, out_cb)
    S.emit()
    return nc


def build_moe(T, H):
    nc = bass.Bass("TRN2", target_bir_lowering=False)
    k = K(nc)
    S = k.S
    NB = BLK // 128
    hn = S.dram("hn", [T, D], BF16, kind="ExternalInput")
    gate = S.dram("gate", [T, 1], F32, kind="ExternalInput")
    w1 = S.dram("w1", [D, H], F32, kind="ExternalInput")
    w3 = S.dram("w3", [D, H], F32, kind="ExternalInput")
    w2 = S.dram("w2", [H, D], F32, kind="ExternalInput")
    part = S.dram("part", [T, D], BF16, kind="ExternalOutput")
    sw = Swiglu(k, H)
    hbuf = Rot([S.sb("hn_t", [128, D], BF16) for _ in range(2)])
    hT = S.sb("hT", [128, KC, BLK], BF16)
    gts = S.sb("gts", [128, NB], F32)
    obuf = S.sb("obuf", [128, NB, D], BF16)
    for blk in range(T // BLK):
        for bt in range(NB):
            t0 = blk * BLK + bt * 128
            hb = hbuf.next()
            S.dma("sp", hb[:], hn[t0:t0 + 128, :], [hn], [hb], hb)
            S.dma("sp", gts[:, bt:bt + 1], gate[t0:t0 + 128, :], [gate], [gts], gts)
            k.transpose_to(hb, lambda c, hb=hb: hb[:, c * 128:(c + 1) * 128], KC, hT, lambda c0, c1, bt=bt: hT[:, c0:c1, bt * 128:(bt + 1) * 128])

        def out_cb(tq, n0, n1, acc, blk=blk):
            S.op("act", lambda e: e.activation(out=obuf[:, tq, n0:n1], in_=acc[:, 0:n1 - n0], func=AF.Copy, scale=gts[:, tq:tq + 1]), [acc, gts], [obuf])
            if n1 == D:
                t0 = blk * BLK + tq * 128
                S.dma("sp", part[t0:t0 + 128, :], obuf[:, tq, :], [obuf], [part], obuf)
        sw.run(hT, w1, w3, w2, out_cb)
    S.emit()
    return nc


def build_fin(T, n_part):
    nc = bass.Bass("TRN2", target_bir_lowering=False)
    S = Sched(nc)
    xres = S.dram("xres", [T, D], F32, kind="ExternalInput")
    parts = S.dram("parts", [n_part, T, D], BF16, kind="ExternalInput")
    out = S.dram("out", [T, D], F32, kind="ExternalOutput")
    hbuf = Rot([S.sb("h_f", [128, D], F32) for _ in range(2)])
    pbuf = Rot([S.sb("p_b", [128, D], BF16) for _ in range(3)])
    for tt in range(T // 128):
        h = hbuf.next()
        S.dma("sp", h[:], xres[tt * 128:(tt + 1) * 128, :], [xres], [h], h)
        for p in range(n_part):
            pb = pbuf.next()
            S.dma("sp", pb[:], parts[p, tt * 128:(tt + 1) * 128, :], [parts], [pb], pb)
            S.op("dve", lambda e, h=h, pb=pb: e.tensor_tensor(out=h[:], in0=h[:], in1=pb[:], op=ALU.add), [h, pb], [h])
        S.dma("sp", out[tt * 128:(tt + 1) * 128, :], h[:], [h], [out], h)
    S.emit()
    return nc


RH, RC, RN = 16, 1024, 64
GN_EPS = 1e-5 * 64


def build_rwkv(T):
    nc = bass.Bass("TRN2", target_bir_lowering=False)
    k = K(nc, n_tp=1, n_acc=1)
    S = k.S
    NT = T // 128
    di = lambda n, s, dt=F32: S.dram(n, s, dt, kind="ExternalInput")
    h1 = di("h1", [T, D])
    g_n = di("g_n", [D])
    mu = di("mu", [6, D])
    w_r, w_k, w_v = di("w_r", [D, RC]), di("w_k", [D, RC]), di("w_v", [D, RC])
    w_o = di("w_o", [RC, D])
    vecs = {n: di(n, [RC]) for n in ["w0", "a0", "k_k", "k_a", "r_k", "ln_w", "ln_b"]}
    w1, w2 = di("w1", [D, 96]), di("w2", [96, RC])
    a1, a2 = di("a1", [D, 96]), di("a2", [96, RC])
    g1, g2 = di("g1", [D, 256]), di("g2", [256, RC])
    part = S.dram("part", [T, D], BF16, kind="ExternalOutput")
    HN = S.dram("HN", [T + 1, D], F32)
    P_r, P_k, P_v = S.dram("P_r", [T, RC], F32), S.dram("P_k", [T, RC], F32), S.dram("P_v", [T, RC], F32)
    P_w1, P_a1, P_g1 = S.dram("P_w1", [T, 96], F32), S.dram("P_a1", [T, 96], F32), S.dram("P_g1", [T, 256], F32)

    ident_f = S.sb("ident_f", [128, 128], F32)
    S.dma("sp", ident_f[:], k.ident_d[:], [k.ident_d], [ident_f], ident_f)

    S.phase_begin()
    gn_bc = k.gain_bc("gn", g_n, D)
    zrow = S.sb("zrow", [1, D], F32)
    S.op("pool", lambda e: e.memset(zrow[:], 0.0), [], [zrow])
    S.dma("sp", HN[0:1, :], zrow[:], [zrow], [HN], zrow)
    hb = Rot([S.sb("hb", [128, D], F32) for _ in range(2)])
    ho = Rot([S.sb("ho", [128, D], F32) for _ in range(2)])
    for tt in range(NT):
        h = hb.next()
        o = ho.next()
        S.dma("sp", h[:], h1[tt * 128:(tt + 1) * 128, :], [h1], [h], h)
        k.rmsnorm(h, h[:], gn_bc, gn_bc[:], o, o[:], D)
        S.dma("sp", HN[1 + tt * 128:1 + (tt + 1) * 128, :], o[:], [o], [HN], o)
    S.phase_end()

    TH = min(T, 1024)
    groups = []
    for (wd, j, Pd) in [(w_r, 0, P_r), (w_k, 2, P_k), (w_v, 3, P_v)]:
        for c0 in range(0, RC, 256):
            groups.append((wd, c0, 256, j, Pd, c0))
    groups += [(w1, 0, 96, 1, P_w1, 0), (a1, 0, 96, 4, P_a1, 0), (g1, 0, 256, 5, P_g1, 0)]
    for th in range(T // TH):
        S.phase_begin()
        XT = S.sb("XT", [128, 2 * KC, TH], BF16)
        muT = S.sb("muT", [128, 6, KC], F32)
        S.dma("sp", muT[:], mu.t.rearrange("j (kc p) -> p j kc", p=128), [mu], [muT], muT, allow_slow_non_contiguous=True)
        hnb = Rot([S.sb("hnb", [128, D], F32) for _ in range(2)])
        shb = Rot([S.sb("shb", [128, D], F32) for _ in range(2)])
        cb = Rot([S.sb("cb", [128, 2 * D], BF16) for _ in range(2)])
        for tl in range(TH // 128):
            t0 = th * TH + tl * 128
            hn, sh, c = hnb.next(), shb.next(), cb.next()
            S.dma("sp", hn[:], HN[1 + t0:1 + t0 + 128, :], [HN], [hn], hn)
            S.dma("sp", sh[:], HN[t0:t0 + 128, :], [HN], [sh], sh)
            S.op("act", lambda e, hn=hn, c=c: e.activation(out=c[:, 0:D], in_=hn[:], func=AF.Copy), [hn], [c])
            S.op("dve", lambda e, hn=hn, sh=sh, c=c: e.tensor_tensor(out=c[:, D:2 * D], in0=sh[:], in1=hn[:], op=ALU.subtract), [hn, sh], [c])
            k.transpose_to(c, lambda q, c=c: c[:, q * 128:(q + 1) * 128], 2 * KC, XT, lambda c0, c1, tl=tl: XT[:, c0:c1, tl * 128:(tl + 1) * 128])
        wgb = Rot([S.sb("wg", [128, KC, 256], BF16) for _ in range(2)])
        mwb = Rot([S.sb("mwg", [128, KC, 256], BF16) for _ in range(2)])
        stg = Rot([S.sb("stg", [128, 256], F32) for _ in range(3)])
        for (wd, c0, n, j, Pd, pc0) in groups:
            wg, mw = wgb.next(), mwb.next()
            S.dma("pool", wg[:, :, 0:n], wd.t.rearrange("(kc p) n -> p kc n", p=128)[:, :, c0:c0 + n], [wd], [wg], wg)
            S.op("dve", lambda e, wg=wg, mw=mw, j=j, n=n: e.tensor_tensor(out=mw[:, :, 0:n], in0=wg[:, :, 0:n], in1=muT[:, j, :].unsqueeze(2).to_broadcast([128, KC, n]), op=ALU.mult), [wg, muT], [mw])
            for tl in range(TH // 128):
                t0 = th * TH + tl * 128
                acc = k.acc.next()
                for kc in range(2 * KC):
                    src_w = wg if kc < KC else mw
                    S.op("pe", lambda e, acc=acc, kc=kc, src_w=src_w, tl=tl, n=n: e.matmul(acc[:, 0:n], lhsT=XT[:, kc, tl * 128:(tl + 1) * 128], rhs=src_w[:, kc % KC, 0:n], start=(kc == 0), stop=(kc == 2 * KC - 1)), [XT, src_w], [acc])
                st = stg.next()
                S.op("act", lambda e, acc=acc, st=st, n=n: e.activation(out=st[:, 0:n], in_=acc[:, 0:n], func=AF.Copy), [acc], [st])
                S.dma("sp", Pd[t0:t0 + 128, pc0:pc0 + n], st[:, 0:n], [st], [Pd], st)
        S.phase_end()

    S.phase_begin()
    vb = {}
    for n in vecs:
        vb[n] = k.gain_bc("v_" + n, vecs[n], RC)
    omka = S.sb("omka", [128, RC], F32)
    S.op("dve", lambda e: e.tensor_scalar(out=omka[:], in0=vb["k_a"][:], scalar1=-1.0, scalar2=1.0, op0=ALU.mult, op1=ALU.add), [vb["k_a"]], [omka])
    w2_s = S.sb("w2_s", [96, RC], BF16)
    a2_s = S.sb("a2_s", [96, RC], BF16)
    g2_s = S.sb("g2_s", [128, 2, RC], BF16)
    wo_s = S.sb("wo_s", [128, 8, D], BF16)
    S.dma("pool", w2_s[:], w2[:, :], [w2], [w2_s], w2_s)
    S.dma("pool", a2_s[:], a2[:, :], [a2], [a2_s], a2_s)
    S.dma("pool", g2_s[:], g2.t.rearrange("(c p) n -> p c n", p=128), [g2], [g2_s], g2_s)
    S.dma("pool", wo_s[:], w_o.t.rearrange("(c p) n -> p c n", p=128), [w_o], [wo_s], wo_s)
    St = S.sb("St", [RN, RC], F32)
    S.op("pool", lambda e: e.memset(St[:], 0.0), [], [St])
    bcp = Rot([S.ps("bcp", [128, 1024], F32) for _ in range(3)])
    pr, pk, pv = S.sb("pr", [128, RC], F32), S.sb("pk", [128, RC], F32), S.sb("pv", [128, RC], F32)
    pw1, pa1, pg1 = S.sb("pw1", [128, 96], F32), S.sb("pa1", [128, 96], F32), S.sb("pg1", [128, 256], F32)
    lb = S.sb("lb", [128, 256], BF16)
    lT = S.sb("lT", [128, 2, 128], BF16)
    wr, av, gv = S.sb("wr", [128, RC], F32), S.sb("av", [128, RC], F32), S.sb("gv", [128, RC], F32)
    tA, tW, tB, tK = S.sb("tA", [128, RC], F32), S.sb("tW", [128, RC], F32), S.sb("tB", [128, RC], F32), S.sb("tK", [128, RC], F32)
    tm1, tm2 = S.sb("tm1", [128, RC], F32), S.sb("tm2", [128, RC], F32)
    s16 = S.sb("s16", [128, 4, RH], F32)
    VT = S.sb("VT", [RN, RH, 128], F32)
    YT = S.sb("YT", [RN, RH, 128], F32)
    T1, T2 = S.sb("T1", [RN, RC], F32), S.sb("T2", [RN, RC], F32)
    sa = S.sb("sa", [RN, RH], F32)
    yb = S.sb("yb", [128, RC], BF16)
    ybT = S.sb("ybT", [128, 8, 128], BF16)
    ob = S.sb("ob", [128, D], BF16)
    v3 = lambda ap: ap.rearrange("p (h j) -> p h j", h=RH)
    bc3 = lambda ap, p=128: ap.unsqueeze(2).to_broadcast([p, RH, RN])

    def lora2(src, n, func, w_s, kparts, dst, bias):
        nch = (n + 127) // 128
        S.op("act", lambda e: e.activation(out=lb[:, 0:n], in_=src[:, 0:n], func=func), [src], [lb])
        k.transpose_to(lb, lambda c: lb[:, c * 128:(c + 1) * 128], nch, lT, lambda c0, c1: lT[:, c0:c1, :])
        for half in range(2):
            acc = k.acc.next()
            for c in range(nch):
                rows = min(n, 128)
                rhs = w_s[0:rows, half * 512:(half + 1) * 512] if kparts == 1 else w_s[:, c, half * 512:(half + 1) * 512]
                S.op("pe", lambda e, acc=acc, c=c, rhs=rhs, rows=rows: e.matmul(acc[:], lhsT=lT[0:rows, c, :], rhs=rhs, start=(c == 0), stop=(c == nch - 1)), [lT, w_s], [acc])
            if bias is not None:
                S.op("dve", lambda e, acc=acc, half=half: e.tensor_tensor(out=dst[:, half * 512:(half + 1) * 512], in0=acc[:], in1=bias[:, half * 512:(half + 1) * 512], op=ALU.add), [acc, bias], [dst])
            else:
                S.op("dve", lambda e, acc=acc, half=half: e.tensor_copy(out=dst[:, half * 512:(half + 1) * 512], in_=acc[:]), [acc], [dst])

    for tt in range(NT):
        t0 = tt * 128
        for (b_, P_, n) in [(pr, P_r, RC), (pk, P_k, RC), (pv, P_v, RC), (pw1, P_w1, 96), (pa1, P_a1, 96), (pg1, P_g1, 256)]:
            S.dma("sp", b_[:, 0:n], P_[t0:t0 + 128, :], [P_], [b_], b_)
        lora2(pw1, 96, AF.Tanh, w2_s, 1, wr, vb["w0"])
        lora2(pa1, 96, AF.Copy, a2_s, 1, av, vb["a0"])
        lora2(pg1, 256, AF.Sigmoid, g2_s, 2, gv, None)
        S.op("act", lambda e: e.activation(out=av[:], in_=av[:], func=AF.Sigmoid), [av], [av])
        S.op("act", lambda e: e.activation(out=tm1[:], in_=wr[:], func=AF.Exp, scale=-1.0), [wr], [tm1])
        S.op("act", lambda e: e.activation(out=tm1[:], in_=tm1[:], func=AF.Ln, bias=1.0), [tm1], [tm1])
        S.op("act", lambda e: e.activation(out=tm1[:], in_=tm1[:], func=AF.Exp, scale=-1.0, bias=-0.5), [tm1], [tm1])
        S.op("act", lambda e: e.activation(out=tW[:], in_=tm1[:], func=AF.Exp, scale=-1.0), [tm1], [tW])
        S.op("dve", lambda e: e.tensor_tensor(out=tm2[:], in0=pk[:], in1=vb["k_k"][:], op=ALU.mult), [pk, vb["k_k"]], [tm2])
        S.op("dve", lambda e: e.tensor_tensor(out=tm1[:], in0=tm2[:], in1=tm2[:], op=ALU.mult), [tm2], [tm1])
        S.op("dve", lambda e: e.tensor_reduce(out=s16[:, 0, :], in_=v3(tm1[:]), axis=AX.X, op=ALU.add), [tm1], [s16])
        S.op("act", lambda e: e.activation(out=s16[:, 1, :], in_=s16[:, 0, :], func=AF.Sqrt), [s16], [s16])
        S.op("dve", lambda e: e.tensor_scalar(out=s16[:, 1, :], in0=s16[:, 1, :], scalar1=1e-12, scalar2=None, op0=ALU.max), [s16], [s16])
        S.op("dve", lambda e: e.reciprocal(out=s16[:, 2, :], in_=s16[:, 1, :]), [s16], [s16])
        S.op("dve", lambda e: e.tensor_tensor(out=v3(tm2[:]), in0=v3(tm2[:]), in1=bc3(s16[:, 2, :]), op=ALU.mult), [tm2, s16], [tm2])
        S.op("dve", lambda e: e.tensor_scalar(out=tA[:], in0=tm2[:], scalar1=-1.0, scalar2=None, op0=ALU.mult), [tm2], [tA])
        S.op("dve", lambda e: e.tensor_tensor(out=tB[:], in0=tm2[:], in1=av[:], op=ALU.mult), [tm2, av], [tB])
        S.op("dve", lambda e: e.tensor_tensor(out=tm1[:], in0=av[:], in1=vb["k_a"][:], op=ALU.mult), [av, vb["k_a"]], [tm1])
        S.op("dve", lambda e: e.tensor_tensor(out=tm1[:], in0=tm1[:], in1=omka[:], op=ALU.add), [tm1, omka], [tm1])
        S.op("dve", lambda e: e.tensor_tensor(out=tK[:], in0=pk[:], in1=tm1[:], op=ALU.mult), [pk, tm1], [tK])
        S.op("dve", lambda e: e.tensor_tensor(out=tm1[:], in0=pr[:], in1=tK[:], op=ALU.mult), [pr, tK], [tm1])
        S.op("dve", lambda e: e.tensor_tensor(out=tm1[:], in0=tm1[:], in1=vb["r_k"][:], op=ALU.mult), [tm1, vb["r_k"]], [tm1])
        S.op("dve", lambda e: e.tensor_reduce(out=s16[:, 3, :], in_=v3(tm1[:]), axis=AX.X, op=ALU.add), [tm1], [s16])
        for h0 in range(0, RH, 8):
            bp = bcp.next()
            for h in range(h0, h0 + 8):
                S.op("pe", lambda e, bp=bp, h=h, h0=h0: e.transpose(out=bp[0:RN, (h - h0) * 128:(h - h0 + 1) * 128], in_=pv[:, h * RN:(h + 1) * RN], identity=ident_f[:]), [pv, ident_f], [bp])
            S.op("act", lambda e, bp=bp, h0=h0: e.activation(out=VT[:, h0:h0 + 8, :].rearrange("p a b -> p (a b)"), in_=bp[0:RN, :], func=AF.Copy), [bp], [VT])
        for t in range(128):
            sel = ident_f[:, t:t + 1].to_broadcast([128, RN])
            bq = {}
            for nm, src in (("A", tA), ("W", tW), ("B", tB), ("K", tK), ("R", pr)):
                bp = bcp.next()
                for half in range(2):
                    S.op("pe", lambda e, bp=bp, src=src, half=half, sel=sel: e.matmul(bp[0:RN, half * 512:(half + 1) * 512], lhsT=sel, rhs=src[:, half * 512:(half + 1) * 512], start=True, stop=True), [ident_f, src], [bp])
                bq[nm] = bp
                if nm == "A":
                    S.op("dve", lambda e, bp=bp: e.tensor_tensor(out=T1[:], in0=St[:], in1=bp[0:RN, :], op=ALU.mult), [St, bp], [T1])
                    S.op("dve", lambda e: e.tensor_reduce(out=sa[:], in_=v3(T1[:]), axis=AX.X, op=ALU.add), [T1], [sa])
                elif nm == "W":
                    S.op("dve", lambda e, bp=bp: e.tensor_tensor(out=St[:], in0=St[:], in1=bp[0:RN, :], op=ALU.mult), [St, bp], [St])
                elif nm == "B":
                    S.op("dve", lambda e, bp=bp: e.tensor_tensor(out=v3(T2[:]), in0=v3(bp[0:RN, :]), in1=bc3(sa[:], RN), op=ALU.mult), [bp, sa], [T2])
                    S.op("dve", lambda e: e.tensor_tensor(out=St[:], in0=St[:], in1=T2[:], op=ALU.add), [St, T2], [St])
                elif nm == "K":
                    S.op("dve", lambda e, bp=bp, t=t: e.tensor_tensor(out=v3(T2[:]), in0=v3(bp[0:RN, :]), in1=bc3(VT[:, :, t], RN), op=ALU.mult), [bp, VT], [T2])
                    S.op("dve", lambda e: e.tensor_tensor(out=St[:], in0=St[:], in1=T2[:], op=ALU.add), [St, T2], [St])
                else:
                    S.op("dve", lambda e, bp=bp: e.tensor_tensor(out=T1[:], in0=St[:], in1=bp[0:RN, :], op=ALU.mult), [St, bp], [T1])
                    S.op("dve", lambda e, t=t: e.tensor_reduce(out=YT[:, :, t], in_=v3(T1[:]), axis=AX.X, op=ALU.add), [T1], [YT])
        for h0 in range(0, RH, 8):
            bp = bcp.next()
            for h in range(h0, h0 + 8):
                S.op("pe", lambda e, bp=bp, h=h, h0=h0: e.transpose(out=bp[:, (h - h0) * RN:(h - h0 + 1) * RN], in_=YT[:, h, :], identity=ident_f[0:RN, 0:RN]), [YT, ident_f], [bp])
            S.op("act", lambda e, bp=bp, h0=h0: e.activation(out=tm1[:, h0 * RN:(h0 + 8) * RN], in_=bp[:, 0:8 * RN], func=AF.Copy), [bp], [tm1])
        y = tm1
        S.op("dve", lambda e: e.tensor_reduce(out=s16[:, 0, :], in_=v3(y[:]), axis=AX.X, op=ALU.add), [y], [s16])
        S.op("dve", lambda e: e.tensor_scalar(out=s16[:, 0, :], in0=s16[:, 0, :], scalar1=-1.0 / RN, scalar2=None, op0=ALU.mult), [s16], [s16])
        S.op("dve", lambda e: e.tensor_tensor(out=v3(y[:]), in0=v3(y[:]), in1=bc3(s16[:, 0, :]), op=ALU.add), [y, s16], [y])
        S.op("dve", lambda e: e.tensor_tensor(out=tm2[:], in0=y[:], in1=y[:], op=ALU.mult), [y], [tm2])
        S.op("dve", lambda e: e.tensor_reduce(out=s16[:, 1, :], in_=v3(tm2[:]), axis=AX.X, op=ALU.add), [tm2], [s16])
        S.op("dve", lambda e: e.tensor_scalar(out=s16[:, 1, :], in0=s16[:, 1, :], scalar1=1.0 / RN, scalar2=GN_EPS, op0=ALU.mult, op1=ALU.add), [s16], [s16])
        S.op("act", lambda e: e.activation(out=s16[:, 1, :], in_=s16[:, 1, :], func=AF.Sqrt), [s16], [s16])
        S.op("dve", lambda e: e.reciprocal(out=s16[:, 2, :], in_=s16[:, 1, :]), [s16], [s16])
        S.op("dve", lambda e: e.tensor_tensor(out=v3(y[:]), in0=v3(y[:]), in1=bc3(s16[:, 2, :]), op=ALU.mult), [y, s16], [y])
        S.op("dve", lambda e: e.tensor_tensor(out=y[:], in0=y[:], in1=vb["ln_w"][:], op=ALU.mult), [y, vb["ln_w"]], [y])
        S.op("dve", lambda e: e.tensor_tensor(out=y[:], in0=y[:], in1=vb["ln_b"][:], op=ALU.add), [y, vb["ln_b"]], [y])
        S.op("dve", lambda e: e.tensor_tensor(out=v3(tm2[:]), in0=v3(pv[:]), in1=bc3(s16[:, 3, :]), op=ALU.mult), [pv, s16], [tm2])
        S.op("dve", lambda e: e.tensor_tensor(out=y[:], in0=y[:], in1=tm2[:], op=ALU.add), [y, tm2], [y])
        S.op("dve", lambda e: e.tensor_tensor(out=yb[:], in0=y[:], in1=gv[:], op=ALU.mult), [y, gv], [yb])
        k.transpose_to(yb, lambda c: yb[:, c * 128:(c + 1) * 128], 8, ybT, lambda c0, c1: ybT[:, c0:c1, :])
        for nn in range(4):
            acc = k.acc.next()
            for c in range(8):
                S.op("pe", lambda e, acc=acc, c=c, nn=nn: e.matmul(acc[:], lhsT=ybT[:, c, :], rhs=wo_s[:, c, nn * 512:(nn + 1) * 512], start=(c == 0), stop=(c == 7)), [ybT, wo_s], [acc])
            S.op("act", lambda e, acc=acc, nn=nn: e.activation(out=ob[:, nn * 512:(nn + 1) * 512], in_=acc[:], func=AF.Copy), [acc], [ob])
        S.dma("sp", part[t0:t0 + 128, :], ob[:], [ob], [part], ob)
    S.phase_end()
    S.emit()
    return nc


def proj_phase(k, T, xd, gd, Wd, ncols, Pd, row_off, TH=1024):
    S = k.S
    TH = min(T, TH)
    groups = [(c0, min(256, ncols - c0)) for c0 in range(0, ncols, 256)]
    for th in range(T // TH):
        S.phase_begin()
        g_bc = k.gain_bc("pg", gd, D)
        XT = S.sb("XT", [128, KC, TH], BF16)
        hb = Rot([S.sb("hb", [128, D], F32) for _ in range(2)])
        hn = Rot([S.sb("hn", [128, D], BF16) for _ in range(2)])
        for tl in range(TH // 128):
            t0 = th * TH + tl * 128
            h, o = hb.next(), hn.next()
            S.dma("sp", h[:], xd[t0:t0 + 128, :], [xd], [h], h)
            k.rmsnorm(h, h[:], g_bc, g_bc[:], o, o[:], D)
            k.transpose_to(o, lambda q, o=o: o[:, q * 128:(q + 1) * 128], KC, XT, lambda c0, c1, tl=tl: XT[:, c0:c1, tl * 128:(tl + 1) * 128])
        wgb = Rot([S.sb("wg", [128, KC, 256], BF16) for _ in range(2)])
        stg = Rot([S.sb("stg", [128, 256], F32) for _ in range(3)])
        Wv = Wd.t.rearrange("(kc p) n -> p kc n", p=128)
        for (c0, n) in groups:
            wg = wgb.next()
            S.dma("pool", wg[:, :, 0:n], Wv[:, :, c0:c0 + n], [Wd], [wg], wg)
            for tl in range(TH // 128):
                t0 = th * TH + tl * 128
                acc = k.acc.next()
                for kc in range(KC):
                    S.op("pe", lambda e, acc=acc, kc=kc, wg=wg, tl=tl, n=n: e.matmul(acc[:, 0:n], lhsT=XT[:, kc, tl * 128:(tl + 1) * 128], rhs=wg[:, kc, 0:n], start=(kc == 0), stop=(kc == KC - 1)), [XT, wg], [acc])
                st = stg.next()
                S.op("act", lambda e, acc=acc, st=st, n=n: e.activation(out=st[:, 0:n], in_=acc[:, 0:n], func=AF.Copy), [acc], [st])
                S.dma("sp", Pd[row_off + t0:row_off + t0 + 128, c0:c0 + n], st[:, 0:n], [st], [Pd], st)
        S.phase_end()


SH, SP_, SN, SG = 16, 64, 128, 2
SC = SH * SP_
NCOL_SSM = 2 * SC + 2 * SG * SN + SH


def build_ssm(T):
    nc = bass.Bass("TRN2", target_bir_lowering=False)
    k = K(nc, n_tp=1, n_acc=2)
    S = k.S
    NT = T // 128
    di = lambda n, s, dt=F32: S.dram(n, s, dt, kind="ExternalInput")
    xb = di("xb", [T, D])
    g_n = di("g_n", [D])
    w_in = di("w_in", [D, NCOL_SSM])
    cw = di("cw", [4, 1536])
    cb = di("cb", [1536])
    dtb, alog, dsk = di("dtb", [SH]), di("alog", [SH]), di("dsk", [SH])
    nw = di("nw", [SC])
    w_o = di("w_o", [SC, D])
    tri_d, neg_d, ones_d = di("c_tri", [128, 128]), di("c_neg", [128, 128]), di("c_ones", [128, 128])
    part = S.dram("part", [T, D], BF16, kind="ExternalOutput")
    P = S.dram("P", [T + 3, NCOL_SSM], F32)

    zrow = S.sb("zrow", [3, NCOL_SSM], F32)
    S.op("pool", lambda e: e.memset(zrow[:], 0.0), [], [zrow])
    S.dma("sp", P[0:3, :], zrow[:], [zrow], [P], zrow)
    proj_phase(k, T, xb, g_n, w_in, NCOL_SSM, P, 3)

    S.phase_begin()
    ident_f = S.sb("ident_f", [128, 128], F32)
    tri, neg, ones = S.sb("tri", [128, 128], F32), S.sb("neg", [128, 128], F32), S.sb("ones", [128, 128], F32)
    for b_, d_ in ((ident_f, k.ident_d), (tri, tri_d), (neg, neg_d), (ones, ones_d)):
        S.dma("sp", b_[:], d_[:], [d_], [b_], b_)
    cwb = [k.gain_bc(f"cw{i}", cw.t[i], 1536) for i in range(4)]
    cbb = k.gain_bc("cbb", cb, 1536)
    nwb = k.gain_bc("nwb", nw, SC)
    dtbb, ab, dskb = k.gain_bc("dtbb", dtb, SH), k.gain_bc("ab", alog, SH), k.gain_bc("dskb", dsk, SH)
    S.op("act", lambda e: e.activation(out=ab[:], in_=ab[:], func=AF.Exp), [ab], [ab])
    S.op("dve", lambda e: e.tensor_scalar(out=ab[:], in0=ab[:], scalar1=-1.0, scalar2=None, op0=ALU.mult), [ab], [ab])
    wo_s = S.sb("wo_s", [128, 8, D], BF16)
    S.dma("pool", wo_s[:], w_o.t.rearrange("(c p) n -> p c n", p=128), [w_o], [wo_s], wo_s)
    dps = Rot([S.ps("dps", [128, 512], F32) for _ in range(2)])
    ydp, yop, stp = S.ps("ydp", [128, 512], F32), S.ps("yop", [128, 512], F32), S.ps("stp", [128, 512], F32)
    z = S.sb("z", [128, SC], F32)
    xk = [S.sb(f"xk{i}", [128, 1536], F32) for i in range(4)]
    tmpa, tmpb = S.sb("tmpa", [128, 1536], F32), S.sb("tmpb", [128, 1536], F32)
    cv = S.sb("cv", [128, 1536], F32)
    xa = S.sb("xa", [128, 1536], F32)
    s16 = S.sb("s16", [128, 12, SH], F32)
    cs = S.sb("cs", [128, 2 * SH], F32)
    xdt_b, xst_b = S.sb("xdt_b", [128, SC], BF16), S.sb("xst_b", [128, SC], BF16)
    bcb = S.sb("bcb", [128, 512], BF16)
    BCT = S.sb("BCT", [128, 4, 128], BF16)
    CBT = S.sb("CBT", [128, SG, 128], F32)
    DT = S.sb("DT", [128, SH, 128], F32)
    MT = S.sb("MT", [128, SH, 128], BF16)
    ysb, y = S.sb("ysb", [128, SC], F32), S.sb("y", [128, SC], F32)
    hs = S.sb("hs", [128, SG, 512], F32)
    hbf = S.sb("hbf", [128, SG, 512], BF16)
    S.op("pool", lambda e: e.memset(hs[:], 0.0), [], [hs])
    S.op("pool", lambda e: e.memset(hbf[:], 0.0), [], [hbf])
    yb = S.sb("yb", [128, SC], BF16)
    ybT = S.sb("ybT", [128, 8, 128], BF16)
    ob = S.sb("ob", [128, D], BF16)
    v3 = lambda ap: ap.rearrange("p (h j) -> p h j", j=SP_)
    bc3 = lambda ap, nh=SH: ap.unsqueeze(2).to_broadcast([128, nh, SP_])
    DTI, ADT, C_, NEGC, ECL, TOT, CD, DTE, DD = range(9)

    for tt in range(NT):
        t0 = tt * 128
        S.dma("sp", z[:], P[3 + t0:3 + t0 + 128, 0:SC], [P], [z], z)
        for i in range(4):
            S.dma("sp", xk[i][:], P[t0 + i:t0 + i + 128, SC:SC + 1536], [P], [xk[i]], xk[i])
        S.dma("sp", s16[:, DTI, :], P[3 + t0:3 + t0 + 128, SC + 1536:SC + 1536 + SH], [P], [s16], s16)
        S.op("pool", lambda e: e.tensor_tensor(out=cv[:], in0=xk[0][:], in1=cwb[0][:], op=ALU.mult), [xk[0], cwb[0]], [cv])
        S.op("pool", lambda e: e.tensor_tensor(out=tmpa[:], in0=xk[1][:], in1=cwb[1][:], op=ALU.mult), [xk[1], cwb[1]], [tmpa])
        S.op("dve", lambda e: e.tensor_tensor(out=cv[:], in0=cv[:], in1=tmpa[:], op=ALU.add), [cv, tmpa], [cv])
        S.op("pool", lambda e: e.tensor_tensor(out=tmpb[:], in0=xk[2][:], in1=cwb[2][:], op=ALU.mult), [xk[2], cwb[2]], [tmpb])
        S.op("dve", lambda e: e.tensor_tensor(out=cv[:], in0=cv[:], in1=tmpb[:], op=ALU.add), [cv, tmpb], [cv])
        S.op("pool", lambda e: e.tensor_tensor(out=tmpa[:], in0=xk[3][:], in1=cwb[3][:], op=ALU.mult), [xk[3], cwb[3]], [tmpa])
        S.op("dve", lambda e: e.tensor_tensor(out=cv[:], in0=cv[:], in1=tmpa[:], op=ALU.add), [cv, tmpa], [cv])
        S.op("dve", lambda e: e.tensor_tensor(out=cv[:], in0=cv[:], in1=cbb[:], op=ALU.add), [cv, cbb], [cv])
        S.op("act", lambda e: e.activation(out=xa[:], in_=cv[:], func=AF.Silu), [cv], [xa])
        S.op("dve", lambda e: e.tensor_tensor(out=s16[:, DTI, :], in0=s16[:, DTI, :], in1=dtbb[:], op=ALU.add), [s16, dtbb], [s16])
        S.op("act", lambda e: e.activation(out=s16[:, DTI, :], in_=s16[:, DTI, :], func=AF.Exp), [s16], [s16])
        S.op("act", lambda e: e.activation(out=s16[:, DTI, :], in_=s16[:, DTI, :], func=AF.Ln, bias=1.0), [s16], [s16])
        S.op("dve", lambda e: e.tensor_tensor(out=s16[:, ADT, :], in0=s16[:, DTI, :], in1=ab[:], op=ALU.mult), [s16, ab], [s16])
        acc = k.acc.next()
        S.op("pe", lambda e, acc=acc: e.matmul(acc[:, 0:SH], lhsT=tri[:], rhs=s16[:, ADT, :], start=True, stop=True), [tri, s16], [acc])
        S.op("pe", lambda e, acc=acc: e.matmul(acc[:, SH:2 * SH], lhsT=ones[:], rhs=s16[:, ADT, :], start=True, stop=True), [ones, s16], [acc])
        S.op("dve", lambda e, acc=acc: e.tensor_copy(out=cs[:], in_=acc[:, 0:2 * SH]), [acc], [cs])
        S.op("dve", lambda e: e.tensor_scalar(out=s16[:, NEGC, :], in0=cs[:, 0:SH], scalar1=-1.0, scalar2=None, op0=ALU.mult), [cs], [s16])
        S.op("act", lambda e: e.activation(out=s16[:, ECL, :], in_=cs[:, 0:SH], func=AF.Exp), [cs], [s16])
        S.op("act", lambda e: e.activation(out=s16[:, CD, :], in_=cs[:, SH:2 * SH], func=AF.Exp), [cs], [s16])
        S.op("dve", lambda e: e.tensor_tensor(out=s16[:, TOT, :], in0=cs[:, SH:2 * SH], in1=cs[:, 0:SH], op=ALU.subtract), [cs], [s16])
        S.op("act", lambda e: e.activation(out=s16[:, DTE, :], in_=s16[:, TOT, :], func=AF.Exp), [s16], [s16])
        S.op("dve", lambda e: e.tensor_tensor(out=s16[:, DD, :], in0=s16[:, DTE, :], in1=s16[:, DTI, :], op=ALU.mult), [s16], [s16])
        S.op("dve", lambda e: e.tensor_tensor(out=v3(xdt_b[:]), in0=v3(xa[:, 0:SC]), in1=bc3(s16[:, DTI, :]), op=ALU.mult), [xa, s16], [xdt_b])
        S.op("dve", lambda e: e.tensor_tensor(out=v3(xst_b[:]), in0=v3(xa[:, 0:SC]), in1=bc3(s16[:, DD, :]), op=ALU.mult), [xa, s16], [xst_b])
        S.op("act", lambda e: e.activation(out=bcb[:], in_=xa[:, SC:SC + 512], func=AF.Copy), [xa], [bcb])
        k.transpose_to(bcb, lambda c: bcb[:, c * 128:(c + 1) * 128], 4, BCT, lambda c0, c1: BCT[:, c0:c1, :])
        acc = k.acc.next()
        for g in range(SG):
            S.op("pe", lambda e, acc=acc, g=g: e.matmul(acc[:, g * 128:(g + 1) * 128], lhsT=BCT[:, g, :], rhs=BCT[:, 2 + g, :], start=True, stop=True), [BCT], [acc])
        S.op("act", lambda e, acc=acc: e.activation(out=CBT[:].rearrange("p a b -> p (a b)"), in_=acc[:, 0:256], func=AF.Copy), [acc], [CBT])
        for hq in range(4):
            dp = dps.next()
            for j in range(4):
                h = hq * 4 + j
                S.op("pe", lambda e, dp=dp, j=j, h=h: e.matmul(dp[:, j * 128:(j + 1) * 128], lhsT=cs[:, h:h + 1].to_broadcast([128, 128]), rhs=ident_f[:], start=True, stop=False), [cs, ident_f], [dp])
                S.op("pe", lambda e, dp=dp, j=j: e.matmul(dp[:, j * 128:(j + 1) * 128], lhsT=ident_f[:], rhs=neg[:], start=False, stop=True), [ident_f, neg], [dp])
            for j in range(4):
                h = hq * 4 + j
                S.op("act", lambda e, dp=dp, j=j, h=h: e.activation(out=DT[:, h, :], in_=dp[:, j * 128:(j + 1) * 128], func=AF.Exp, bias=s16[:, NEGC, h:h + 1]), [dp, s16], [DT])
        for g in range(SG):
            S.op("dve", lambda e, g=g: e.tensor_tensor(out=MT[:, g * 8:(g + 1) * 8, :], in0=DT[:, g * 8:(g + 1) * 8, :], in1=CBT[:, g, :].unsqueeze(1).to_broadcast([128, 8, 128]), op=ALU.mult), [DT, CBT], [MT])
        for g in range(SG):
            for j in range(8):
                h = g * 8 + j
                S.op("pe", lambda e, h=h, j=j: e.matmul(ydp[:, j * 64:(j + 1) * 64], lhsT=MT[:, h, :], rhs=xdt_b[:, h * 64:(h + 1) * 64], start=True, stop=True), [MT, xdt_b], [ydp])
            S.op("pe", lambda e, g=g: e.matmul(yop[:], lhsT=BCT[:, 2 + g, :], rhs=hbf[:, g, :], start=True, stop=True), [BCT, hbf], [yop])
            S.op("act", lambda e, g=g: e.activation(out=ysb[:, g * 512:(g + 1) * 512], in_=ydp[:], func=AF.Copy), [ydp], [ysb])
            S.op("dve", lambda e, g=g: e.tensor_tensor(out=v3(y[:, g * 512:(g + 1) * 512]), in0=v3(yop[:]), in1=bc3(s16[:, ECL, g * 8:(g + 1) * 8], 8), op=ALU.mult), [yop, s16], [y])
            S.op("pe", lambda e, g=g: e.matmul(stp[:], lhsT=bcb[:, g * 128:(g + 1) * 128], rhs=xst_b[:, g * 512:(g + 1) * 512], start=True, stop=True), [bcb, xst_b], [stp])
            S.op("dve", lambda e, g=g: e.tensor_tensor(out=v3(hs[:, g, :]), in0=v3(hs[:, g, :]), in1=bc3(s16[:, CD, g * 8:(g + 1) * 8], 8), op=ALU.mult), [hs, s16], [hs])
            S.op("dve", lambda e, g=g: e.tensor_tensor(out=hs[:, g, :], in0=hs[:, g, :], in1=stp[:], op=ALU.add), [hs, stp], [hs])
            S.op("act", lambda e, g=g: e.activation(out=hbf[:, g, :], in_=hs[:, g, :], func=AF.Copy), [hs], [hbf])
        S.op("dve", lambda e: e.tensor_tensor(out=y[:], in0=y[:], in1=ysb[:], op=ALU.add), [y, ysb], [y])
        S.op("dve", lambda e: e.tensor_tensor(out=v3(ysb[:]), in0=v3(xa[:, 0:SC]), in1=bc3(dskb[:]), op=ALU.mult), [xa, dskb], [ysb])
        S.op("dve", lambda e: e.tensor_tensor(out=y[:], in0=y[:], in1=ysb[:], op=ALU.add), [y, ysb], [y])
        S.op("act", lambda e: e.activation(out=ysb[:], in_=z[:], func=AF.Silu), [z], [ysb])
        S.op("dve", lambda e: e.tensor_tensor(out=y[:], in0=y[:], in1=ysb[:], op=ALU.mult), [y, ysb], [y])
        for g in range(SG):
            k.rmsnorm(y, y[:, g * 512:(g + 1) * 512], nwb, nwb[:, g * 512:(g + 1) * 512], yb, yb[:, g * 512:(g + 1) * 512], 512)
        k.transpose_to(yb, lambda c: yb[:, c * 128:(c + 1) * 128], 8, ybT, lambda c0, c1: ybT[:, c0:c1, :])
        for nn in range(4):
            acc = k.acc.next()
            for c in range(8):
                S.op("pe", lambda e, acc=acc, c=c, nn=nn: e.matmul(acc[:], lhsT=ybT[:, c, :], rhs=wo_s[:, c, nn * 512:(nn + 1) * 512], start=(c == 0), stop=(c == 7)), [ybT, wo_s], [acc])
            S.op("act", lambda e, acc=acc, nn=nn: e.activation(out=ob[:, nn * 512:(nn + 1) * 512], in_=acc[:], func=AF.Copy), [acc], [ob])
        S.dma("sp", part[t0:t0 + 128, :], ob[:], [ob], [part], ob)
    S.phase_end()
    S.emit()
    return nc


def _ssm_consts():
    s = np.arange(128)[:, None]
    l = np.arange(128)[None, :]
    return dict(c_tri=(s <= l).astype(np.float32), c_neg=np.where(s > l, -30000.0, 0.0).astype(np.float32), c_ones=np.ones((128, 128), np.float32))


def _ssm_cols(hh):
    z = np.arange(hh * 1024, hh * 1024 + 1024)
    xc = 2048 + np.arange(hh * 1024, hh * 1024 + 1024)
    Bc = 2048 + 2048 + np.arange(2 * hh * 128, 2 * hh * 128 + 256)
    Cc = 2048 + 2048 + 512 + np.arange(2 * hh * 128, 2 * hh * 128 + 256)
    dtc = 2048 + 3072 + np.arange(hh * 16, hh * 16 + 16)
    conv_rows = np.concatenate([xc, Bc, Cc]) - 2048
    return np.concatenate([z, xc, Bc, Cc, dtc]), conv_rows


def _ssm_inputs(inp, xb, hh):
    f32 = lambda a: np.ascontiguousarray(np.asarray(a), dtype=np.float32)
    cols, crow = _ssm_cols(hh)
    m = dict(c_ident=_ident(), xb=xb, g_n=f32(inp["norm_mix"][0]), w_in=f32(np.asarray(inp["ev_w_in"][0])[:, cols]),
             cw=f32(np.asarray(inp["ev_conv_w"][0])[crow].T), cb=f32(np.asarray(inp["ev_conv_b"][0])[crow]),
             dtb=f32(np.asarray(inp["ev_dt_bias"][0])[hh * 16:(hh + 1) * 16]), alog=f32(np.asarray(inp["ev_a_log"][0])[hh * 16:(hh + 1) * 16]),
             dsk=f32(np.asarray(inp["ev_d_skip"][0])[hh * 16:(hh + 1) * 16]), nw=f32(np.asarray(inp["ev_ssm_norm"][0])[hh * 1024:(hh + 1) * 1024]),
             w_o=f32(np.asarray(inp["ev_w_out"][0])[hh * 1024:(hh + 1) * 1024]))
    m.update(_ssm_consts())
    return m


NG, NR, HD = 2, 4, 128
NQH = NG * NR
NCOL_NSA = NQH * HD + 6 * NG * HD + NQH * 3
OQ, OKC, OVC, OKS, OVS, OKW, OVW, OGL = 0, 1024, 1280, 1536, 1792, 2048, 2304, 2560
VW_ = 132
CW_ = 196


def build_nsa(T, debug=False):
    nc = bass.Bass("TRN2", target_bir_lowering=False)
    k = K(nc, n_tp=1, n_acc=1)
    k.debug = debug
    S = k.S
    NT = T // 128
    NCMP = (T - 32) // 16 + 1
    NIT = (NCMP + 127) // 128
    di = lambda n, s, dt=F32: S.dram(n, s, dt, kind="ExternalInput")
    xb = di("xb", [T, D])
    g_n = di("g_n", [D])
    w_in = di("w_in", [D, NCOL_NSA])
    pos = di("pos", [T, 1], I32)
    invf = di("c_invf", [16])
    qg, kcg, ksg, kwg = di("qg", [HD]), di("kcg", [HD]), di("ksg", [HD]), di("kwg", [HD])
    pe_k, pe_v = di("pe_k", [32, HD]), di("pe_v", [32, HD])
    wk1, wv1 = di("wk1", [32 * HD, 256]), di("wv1", [32 * HD, 256])
    wk2, wv2 = di("wk2", [256, HD]), di("wv2", [256, HD])
    w_o = di("w_o", [NQH * HD, D])
    ov_d = di("c_ov", [NIT * 128, 64])
    efull_d = di("c_efull", [64, T])
    cmask_d = di("c_cmask", [16, 128, 128])
    diag_d = di("c_diag", [128, 128])
    fadd_d = di("c_fadd", [NT, 128, 64])
    part = S.dram("part", [T, D], BF16, kind="ExternalOutput")
    P = S.dram("P", [T, NCOL_NSA], F32)

    KsT = S.sb("KsT", [128, NG, T], BF16)
    KwT = S.sb("KwT", [128, NG, T], BF16)
    Vs = S.sb("Vs", [128, NT, NG, VW_], BF16)
    Vw = S.sb("Vw", [128, NT, NG, VW_], BF16)
    KcmpT = S.sb("KcmpT", [128, NG, NIT * 128], BF16)
    Vcmp = S.sb("Vcmp", [128, NG, NIT, CW_], BF16)
    cosb = S.sb("cosb", [128, NT, 16], F32)
    sinb = S.sb("sinb", [128, NT, 16], F32)
    S.op("pool", lambda e: e.memset(Vs[:], 1.0), [], [Vs])
    S.op("pool", lambda e: e.memset(Vw[:], 1.0), [], [Vw])
    S.op("pool", lambda e: e.memset(Vcmp[:], 0.0), [], [Vcmp])
    S.op("pool", lambda e: e.memset(KcmpT[:], 0.0), [], [KcmpT])

    proj_phase(k, T, xb, g_n, w_in, NCOL_NSA, P, 0)

    def rope(buf, ap3, nh, tt, t1, t2, t3, t4):
        cb_ = cosb[:, tt, :].unsqueeze(1).to_broadcast([128, nh, 16])
        sb_ = sinb[:, tt, :].unsqueeze(1).to_broadcast([128, nh, 16])
        x1, x2 = ap3[:, :, 0:16], ap3[:, :, 16:32]
        v = lambda b: b[:, 0:nh * 16].rearrange("p (h i) -> p h i", i=16)
        S.op("dve", lambda e: e.tensor_tensor(out=v(t1), in0=x1, in1=cb_, op=ALU.mult), [buf, cosb], [t1])
        S.op("dve", lambda e: e.tensor_tensor(out=v(t2), in0=x2, in1=sb_, op=ALU.mult), [buf, sinb], [t2])
        S.op("dve", lambda e: e.tensor_tensor(out=v(t3), in0=x2, in1=cb_, op=ALU.mult), [buf, cosb], [t3])
        S.op("dve", lambda e: e.tensor_tensor(out=v(t4), in0=x1, in1=sb_, op=ALU.mult), [buf, sinb], [t4])
        S.op("dve", lambda e: e.tensor_tensor(out=x1, in0=v(t1), in1=v(t2), op=ALU.subtract), [t1, t2], [buf])
        S.op("dve", lambda e: e.tensor_tensor(out=x2, in0=v(t3), in1=v(t4), op=ALU.add), [t3, t4], [buf])

    S.phase_begin()
    gb = {n: k.gain_bc("g_" + n, d_, HD) for n, d_ in (("kc", kcg), ("ks", ksg), ("kw", kwg))}
    invf_bc = k.gain_bc("invf", invf, 16)
    KcT = S.sb("KcT", [128, NG, T], BF16)
    VcT = S.sb("VcT", [128, NG, T], BF16)
    w1s = {"k": S.sb("w1k", [128, 32, 256], BF16), "v": S.sb("w1v", [128, 32, 256], BF16)}
    w2s = {"k": S.sb("w2k", [128, 2, HD], BF16), "v": S.sb("w2v", [128, 2, HD], BF16)}
    peT = {"k": S.sb("peTk", [128, 32], BF16), "v": S.sb("peTv", [128, 32], BF16)}
    for nm, w1d, w2d, ped in (("k", wk1, wk2, pe_k), ("v", wv1, wv2, pe_v)):
        S.dma("pool", w1s[nm][:], w1d.t.rearrange("(j d) n -> d j n", d=HD), [w1d], [w1s[nm]], w1s[nm])
        S.dma("pool", w2s[nm][:], w2d.t.rearrange("(c p) n -> p c n", p=128), [w2d], [w2s[nm]], w2s[nm])
        S.dma("pool", peT[nm][:], ped.t.rearrange("j d -> d j"), [ped], [peT[nm]], peT[nm], allow_slow_non_contiguous=True)
    ov_s = S.sb("ov_s", [128, NIT, 64], BF16)
    S.dma("pool", ov_s[:], ov_d.t.rearrange("(c p) n -> p c n", p=128), [ov_d], [ov_s], ov_s)
    kvl = S.sb("kvl", [128, 6 * NG * HD], F32)
    kvn = S.sb("kvn", [128, 4 * NG * HD], BF16)
    pos_i = S.sb("pos_i", [128, 1], I32)
    ang = S.sb("ang", [128, 16], F32)
    angi = S.sb("angi", [128, 16], I32)
    rt = [S.sb(f"rt{i}", [128, 128], F32) for i in range(4)]
    kT4 = S.sb("kT4", [128, 8, 128], BF16)
    for tt in range(NT):
        t0 = tt * 128
        S.dma("sp", kvl[:], P[t0:t0 + 128, OKC:OKC + 6 * NG * HD], [P], [kvl], kvl)
        S.dma("sp", pos_i[:], pos[t0:t0 + 128, :], [pos], [pos_i], pos_i)
        S.op("dve", lambda e: e.tensor_copy(out=ang[:, 0:1], in_=pos_i[:]), [pos_i], [ang])
        S.op("dve", lambda e: e.tensor_scalar(out=ang[:], in0=invf_bc[:], scalar1=ang[:, 0:1], scalar2=None, op0=ALU.mult), [ang, invf_bc], [ang])
        for ri, shift in ((0, 0.0), (1, 0.5 * math.pi)):
            r_ = rt[ri]
            S.op("dve", lambda e, r_=r_, shift=shift: e.tensor_scalar(out=r_[:, 0:16], in0=ang[:], scalar1=shift, scalar2=None, op0=ALU.add), [ang], [r_])
            S.op("dve", lambda e, r_=r_: e.tensor_scalar(out=r_[:, 16:32], in0=r_[:, 0:16], scalar1=1.0 / (2 * math.pi), scalar2=None, op0=ALU.mult), [r_], [r_])
            S.op("dve", lambda e, r_=r_: e.tensor_scalar(out=r_[:, 16:32], in0=r_[:, 16:32], scalar1=12582912.0, scalar2=None, op0=ALU.add), [r_], [r_])
            S.op("dve", lambda e, r_=r_: e.tensor_scalar(out=r_[:, 16:32], in0=r_[:, 16:32], scalar1=-12582912.0, scalar2=None, op0=ALU.add), [r_], [r_])
            S.op("dve", lambda e, r_=r_: e.scalar_tensor_tensor(out=r_[:, 0:16], in0=r_[:, 16:32], scalar=-2 * math.pi, in1=r_[:, 0:16], op0=ALU.mult, op1=ALU.add), [r_], [r_])
            S.op("dve", lambda e, r_=r_: e.tensor_scalar(out=r_[:, 16:32], in0=r_[:, 0:16], scalar1=math.pi, scalar2=None, op0=ALU.is_gt), [r_], [r_])
            S.op("dve", lambda e, r_=r_: e.scalar_tensor_tensor(out=r_[:, 0:16], in0=r_[:, 16:32], scalar=-2 * math.pi, in1=r_[:, 0:16], op0=ALU.mult, op1=ALU.add), [r_], [r_])
            S.op("dve", lambda e, r_=r_: e.tensor_scalar(out=r_[:, 16:32], in0=r_[:, 0:16], scalar1=-math.pi, scalar2=None, op0=ALU.is_lt), [r_], [r_])
            S.op("dve", lambda e, r_=r_: e.scalar_tensor_tensor(out=r_[:, 0:16], in0=r_[:, 16:32], scalar=2 * math.pi, in1=r_[:, 0:16], op0=ALU.mult, op1=ALU.add), [r_], [r_])
        if tt == NT - 1 and debug:
            dbgr = S.sb("dbgr", [128, 64], F32)
            S.op("dve", lambda e: e.tensor_copy(out=dbgr[:, 0:32], in_=rt[0][:, 0:32]), [rt[0]], [dbgr])
            S.op("dve", lambda e: e.tensor_copy(out=dbgr[:, 32:64], in_=rt[1][:, 0:32]), [rt[1]], [dbgr])
        S.op("act", lambda e, tt=tt: e.activation(out=sinb[:, tt, :], in_=rt[0][:, 0:16], func=AF.Sin), [rt[0]], [sinb])
        S.op("act", lambda e, tt=tt: e.activation(out=cosb[:, tt, :], in_=rt[1][:, 0:16], func=AF.Sin), [rt[1]], [cosb])
        for gi in range(NG):
            for (src_off, gname) in ((2 * NG * HD, "ks"), (4 * NG * HD, "kw")):
                ap = kvl[:, src_off + gi * HD:src_off + (gi + 1) * HD]
                st = k.rms_rstd(kvl, ap, HD)
                S.op("dve", lambda e, ap=ap, st=st, gname=gname: e.scalar_tensor_tensor(out=ap, in0=ap, scalar=st[:, 2:3], in1=gb[gname][:], op0=ALU.mult, op1=ALU.mult), [kvl, st, gb[gname]], [kvl])
        for src_off in (2 * NG * HD, 4 * NG * HD):
            rope(kvl, kvl[:, src_off:src_off + NG * HD].rearrange("p (h d) -> p h d", d=HD), NG, tt, *rt)
        S.op("act", lambda e: e.activation(out=kvn[:, 0:256], in_=kvl[:, 2 * NG * HD:3 * NG * HD], func=AF.Copy), [kvl], [kvn])
        S.op("act", lambda e: e.activation(out=kvn[:, 256:512], in_=kvl[:, 4 * NG * HD:5 * NG * HD], func=AF.Copy), [kvl], [kvn])
        S.op("act", lambda e: e.activation(out=kvn[:, 512:1024], in_=kvl[:, 0:2 * NG * HD], func=AF.Copy), [kvl], [kvn])
        k.transpose_to(kvn, lambda c: kvn[:, c * 128:(c + 1) * 128], 8, kT4, lambda c0, c1: kT4[:, c0:c1, :])
        for gi in range(NG):
            S.op("dve", lambda e, gi=gi, t0=t0: e.tensor_copy(out=KsT[:, gi, t0:t0 + 128], in_=kT4[:, gi, :]), [kT4], [KsT])
            S.op("dve", lambda e, gi=gi, t0=t0: e.tensor_copy(out=KwT[:, gi, t0:t0 + 128], in_=kT4[:, 2 + gi, :]), [kT4], [KwT])
            S.op("pool", lambda e, gi=gi, t0=t0: e.tensor_copy(out=KcT[:, gi, t0:t0 + 128], in_=kT4[:, 4 + gi, :]), [kT4], [KcT])
            S.op("pool", lambda e, gi=gi, t0=t0: e.tensor_copy(out=VcT[:, gi, t0:t0 + 128], in_=kT4[:, 6 + gi, :]), [kT4], [VcT])
        S.op("act", lambda e, tt=tt: e.activation(out=Vs[:, tt, :, 0:HD], in_=kvl[:, 3 * NG * HD:4 * NG * HD].rearrange("p (g d) -> p g d", d=HD), func=AF.Copy), [kvl], [Vs])
        S.op("act", lambda e, tt=tt: e.activation(out=Vw[:, tt, :, 0:HD], in_=kvl[:, 5 * NG * HD:6 * NG * HD].rearrange("p (g d) -> p g d", d=HD), func=AF.Copy), [kvl], [Vw])
    hT = S.sb("hT", [128, 2, NIT * 128], BF16)
    cbias = S.sb("cbias", [128, 2], F32)
    cm_f = S.sb("cm_f", [128, HD], F32)
    cm_b = S.sb("cm_b", [128, HD], BF16)
    for nm, UT in (("k", KcT), ("v", VcT)):
        for hc in range(2):
            acc = k.acc.next()
            for j in range(32):
                S.op("pe", lambda e, acc=acc, j=j, hc=hc, nm=nm: e.matmul(acc[:, 0:1], lhsT=w1s[nm][:, j, hc * 128:(hc + 1) * 128], rhs=peT[nm][:, j:j + 1], start=(j == 0), stop=(j == 31)), [w1s[nm], peT[nm]], [acc])
            S.op("dve", lambda e, acc=acc, hc=hc: e.tensor_copy(out=cbias[:, hc:hc + 1], in_=acc[:, 0:1]), [acc], [cbias])
        for gi in range(NG):
            for hc in range(2):
                acc = k.acc.next()
                for j in range(32):
                    S.op("pe", lambda e, acc=acc, j=j, hc=hc, nm=nm, gi=gi, UT=UT: e.matmul(acc[:, 0:NCMP], lhsT=w1s[nm][:, j, hc * 128:(hc + 1) * 128], rhs=UT[:, gi, j:j + 16 * (NCMP - 1) + 1:16], start=(j == 0), stop=(j == 31)), [w1s[nm], UT], [acc])
                S.op("act", lambda e, acc=acc, hc=hc: e.activation(out=hT[:, hc, 0:NCMP], in_=acc[:, 0:NCMP], func=AF.Silu, bias=cbias[:, hc:hc + 1]), [acc, cbias], [hT])
            for it in range(NIT):
                ni = min(128, NCMP - it * 128)
                acc = k.acc.next()
                for hc in range(2):
                    S.op("pe", lambda e, acc=acc, hc=hc, it=it, ni=ni, nm=nm: e.matmul(acc[0:ni, 0:HD], lhsT=hT[:, hc, it * 128:it * 128 + ni], rhs=w2s[nm][:, hc, :], start=(hc == 0), stop=(hc == 1)), [hT, w2s[nm]], [acc])
                if nm == "k":
                    S.op("pool", lambda e: e.memset(cm_f[:], 0.0), [], [cm_f])
                    S.op("act", lambda e, acc=acc, ni=ni: e.activation(out=cm_f[0:ni, :], in_=acc[0:ni, 0:HD], func=AF.Copy), [acc], [cm_f])
                    k.rmsnorm(cm_f, cm_f[:], gb["kc"], gb["kc"][:], cm_b, cm_b[:], HD)
                    k.transpose_to(cm_b, lambda c: cm_b[:], 1, KcmpT, lambda c0, c1, gi=gi, it=it: KcmpT[:, gi, it * 128:(it + 1) * 128])
                else:
                    S.op("act", lambda e, acc=acc, ni=ni, gi=gi, it=it: e.activation(out=Vcmp[0:ni, gi, it, 0:HD], in_=acc[0:ni, 0:HD], func=AF.Copy), [acc], [Vcmp])
                    S.op("dve", lambda e, ni=ni, gi=gi, it=it: e.tensor_copy(out=Vcmp[0:ni, gi, it, HD:HD + 64], in_=ov_s[0:ni, it, :]), [ov_s], [Vcmp])
                    S.op("pool", lambda e, ni=ni, gi=gi, it=it: e.memset(Vcmp[0:ni, gi, it, HD + 64:HD + 65], 1.0), [], [Vcmp])
    k.dump("ang", ang, ang[:], [128, 16])
    if debug:
        k.dump("rt0", dbgr, dbgr[:], [128, 64])
    k.dump("posi", pos_i, pos_i[:], [128, 1], I32)
    k.dump("invf", invf_bc, invf_bc[:], [128, 16])
    k.dump("cos", cosb, cosb[:], [128, NT, 16])
    k.dump("sin", sinb, sinb[:], [128, NT, 16])
    k.dump("KsT", KsT, KsT[:], [128, NG, T], BF16)
    k.dump("KwT", KwT, KwT[:], [128, NG, T], BF16)
    k.dump("KcmpT", KcmpT, KcmpT[:], [128, NG, NIT * 128], BF16)
    k.dump("Vcmp", Vcmp, Vcmp[:], [128, NG, NIT, CW_], BF16)
    S.phase_end()

    S.phase_begin()
    qg_bc = k.gain_bc("g_q", qg, HD)
    wo_s = S.sb("wo_s", [128, NQH, D], BF16)
    S.dma("pool", wo_s[:], w_o.t.rearrange("(c p) n -> p c n", p=128), [w_o], [wo_s], wo_s)
    efull = S.sb("efull", [64, T], BF16)
    S.dma("pool", efull[:], efull_d[:, :], [efull_d], [efull], efull)
    cmask = S.sb("cmask", [128, 16, 128], BF16)
    S.dma("pool", cmask[:], cmask_d.t.rearrange("c p q -> p c q"), [cmask_d], [cmask], cmask)
    diag = S.sb("diag", [128, 128], BF16)
    sup = S.sb("sup", [128, 128], BF16)
    S.dma("pool", diag[:], diag_d[:, :], [diag_d], [diag], diag)
    S.op("dve", lambda e: e.tensor_scalar(out=sup[:], in0=diag[:], scalar1=-1.0, scalar2=1.0, op0=ALU.mult, op1=ALU.add), [diag], [sup])
    fadd = S.sb("fadd", [128, NT, 64], F32)
    S.dma("sp", fadd[:], fadd_d.t.rearrange("t p j -> p t j"), [fadd_d], [fadd], fadd)
    stp_ = Rot([S.ps("sT", [128, 512], F32) for _ in range(1)])
    mxp = S.ps("mxp", [128, 128], F32)
    oacc = [S.ps("oacc", [128, 512], F32) for _ in range(4)]
    ql = S.sb("ql", [128, NQH * HD], F32)
    gl = S.sb("gl", [128, NQH * 3], F32)
    qn = S.sb("qn", [128, NQH * HD], BF16)
    QT = S.sb("QT", [128, NQH, 128], BF16)
    rq = [S.sb(f"rq{i}", [128, 128], F32) for i in range(4)]
    Eb = Rot([S.sb("Eb", [128, 512], F32) for _ in range(2)])
    Pb = Rot([S.sb("Pb", [128, 512], BF16) for _ in range(2)])
    y = S.sb("y", [128, NQH * HD], F32)
    yb = S.sb("yb", [128, NQH * HD], BF16)
    ybT = S.sb("ybT", [128, NQH, 128], BF16)
    ob = S.sb("ob", [128, D], BF16)
    rc = S.sb("rc", [128, 8], F32)
    imp = S.sb("imp", [128, 64], F32)
    sc2 = S.sb("sc2", [128, 64], F32)
    m8 = S.sb("m8", [128, 16], F32)
    selb = S.sb("selb", [128, 128], BF16)
    selT = S.sb("selT", [64, 128], BF16)
    cf = S.sb("cf", [128, NQH * 3], F32)
    scale = HD ** -0.5
    S.op("pool", lambda e: e.memset(selb[:], 0.0), [], [selb])
    q3 = lambda ap, w=128: ap.rearrange("p (r q) -> p r q", q=w)

    def branch_out(gi, br, first):
        for r in range(NR):
            oa = oacc[r]
            base = 0
            h = gi * NR + r
            S.op("dve", lambda e, oa=oa, base=base: e.reciprocal(out=rc[:, 0:1], in_=oa[:, base + HD:base + HD + 1]), [oa], [rc])
            S.op("dve", lambda e, h=h, br=br: e.tensor_tensor(out=rc[:, 1:2], in0=rc[:, 0:1], in1=gl[:, h * 3 + br:h * 3 + br + 1], op=ALU.mult), [rc, gl], [rc])
            if first:
                S.op("dve", lambda e, oa=oa, base=base, h=h: e.tensor_scalar(out=y[:, h * HD:(h + 1) * HD], in0=oa[:, base:base + HD], scalar1=rc[:, 1:2], scalar2=None, op0=ALU.mult), [oa, rc], [y])
            else:
                S.op("dve", lambda e, oa=oa, base=base, h=h: e.scalar_tensor_tensor(out=y[:, h * HD:(h + 1) * HD], in0=oa[:, base:base + HD], scalar=rc[:, 1:2], in1=y[:, h * HD:(h + 1) * HD], op0=ALU.mult, op1=ALU.add), [oa, rc, y], [y])

    for qt in range(NT):
        t0 = qt * 128
        S.dma("sp", ql[:], P[t0:t0 + 128, OQ:OQ + NQH * HD], [P], [ql], ql)
        S.dma("sp", gl[:], P[t0:t0 + 128, OGL:OGL + NQH * 3], [P], [gl], gl)
        S.op("act", lambda e: e.activation(out=gl[:], in_=gl[:], func=AF.Sigmoid), [gl], [gl])
        for h in range(NQH):
            ap = ql[:, h * HD:(h + 1) * HD]
            st = k.rms_rstd(ql, ap, HD)
            S.op("dve", lambda e, ap=ap, st=st: e.scalar_tensor_tensor(out=ap, in0=ap, scalar=st[:, 2:3], in1=qg_bc[:], op0=ALU.mult, op1=ALU.mult), [ql, st, qg_bc], [ql])
        rope(ql, ql[:].rearrange("p (h d) -> p h d", d=HD), NQH, qt, *rq)
        S.op("act", lambda e: e.activation(out=qn[:], in_=ql[:], func=AF.Copy), [ql], [qn])
        k.transpose_to(qn, lambda c: qn[:, c * 128:(c + 1) * 128], NQH, QT, lambda c0, c1: QT[:, c0:c1, :])
        if qt == 0:
            k.dump("QT", QT, QT[:], [128, NQH, 128], BF16)
        for gi in range(NG):
            qrhs = QT[:, gi * NR:(gi + 1) * NR, :].rearrange("p r q -> p (r q)")
            its = [it for it in range(NIT) if 16 * it * 128 + 31 <= t0 + 127]
            for ii, it in enumerate(its):
                sT = stp_.next()
                S.op("pe", lambda e, sT=sT, it=it, gi=gi, qrhs=qrhs: e.matmul(sT[:], lhsT=KcmpT[:, gi, it * 128:(it + 1) * 128], rhs=qrhs, start=True, stop=True), [KcmpT, QT], [sT])
                E = Eb.next()
                S.op("act", lambda e, sT=sT, E=E: e.activation(out=E[:], in_=sT[:], func=AF.Exp, scale=scale), [sT], [E])
                Pm = Pb.next()
                delta = qt - 16 * it
                if delta >= 16:
                    S.op("dve", lambda e, E=E, Pm=Pm: e.tensor_copy(out=Pm[:], in_=E[:]), [E], [Pm])
                else:
                    S.op("dve", lambda e, E=E, Pm=Pm, delta=delta: e.tensor_tensor(out=q3(Pm[:]), in0=q3(E[:]), in1=cmask[:, delta, :].unsqueeze(1).to_broadcast([128, NR, 128]), op=ALU.mult), [E, cmask], [Pm])
                for r in range(NR):
                    oa = oacc[r]
                    base = 0
                    S.op("pe", lambda e, oa=oa, base=base, Pm=Pm, r=r, it=it, gi=gi, ii=ii: e.matmul(oa[:, base:base + HD + 65], lhsT=Pm[:, r * 128:(r + 1) * 128], rhs=Vcmp[:, gi, it, 0:HD + 65], start=(ii == 0), stop=(ii == len(its) - 1)), [Pm, Vcmp], [oa])
            if its:
                for r in range(NR):
                    oa = oacc[r]
                    base = 0
                    S.op("dve", lambda e, oa=oa, base=base, r=r: e.tensor_scalar(out=rc[:, 2 + r:3 + r], in0=oa[:, base + HD + 64:base + HD + 65], scalar1=1e-30, scalar2=None, op0=ALU.max), [oa], [rc])
                    S.op("dve", lambda e, r=r: e.reciprocal(out=rc[:, 2 + r:3 + r], in_=rc[:, 2 + r:3 + r]), [rc], [rc])
                    if r == 0:
                        S.op("dve", lambda e, oa=oa, base=base, r=r: e.tensor_scalar(out=imp[:], in0=oa[:, base + HD:base + HD + 64], scalar1=rc[:, 2 + r:3 + r], scalar2=None, op0=ALU.mult), [oa, rc], [imp])
                    else:
                        S.op("dve", lambda e, oa=oa, base=base, r=r: e.scalar_tensor_tensor(out=imp[:], in0=oa[:, base + HD:base + HD + 64], scalar=rc[:, 2 + r:3 + r], in1=imp[:], op0=ALU.mult, op1=ALU.add), [oa, rc, imp], [imp])
                    h = gi * NR + r
                    S.op("dve", lambda e, r=r, h=h: e.tensor_tensor(out=rc[:, 1:2], in0=rc[:, 2 + r:3 + r], in1=gl[:, h * 3:h * 3 + 1], op=ALU.mult), [rc, gl], [rc])
                    S.op("dve", lambda e, oa=oa, base=base, h=h: e.tensor_scalar(out=y[:, h * HD:(h + 1) * HD], in0=oa[:, base:base + HD], scalar1=rc[:, 1:2], scalar2=None, op0=ALU.mult), [oa, rc], [y])
            else:
                S.op("pool", lambda e: e.memset(imp[:], 0.0), [], [imp])
                S.op("pool", lambda e, gi=gi: e.memset(y[:, gi * NR * HD:(gi + 1) * NR * HD], 0.0), [], [y])
            S.op("dve", lambda e, qt=qt: e.tensor_tensor(out=imp[:], in0=imp[:], in1=fadd[:, qt, :], op=ALU.add), [imp, fadd], [imp])
            S.op("dve", lambda e: e.max(out=m8[:, 0:8], in_=imp[:]), [imp], [m8])
            S.op("dve", lambda e: e.match_replace(out=sc2[:], in_to_replace=m8[:, 0:8], in_values=imp[:], imm_value=-3.0e9), [imp, m8], [sc2])
            S.op("dve", lambda e: e.max(out=m8[:, 8:16], in_=sc2[:]), [sc2], [m8])
            S.op("dve", lambda e: e.tensor_scalar(out=sc2[:], in0=imp[:], scalar1=m8[:, 15:16], scalar2=None, op0=ALU.is_ge), [imp, m8], [sc2])
            S.op("dve", lambda e: e.tensor_scalar(out=imp[:], in0=imp[:], scalar1=-1.0e8, scalar2=None, op0=ALU.is_gt), [imp], [imp])
            S.op("dve", lambda e: e.tensor_tensor(out=selb[:, 0:64], in0=sc2[:], in1=imp[:], op=ALU.mult), [sc2, imp], [selb])
            tp = k.tp.next()
            S.op("pe", lambda e, tp=tp: e.transpose(out=tp[:, 0:128], in_=selb[:], identity=k.ident[:]), [selb, k.ident], [tp])
            S.op("dve", lambda e, tp=tp: e.tensor_copy(out=selT[:], in_=tp[0:64, 0:128]), [tp], [selT])
            for kt in range(qt + 1):
                sT = stp_.next()
                S.op("pe", lambda e, sT=sT, kt=kt, gi=gi, qrhs=qrhs: e.matmul(sT[:], lhsT=KsT[:, gi, kt * 128:(kt + 1) * 128], rhs=qrhs, start=True, stop=True), [KsT, QT], [sT])
                S.op("pe", lambda e, kt=kt: e.matmul(mxp[:], lhsT=efull[:, kt * 128:(kt + 1) * 128], rhs=selT[:], start=True, stop=True), [efull, selT], [mxp])
                E = Eb.next()
                S.op("act", lambda e, sT=sT, E=E: e.activation(out=E[:], in_=sT[:], func=AF.Exp, scale=scale), [sT], [E])
                Pm = Pb.next()
                if kt == qt:
                    S.op("dve", lambda e, E=E: e.tensor_tensor(out=q3(E[:]), in0=q3(E[:]), in1=diag[:].unsqueeze(1).to_broadcast([128, NR, 128]), op=ALU.mult), [E, diag], [E])
                S.op("dve", lambda e, E=E, Pm=Pm: e.tensor_tensor(out=q3(Pm[:]), in0=q3(E[:]), in1=mxp[:].unsqueeze(1).to_broadcast([128, NR, 128]), op=ALU.mult), [E, mxp], [Pm])
                for r in range(NR):
                    oa = oacc[r]
                    base = 0
                    S.op("pe", lambda e, oa=oa, base=base, Pm=Pm, r=r, kt=kt, gi=gi, qt=qt: e.matmul(oa[:, base:base + HD + 1], lhsT=Pm[:, r * 128:(r + 1) * 128], rhs=Vs[:, kt, gi, 0:HD + 1], start=(kt == 0), stop=(kt == qt)), [Pm, Vs], [oa])
            branch_out(gi, 1, False)
            kts = [kt for kt in range(qt - 4, qt + 1) if kt >= 0]
            for kt in kts:
                sT = stp_.next()
                S.op("pe", lambda e, sT=sT, kt=kt, gi=gi, qrhs=qrhs: e.matmul(sT[:], lhsT=KwT[:, gi, kt * 128:(kt + 1) * 128], rhs=qrhs, start=True, stop=True), [KwT, QT], [sT])
                E = Eb.next()
                S.op("act", lambda e, sT=sT, E=E: e.activation(out=E[:], in_=sT[:], func=AF.Exp, scale=scale), [sT], [E])
                Pm = Pb.next()
                if kt == qt:
                    S.op("dve", lambda e, E=E, Pm=Pm: e.tensor_tensor(out=q3(Pm[:]), in0=q3(E[:]), in1=diag[:].unsqueeze(1).to_broadcast([128, NR, 128]), op=ALU.mult), [E, diag], [Pm])
                elif kt == qt - 4:
                    S.op("dve", lambda e, E=E, Pm=Pm: e.tensor_tensor(out=q3(Pm[:]), in0=q3(E[:]), in1=sup[:].unsqueeze(1).to_broadcast([128, NR, 128]), op=ALU.mult), [E, sup], [Pm])
                else:
                    S.op("dve", lambda e, E=E, Pm=Pm: e.tensor_copy(out=Pm[:], in_=E[:]), [E], [Pm])
                for r in range(NR):
                    oa = oacc[r]
                    base = 0
                    S.op("pe", lambda e, oa=oa, base=base, Pm=Pm, r=r, kt=kt, gi=gi, kts=kts: e.matmul(oa[:, base:base + HD + 1], lhsT=Pm[:, r * 128:(r + 1) * 128], rhs=Vw[:, kt, gi, 0:HD + 1], start=(kt == kts[0]), stop=(kt == kts[-1])), [Pm, Vw], [oa])
            branch_out(gi, 2, False)
        S.op("act", lambda e: e.activation(out=yb[:], in_=y[:], func=AF.Copy), [y], [yb])
        k.transpose_to(yb, lambda c: yb[:, c * 128:(c + 1) * 128], NQH, ybT, lambda c0, c1: ybT[:, c0:c1, :])
        for nn in range(4):
            acc = k.acc.next()
            for c in range(NQH):
                S.op("pe", lambda e, acc=acc, c=c, nn=nn: e.matmul(acc[:], lhsT=ybT[:, c, :], rhs=wo_s[:, c, nn * 512:(nn + 1) * 512], start=(c == 0), stop=(c == NQH - 1)), [ybT, wo_s], [acc])
            S.op("act", lambda e, acc=acc, nn=nn: e.activation(out=ob[:, nn * 512:(nn + 1) * 512], in_=acc[:], func=AF.Copy), [acc], [ob])
        S.dma("sp", part[t0:t0 + 128, :], ob[:], [ob], [part], ob)
    S.phase_end()
    S.emit()
    return nc


def _nsa_consts(T):
    NT = T // 128
    NCMP = (T - 32) // 16 + 1
    NIT = (NCMP + 127) // 128
    NBLK = T // 64
    c0 = np.arange(NIT * 128)[:, None] * 16
    s0 = np.arange(64)[None, :] * 64
    ov = np.clip(np.minimum(c0 + 32, s0 + 64) - np.maximum(c0, s0), 0, None) / 16.0
    ov[NCMP:, :] = 0.0
    ov[:, NBLK:] = 0.0
    efull = (np.arange(T)[None, :] // 64 == np.arange(64)[:, None]).astype(np.float32)
    ip = np.arange(128)[:, None]
    qp = np.arange(128)[None, :]
    cmask = np.stack([(16 * ip + 31 <= qp + 128 * d) for d in range(16)], 0).astype(np.float32)
    diag = (ip <= qp).astype(np.float32)
    t = np.arange(T)[:, None]
    blk = np.arange(64)[None, :]
    cur = t // 64
    causal = blk <= cur
    forced = ((blk == 0) | (blk >= cur - 1)) & causal
    fadd = np.where(forced, 1.0e9, np.where(causal, 0.0, -1.0e9)).astype(np.float32).reshape(NT, 128, 64)
    invf = np.exp(-math.log(500000.0) * np.arange(0, 32, 2, dtype=np.float32) / 32).astype(np.float32)
    return dict(c_ov=ov.astype(np.float32), c_efull=efull, c_cmask=cmask, c_diag=diag, c_fadd=fadd, c_invf=invf)


def _nsa_cols(gg):
    Q0 = 2048 + 3072 + 32
    q = Q0 + np.arange(8 * gg * 128, 8 * gg * 128 + 1024)
    parts = [q]
    off = Q0 + 2048
    for i in range(6):
        parts.append(off + i * 512 + np.arange(2 * gg * 128, 2 * gg * 128 + 256))
    gl = off + 6 * 512 + np.arange(8 * gg * 3, 8 * gg * 3 + 24)
    parts.append(gl)
    return np.concatenate(parts)


def _nsa_inputs(inp, xb, posb, gg, T):
    f32 = lambda a: np.ascontiguousarray(np.asarray(a), dtype=np.float32)
    m = dict(c_ident=_ident(), xb=xb, g_n=f32(inp["norm_mix"][0]), w_in=f32(np.asarray(inp["ev_w_in"][0])[:, _nsa_cols(gg)]),
             pos=np.ascontiguousarray(np.asarray(posb).astype(np.int32).reshape(T, 1)),
             qg=f32(inp["ev_q_gain"][0]), kcg=f32(inp["ev_kc_gain"][0]), ksg=f32(inp["ev_ks_gain"][0]), kwg=f32(inp["ev_kw_gain"][0]),
             pe_k=f32(inp["ev_pe_k"][0]), pe_v=f32(inp["ev_pe_v"][0]), wk1=f32(inp["ev_cmp_wk1"][0]), wv1=f32(inp["ev_cmp_wv1"][0]),
             wk2=f32(inp["ev_cmp_wk2"][0]), wv2=f32(inp["ev_cmp_wv2"][0]),
             w_o=f32(np.asarray(inp["ev_w_out"][0])[2048 + gg * 1024:2048 + (gg + 1) * 1024]))
    m.update(_nsa_consts(T))
    return m


NCORES = 8
SEQ = 4096
BATCH = 4
TSH = 2048
_cache = {}


def _run(key, builder, in_maps):
    if key not in _cache:
        _cache[key] = builder()
    res = run_bass_kernel_spmd(_cache[key], in_maps, core_ids=list(range(NCORES)))
    return res.results


def _rwkv_inputs(inp, h1b, hh):
    f32 = lambda a: np.ascontiguousarray(np.asarray(a), dtype=np.float32)
    cols = slice(hh * RC, (hh + 1) * RC)
    g = lambda n: np.asarray(inp[n][0])
    return dict(c_ident=_ident(), h1=h1b, g_n=f32(inp["norm_mix"][1]), mu=f32(g("od_mu")),
                w_r=f32(g("od_w_r")[:, cols]), w_k=f32(g("od_w_k")[:, cols]), w_v=f32(g("od_w_v")[:, cols]), w_o=f32(g("od_w_o")[cols, :]),
                w0=f32(g("od_w0")[cols]), a0=f32(g("od_a0")[cols]), k_k=f32(g("od_k_k")[cols]), k_a=f32(g("od_k_a")[cols]),
                r_k=f32(g("od_r_k").reshape(-1)[cols]), ln_w=f32(g("od_ln_w")[cols]), ln_b=f32(g("od_ln_b")[cols]),
                w1=f32(g("od_w1")), w2=f32(g("od_w2")[:, cols]), a1=f32(g("od_a1")), a2=f32(g("od_a2")[:, cols]),
                g1=f32(g("od_g1")), g2=f32(g("od_g2")[:, cols]))


def kernel(**inp):
    f32 = lambda a: np.ascontiguousarray(np.asarray(a), dtype=np.float32)
    x = f32(inp["x"])
    mem = f32(inp["mem"])
    positions = np.asarray(inp["positions"])
    ident = _ident()

    def tok_shard(arr, c):
        b, half = c // 2, c % 2
        return np.ascontiguousarray(arr[b, half * TSH:(half + 1) * TSH])

    def xattn_w(layer, c):
        return dict(mem=mem[c // 2], g_x=f32(inp["norm_xattn"][layer]), g_m=f32(inp["norm_mem"][layer]),
                    g_f=f32(inp["norm_ffn"][layer]), wq=f32(inp["xattn_wq"][layer]), wkv=f32(inp["xattn_wkv"][layer]),
                    wo=f32(inp["xattn_wo"][layer]), qg=f32(inp["xattn_q_gain"][layer]), kg=f32(inp["xattn_k_gain"][layer]))

    r_ssm = _run("ssm", lambda: build_ssm(SEQ), [_ssm_inputs(inp, x[c // 2], c % 2) for c in range(NCORES)])
    p_ssm = [r_ssm[c]["part"] for c in range(NCORES)]
    r_nsa = _run("nsa", lambda: build_nsa(SEQ), [_nsa_inputs(inp, x[c // 2], positions[c // 2], c % 2, SEQ) for c in range(NCORES)])
    p_nsa = [r_nsa[c]["part"] for c in range(NCORES)]
    maps = []
    for c in range(NCORES):
        b, half = c // 2, c % 2
        sl = slice(half * TSH, (half + 1) * TSH)
        parts = np.stack([p_ssm[2 * b][sl], p_ssm[2 * b + 1][sl], p_nsa[2 * b][sl], p_nsa[2 * b + 1][sl]], axis=0)
        m = dict(c_ident=ident, xres=tok_shard(x, c), parts=parts, w1=f32(inp["ev_ffn_w1"][0]), w3=f32(inp["ev_ffn_w3"][0]), w2=f32(inp["ev_ffn_w2"][0]))
        m.update(xattn_w(0, c))
        maps.append(m)
    r = _run("mid0", lambda: build_mid(TSH, 4, 5632, False), maps)
    h1 = [r[c]["h_out"] for c in range(NCORES)]
    maps = [_rwkv_inputs(inp, np.concatenate([h1[2 * (c // 2)], h1[2 * (c // 2) + 1]], axis=0), c % 2) for c in range(NCORES)]
    r_rw = _run("rwkv", lambda: build_rwkv(SEQ), maps)
    p_rw = [r_rw[c]["part"] for c in range(NCORES)]
    maps = []
    for c in range(NCORES):
        b, half = c // 2, c % 2
        sl = slice(half * TSH, (half + 1) * TSH)
        parts = np.stack([p_rw[2 * b][sl], p_rw[2 * b + 1][sl]], axis=0)
        m = dict(c_ident=ident, xres=h1[c], parts=parts, router=f32(inp["od_router"][0]))
        m.update(xattn_w(1, c))
        maps.append(m)
    r = _run("mid1", lambda: build_mid(TSH, 2, 0, True), maps)
    h2 = [r[c]["h_out"] for c in range(NCORES)]
    hn_all = np.concatenate([r[c]["hn_out"] for c in range(NCORES)], axis=0)
    gates = np.concatenate([r[c]["gate_out"] for c in range(NCORES)], axis=0)
    maps = []
    for e in range(NCORES):
        maps.append(dict(c_ident=ident, hn=hn_all, gate=np.ascontiguousarray(gates[:, e:e + 1]),
                         w1=f32(inp["od_moe_w1"][0, e]), w3=f32(inp["od_moe_w3"][0, e]), w2=f32(inp["od_moe_w2"][0, e])))
    r = _run("moe", lambda: build_moe(BATCH * SEQ, 7168), maps)
    maps = []
    for c in range(NCORES):
        parts = np.stack([r[e]["part"][c * TSH:(c + 1) * TSH] for e in range(NCORES)], axis=0)
        maps.append(dict(xres=h2[c], parts=parts))
    r = _run("fin", lambda: build_fin(TSH, NCORES), maps)
    out = np.stack([np.concatenate([r[2 * b]["out"], r[2 * b + 1]["out"]], axis=0) for b in range(BATCH)], axis=0)
    return out.astype(np.float32)
```

```python
import math
import numpy as np
import ml_dtypes
import concourse.bass as bass
import concourse.mybir as mybir
from concourse.bass_utils import run_bass_kernel_spmd
from contextlib import ExitStack

F32 = mybir.dt.float32
BF16 = mybir.dt.bfloat16
I32 = mybir.dt.int32
ALU = mybir.AluOpType
AF = mybir.ActivationFunctionType
AX = mybir.AxisListType

D = 2048
KC = D // 128
NORM_EPS = 1e-6


class Buf:
    __slots__ = ("name", "t", "last_w", "readers")

    def __init__(self, name, t):
        self.name = name
        self.t = t
        self.last_w = None
        self.readers = []

    def __getitem__(self, k):
        return self.t[k]


class Sched:
    def __init__(self, nc):
        self.nc = nc
        self.ops = []
        self.es = ExitStack()
        self.uid = 0

    def sb(self, name, shape, dt):
        self.uid += 1
        nm = f"{name}_{self.uid}"
        es = self.phase_es if getattr(self, "phase_es", None) is not None else self.es
        return Buf(nm, es.enter_context(self.nc.sbuf_tensor(nm, list(shape), dt)))

    def phase_begin(self):
        self.phase_es = ExitStack()

    def phase_end(self):
        self.ops.append(dict(barrier=True))
        self.phase_es.close()
        self.phase_es = None

    def ps(self, name, shape, dt=F32):
        self.uid += 1
        nm = f"{name}_{self.uid}"
        return Buf(nm, self.es.enter_context(self.nc.psum_tensor(nm, list(shape), dt)))

    def dram(self, name, shape, dt, kind="Internal"):
        return Buf(name, self.nc.dram_tensor(name, list(shape), dt, kind=kind).ap())

    def op(self, eng, fn, reads=(), writes=(), dma=False, owner=None):
        self.ops.append(dict(eng=eng, fn=fn, reads=list(reads), writes=list(writes), dma=dma, owner=owner))

    def dma(self, q, out_ap, in_ap, reads, writes, owner, **kw):
        self.op(q, lambda e: e.dma_start(out=out_ap, in_=in_ap, **kw), reads, writes, dma=True, owner=owner)

    def emit(self):
        nc = self.nc
        ops = self.ops
        n = len(ops)
        deps = [None] * n
        needed = [False] * n
        last_on = {}
        bar_deps = []
        pending = set()
        for i, o in enumerate(ops):
            if o.get("barrier"):
                bar_deps = list(last_on.values())
                pending = set(["pe", "act", "dve", "pool", "sp"])
                deps[i] = []
                o.update(eng=None, dma=False, reads=[], writes=[])
                continue
            d = set()
            if bar_deps and o["eng"] in pending:
                d.update(bar_deps)
                pending.discard(o["eng"])
            last_on[("dma", o["owner"].name) if o["dma"] else (o["eng"],)] = i
            for r in o["reads"]:
                if r.last_w is not None:
                    d.add(r.last_w)
            for w in o["writes"]:
                if w.last_w is not None:
                    d.add(w.last_w)
                d.update(w.readers)
            d.discard(i)
            dd = []
            for j in d:
                oj = ops[j]
                if (not oj["dma"]) and (not o["dma"]) and oj["eng"] == o["eng"] == "pe":
                    continue
                dd.append(j)
            deps[i] = dd
            for j in dd:
                needed[j] = True
            for r in o["reads"]:
                r.readers.append(i)
            for w in o["writes"]:
                w.last_w = i
                w.readers = []
        engs = ["pe", "act", "dve", "pool", "sp"]
        esem = {e: self.es.enter_context(nc.semaphore("s_" + e)) for e in engs}
        ecount = {e: 0 for e in engs}
        dsem = {}
        dcount = {}
        tok = [None] * n
        for i, o in enumerate(ops):
            if o.get("barrier"):
                continue
            if o["dma"]:
                ow = o["owner"]
                if ow not in dsem:
                    dsem[ow] = self.es.enter_context(nc.semaphore("d_" + ow.name))
                    dcount[ow] = 0
                dcount[ow] += 16
                tok[i] = (dsem[ow], dcount[ow], 16)
            elif needed[i]:
                ecount[o["eng"]] += 1
                tok[i] = (esem[o["eng"]], ecount[o["eng"]], 1)
        streams = {e: [] for e in engs}
        seen = {e: {} for e in engs}
        for i, o in enumerate(ops):
            if o.get("barrier"):
                continue
            e = o["eng"]
            waits = {}
            for j in deps[i]:
                s, v, _ = tok[j]
                if seen[e].get(id(s), 0) >= v:
                    continue
                if waits.get(id(s), (None, 0))[1] < v:
                    waits[id(s)] = (s, v)
            for k, (s, v) in waits.items():
                seen[e][k] = v
            streams[e].append((list(waits.values()), o["fn"], tok[i]))

        def run_stream(engine, lst):
            for waits, fn, t in lst:
                for s, v in waits:
                    engine.wait_ge(s, v)
                ins = fn(engine)
                if t is not None:
                    ins.then_inc(t[0], t[2])

        with nc.Block() as block:
            @block.tensor
            def _(eng):
                run_stream(eng, streams["pe"])

            @block.scalar
            def _(eng):
                run_stream(eng, streams["act"])

            @block.vector
            def _(eng):
                run_stream(eng, streams["dve"])

            @block.gpsimd
            def _(eng):
                run_stream(eng, streams["pool"])

            @block.sync
            def _(eng):
                run_stream(eng, streams["sp"])
                for ow, s in dsem.items():
                    eng.wait_ge(s, dcount[ow])
        self.es.close()


class Rot:
    def __init__(self, bufs):
        self.bufs = bufs
        self.i = 0

    def next(self):
        b = self.bufs[self.i % len(self.bufs)]
        self.i += 1
        return b


class K:
    def __init__(self, nc, n_tp=2, n_acc=4):
        self.nc = nc
        self.S = Sched(nc)
        S = self.S
        self.ident_d = S.dram("c_ident", [128, 128], F32, kind="ExternalInput")
        self.ident = S.sb("ident", [128, 128], BF16)
        S.dma("pool", self.ident[:], self.ident_d[:], [self.ident_d], [self.ident], self.ident)
        self.tp = Rot([S.ps("tp", [128, 1024], BF16) for _ in range(n_tp)])
        self.acc = Rot([S.ps("acc", [128, 512], F32) for _ in range(n_acc)])
        self.junk = S.sb("junk", [128, 2048], BF16)
        self.stat = Rot([S.sb("stat", [128, 8], F32) for _ in range(6)])
        self.wq = 0

    def dq(self):
        return "sp"

    def dump(self, name, buf, ap, shape, dt=F32):
        if not getattr(self, "debug", False):
            return
        S = self.S
        d = S.dram("dbg_" + name, list(shape), dt, kind="ExternalOutput")
        S.dma("sp", d[:], ap, [buf], [d], d)

    def gain_bc(self, name, ap_row, n):
        S = self.S
        b = S.sb(name, [128, n], F32)
        src = ap_row.t if isinstance(ap_row, Buf) else ap_row
        S.dma("sp", b[:], src.partition_broadcast(128), [], [b], b)
        return b

    def rms_rstd(self, x, xap, n, eps=NORM_EPS):
        S = self.S
        st = self.stat.next()
        junk = self.junk
        S.op("act", lambda e: e.activation(out=junk[:, 0:n], in_=xap, func=AF.Square, accum_out=st[:, 0:1]), [x], [junk, st])
        S.op("dve", lambda e: e.tensor_scalar(out=st[:, 1:2], in0=st[:, 0:1], scalar1=1.0 / n, scalar2=eps, op0=ALU.mult, op1=ALU.add), [st], [st])
        S.op("act", lambda e: e.activation(out=st[:, 3:4], in_=st[:, 1:2], func=AF.Sqrt), [st], [st])
        S.op("dve", lambda e: e.reciprocal(out=st[:, 2:3], in_=st[:, 3:4]), [st], [st])
        return st

    def rmsnorm(self, x, xap, gain, gap, out, oap, n, eps=NORM_EPS):
        S = self.S
        st = self.rms_rstd(x, xap, n, eps)
        S.op("dve", lambda e: e.scalar_tensor_tensor(out=oap, in0=xap, scalar=st[:, 2:3], in1=gap, op0=ALU.mult, op1=ALU.mult), [x, st, gain], [out])

    def transpose_to(self, src, src_ap_fn, nchunks, dst, dst_ap_fn, eng_rot=("dve", "act")):
        S = self.S
        for c0 in range(0, nchunks, 8):
            c1 = min(nchunks, c0 + 8)
            tp = self.tp.next()
            for c in range(c0, c1):
                S.op("pe", lambda e, c=c, tp=tp, c0=c0: e.transpose(out=tp[:, (c - c0) * 128:(c - c0 + 1) * 128], in_=src_ap_fn(c), identity=self.ident[:]), [src, self.ident], [tp])
            eng = eng_rot[(c0 // 8) % len(eng_rot)]
            if eng == "dve":
                S.op("dve", lambda e, tp=tp, c0=c0, c1=c1: e.tensor_copy(out=dst_ap_fn(c0, c1), in_=tp[:, 0:(c1 - c0) * 128]), [tp], [dst])
            else:
                S.op("act", lambda e, tp=tp, c0=c0, c1=c1: e.activation(out=dst_ap_fn(c0, c1), in_=tp[:, 0:(c1 - c0) * 128], func=AF.Copy), [tp], [dst])


def _ident():
    return np.eye(128, dtype=np.float32)


BLK = 256
W2W = 128


class Swiglu:
    def __init__(self, k, H, blk=None, w2w=None, n_w2=1):
        S = k.S
        self.k = k
        self.H = H
        self.HC = H // 128
        self.blk = blk or BLK
        self.w2w = w2w or W2W
        self.w1g = Rot([S.sb("w1g", [128, KC, 256], BF16) for _ in range(2)])
        self.w3g = Rot([S.sb("w3g", [128, KC, 256], BF16) for _ in range(2)])
        self.w2n = Rot([S.sb("w2n", [128, self.HC, self.w2w], BF16) for _ in range(n_w2)])
        self.actT = S.sb("actT", [128, self.HC, self.blk], BF16)
        self.sg = Rot([S.sb("sg", [128, self.blk], F32) for _ in range(2)])

    def run(self, xT, w1d, w3d, w2d, out_cb, gate=None):
        k, S, HC, blk, w2w = self.k, self.k.S, self.HC, self.blk, self.w2w
        w1v = w1d.t.rearrange("(kc p) n -> p kc n", p=128)
        w3v = w3d.t.rearrange("(kc p) n -> p kc n", p=128)
        w2v = w2d.t.rearrange("(c p) n -> p c n", p=128)
        actT = self.actT
        for hg in range(self.H // 256):
            w1g = self.w1g.next()
            w3g = self.w3g.next()
            S.dma("pool", w1g[:], w1v[:, :, hg * 256:(hg + 1) * 256], [w1d], [w1g], w1g)
            S.dma("pool", w3g[:], w3v[:, :, hg * 256:(hg + 1) * 256], [w3d], [w3g], w3g)
            for c in range(2):
                ga = k.acc.next()
                ua = k.acc.next()
                for kc in range(KC):
                    S.op("pe", lambda e, ga=ga, w1g=w1g, kc=kc, c=c: e.matmul(ga[:, 0:blk], lhsT=w1g[:, kc, c * 128:(c + 1) * 128], rhs=xT[:, kc, :], start=(kc == 0), stop=(kc == KC - 1)), [w1g, xT], [ga])
                for kc in range(KC):
                    S.op("pe", lambda e, ua=ua, w3g=w3g, kc=kc, c=c: e.matmul(ua[:, 0:blk], lhsT=w3g[:, kc, c * 128:(c + 1) * 128], rhs=xT[:, kc, :], start=(kc == 0), stop=(kc == KC - 1)), [w3g, xT], [ua])
                sg = self.sg.next()
                S.op("act", lambda e, sg=sg, ga=ga: e.activation(out=sg[:], in_=ga[:, 0:blk], func=AF.Silu), [ga], [sg])
                hc = hg * 2 + c
                S.op("dve", lambda e, sg=sg, ua=ua, hc=hc: e.tensor_tensor(out=actT[:, hc, :], in0=sg[:], in1=ua[:, 0:blk], op=ALU.mult), [sg, ua], [actT])
        for nn in range(D // w2w):
            w2n = self.w2n.next()
            S.dma("pool", w2n[:], w2v[:, :, nn * w2w:(nn + 1) * w2w], [w2d], [w2n], w2n)
            for tt in range(blk // 128):
                acc = k.acc.next()
                for c in range(HC):
                    S.op("pe", lambda e, acc=acc, w2n=w2n, c=c, tt=tt: e.matmul(acc[:, 0:w2w], lhsT=actT[:, c, tt * 128:(tt + 1) * 128], rhs=w2n[:, c, :], start=(c == 0), stop=(c == HC - 1)), [actT, w2n], [acc])
                out_cb(tt, nn * w2w, (nn + 1) * w2w, acc)


XH, XD, MEM = 4, 128, 256


def build_mid(T, n_part, H, moe, debug=False):
    nc = bass.Bass("TRN2", target_bir_lowering=False)
    k = K(nc)
    k.debug = debug
    S = k.S
    NT = T // 128
    NB = BLK // 128
    xres = S.dram("xres", [T, D], F32, kind="ExternalInput")
    parts = S.dram("parts", [n_part, T, D], BF16, kind="ExternalInput") if n_part else None
    mem = S.dram("mem", [MEM, D], F32, kind="ExternalInput")
    g_x = S.dram("g_x", [D], F32, kind="ExternalInput")
    g_m = S.dram("g_m", [D], F32, kind="ExternalInput")
    g_f = S.dram("g_f", [D], F32, kind="ExternalInput")
    wq = S.dram("wq", [D, 512], F32, kind="ExternalInput")
    wkv = S.dram("wkv", [D, 1024], F32, kind="ExternalInput")
    wo = S.dram("wo", [512, D], F32, kind="ExternalInput")
    qg = S.dram("qg", [128], F32, kind="ExternalInput")
    kg = S.dram("kg", [128], F32, kind="ExternalInput")
    h_out = S.dram("h_out", [T, D], F32, kind="ExternalOutput")
    if moe:
        router = S.dram("router", [D, 8], F32, kind="ExternalInput")
        hn_out = S.dram("hn_out", [T, D], BF16, kind="ExternalOutput")
        gate_out = S.dram("gate_out", [T, 8], F32, kind="ExternalOutput")
    else:
        w1 = S.dram("w1", [D, H], F32, kind="ExternalInput")
        w3 = S.dram("w3", [D, H], F32, kind="ExternalInput")
        w2 = S.dram("w2", [H, D], F32, kind="ExternalInput")

    gx_bc = k.gain_bc("gx", g_x, D)
    gf_bc = k.gain_bc("gf", g_f, D)
    qg_bc = k.gain_bc("qg", qg, 128)
    kg_bc = k.gain_bc("kg", kg, 128)
    hkeep = S.sb("hkeep", [128, NB, D], F32)
    gm_ap = hkeep[:, 0, :]
    S.dma("sp", gm_ap, g_m.t.partition_broadcast(128), [], [hkeep], hkeep)
    wq_s = S.sb("wq_s", [128, KC, 512], BF16)
    S.dma("pool", wq_s[:], wq.t.rearrange("(kc p) n -> p kc n", p=128), [wq], [wq_s], wq_s)
    wo_s = S.sb("wo_s", [128, 4, D], BF16)
    S.dma("pool", wo_s[:], wo.t.rearrange("(kc p) n -> p kc n", p=128), [wo], [wo_s], wo_s)
    if moe:
        wkvbuf = Rot([S.sb("wkvg", [128, KC, 256], BF16) for _ in range(2)])
    else:
        sw = Swiglu(k, H)
        wkvbuf = sw.w1g
    hbuf = Rot([S.sb("h_f", [128, D], F32) for _ in range(2)])
    pbuf = Rot([S.sb("p_b", [128, D], BF16) for _ in range(2)]) if n_part else None
    hxbuf = Rot([S.sb("hx", [128, D], BF16) for _ in range(2)])
    hxTbuf = Rot([S.sb("hxT", [128, KC, 128], BF16) for _ in range(2)])
    hfT = S.sb("hfT", [128, KC, BLK], BF16)
    kv = S.sb("kv_f", [128, 1024], F32)
    kn = S.sb("k_n", [128, 512], BF16)
    KT = S.sb("KT", [128, XH, MEM], BF16)
    Vx = S.sb("Vx", [128, 2, XH, 132], BF16)
    qf = S.sb("q_f", [128, 512], F32)
    qn = S.sb("q_n", [128, 512], BF16)
    qT = S.sb("qT", [128, XH, 128], BF16)
    pT = S.sb("pT", [128, XH, 2, 128], BF16)
    on = S.sb("o_n", [128, 512], BF16)
    onT = S.sb("o_nT", [128, 4, 128], BF16)
    rs = S.sb("rs", [128, XH], F32)

    S.op("pool", lambda e: e.memset(Vx[:], 1.0), [], [Vx])
    mTs = []
    for mt in range(2):
        mt_f = hbuf.next()
        S.dma("sp", mt_f[:], mem[mt * 128:(mt + 1) * 128, :], [mem], [mt_f], mt_f)
        mn = hxbuf.next()
        k.rmsnorm(mt_f, mt_f[:], hkeep, gm_ap, mn, mn[:], D)
        mT = hxTbuf.next()
        k.transpose_to(mn, lambda c, mn=mn: mn[:, c * 128:(c + 1) * 128], KC, mT, lambda c0, c1, mT=mT: mT[:, c0:c1, :])
        mTs.append(mT)
    wkv_r = wkv.t.rearrange("(kc p) n -> p kc n", p=128)
    kvs = [kv, S.sb("kv_f2", [128, 1024], F32)]
    for nn in range(4):
        wg = wkvbuf.next()
        S.dma("pool", wg[:], wkv_r[:, :, nn * 256:(nn + 1) * 256], [wkv], [wg], wg)
        for mt in range(2):
            acc = k.acc.next()
            for kc in range(KC):
                S.op("pe", lambda e, acc=acc, kc=kc, mt=mt, wg=wg: e.matmul(acc[:, 0:256], lhsT=mTs[mt][:, kc, :], rhs=wg[:, kc, :], start=(kc == 0), stop=(kc == KC - 1)), [mTs[mt], wg], [acc])
            S.op("act", lambda e, acc=acc, nn=nn, mt=mt: e.activation(out=kvs[mt][:, nn * 256:(nn + 1) * 256], in_=acc[:, 0:256], func=AF.Copy), [acc], [kvs[mt]])
    for mt in range(2):
        kvm = kvs[mt]
        for h in range(XH):
            k.rmsnorm(kvm, kvm[:, h * 128:(h + 1) * 128], kg_bc, kg_bc[:], kn, kn[:, h * 128:(h + 1) * 128], 128)
        k.transpose_to(kn, lambda c: kn[:, c * 128:(c + 1) * 128], XH, KT, lambda c0, c1, mt=mt: KT[:, c0:c1, mt * 128:(mt + 1) * 128])
        S.op("dve", lambda e, mt=mt, kvm=kvm: e.tensor_copy(out=Vx[:, mt, :, 0:128], in_=kvm[:, 512:1024].rearrange("p (h d) -> p h d", h=XH)), [kvm], [Vx])

    if moe:
        hf32 = S.sb("hf32", [128, D], F32)
        rt_s = S.sb("rt_s", [128, KC, 8], F32)
        S.dma("sp", rt_s[:], router.t.rearrange("(kc p) e -> p kc e", p=128), [router], [rt_s], rt_s)
        ident_f = S.sb("ident_f", [128, 128], F32)
        S.dma("sp", ident_f[:], k.ident_d[:], [k.ident_d], [ident_f], ident_f)
        hf32T = S.sb("hf32T", [128, KC, 128], F32)
        lg = S.sb("lg", [128, 8], F32)
        m8 = S.sb("m8", [128, 8], F32)
        gt = S.sb("gt", [128, 8], F32)

    scale = XD ** -0.5
    for tt in range(NT):
        bt = tt % NB
        h = hbuf.next()
        S.dma("sp", h[:], xres[tt * 128:(tt + 1) * 128, :], [xres], [h], h)
        for p in range(n_part):
            pb = pbuf.next()
            S.dma("sp", pb[:], parts[p, tt * 128:(tt + 1) * 128, :], [parts], [pb], pb)
            S.op("dve", lambda e, h=h, pb=pb: e.tensor_tensor(out=h[:], in0=h[:], in1=pb[:], op=ALU.add), [h, pb], [h])
        if tt == 0:
            k.dump("h0", h, h[:], [128, D])
        hx = hxbuf.next()
        k.rmsnorm(h, h[:], gx_bc, gx_bc[:], hx, hx[:], D)
        if tt == 0:
            k.dump("hx", hx, hx[:], [128, D], BF16)
        hxT = hxTbuf.next()
        k.transpose_to(hx, lambda c, hx=hx: hx[:, c * 128:(c + 1) * 128], KC, hxT, lambda c0, c1, hxT=hxT: hxT[:, c0:c1, :])
        acc = k.acc.next()
        for kc in range(KC):
            S.op("pe", lambda e, acc=acc, kc=kc, hxT=hxT: e.matmul(acc[:], lhsT=hxT[:, kc, :], rhs=wq_s[:, kc, :], start=(kc == 0), stop=(kc == KC - 1)), [hxT, wq_s], [acc])
        S.op("act", lambda e, acc=acc: e.activation(out=qf[:], in_=acc[:], func=AF.Copy), [acc], [qf])
        if tt == 0:
            k.dump("qf", qf, qf[:], [128, 512])
        for hh in range(XH):
            k.rmsnorm(qf, qf[:, hh * 128:(hh + 1) * 128], qg_bc, qg_bc[:], qn, qn[:, hh * 128:(hh + 1) * 128], 128)
        if tt == 0:
            k.dump("qn", qn, qn[:], [128, 512], BF16)
            k.dump("KT", KT, KT[:], [128, XH, MEM], BF16)
            k.dump("Vx", Vx, Vx[:], [128, 2, XH, 132], BF16)
        k.transpose_to(qn, lambda c: qn[:, c * 128:(c + 1) * 128], XH, qT, lambda c0, c1: qT[:, c0:c1, :])
        for half in range(2):
            sacc = k.acc.next()
            for j in range(4):
                hh, mt = (half * 4 + j) // 2, (half * 4 + j) % 2
                S.op("pe", lambda e, sacc=sacc, j=j, hh=hh, mt=mt: e.matmul(sacc[:, j * 128:(j + 1) * 128], lhsT=KT[:, hh, mt * 128:(mt + 1) * 128], rhs=qT[:, hh, :], start=True, stop=True), [KT, qT], [sacc])
            S.op("act", lambda e, sacc=sacc, half=half: e.activation(out=pT[:, half * 2:(half + 1) * 2, :, :].rearrange("p a b q -> p (a b q)"), in_=sacc[:], func=AF.Exp, scale=scale), [sacc], [pT])
        oacc = k.acc.next()
        for hh in range(XH):
            for mt in range(2):
                S.op("pe", lambda e, oacc=oacc, hh=hh, mt=mt: e.matmul(oacc[:, hh * 128:hh * 128 + 128], lhsT=pT[:, hh, mt, :], rhs=Vx[:, mt, hh, 0:128], start=(mt == 0), stop=(mt == 1)), [pT, Vx], [oacc])
        racc = k.acc.next()
        for hh in range(XH):
            for mt in range(2):
                S.op("pe", lambda e, racc=racc, hh=hh, mt=mt: e.matmul(racc[:, hh:hh + 1], lhsT=pT[:, hh, mt, :], rhs=Vx[:, mt, hh, 128:129], start=(mt == 0), stop=(mt == 1)), [pT, Vx], [racc])
        S.op("dve", lambda e, racc=racc: e.reciprocal(out=rs[:], in_=racc[:, 0:XH]), [racc], [rs])
        for hh in range(XH):
            S.op("dve", lambda e, oacc=oacc, hh=hh: e.tensor_scalar(out=on[:, hh * 128:(hh + 1) * 128], in0=oacc[:, hh * 128:(hh + 1) * 128], scalar1=rs[:, hh:hh + 1], scalar2=None, op0=ALU.mult), [oacc, rs], [on])
        if tt == 0:
            k.dump("pT", pT, pT[:], [128, XH, 2, 128], BF16)
            k.dump("on", on, on[:], [128, 512], BF16)
        k.transpose_to(on, lambda c: on[:, c * 128:(c + 1) * 128], 4, onT, lambda c0, c1: onT[:, c0:c1, :])
        for nn in range(4):
            acc = k.acc.next()
            for kc in range(4):
                S.op("pe", lambda e, acc=acc, kc=kc, nn=nn: e.matmul(acc[:], lhsT=onT[:, kc, :], rhs=wo_s[:, kc, nn * 512:(nn + 1) * 512], start=(kc == 0), stop=(kc == 3)), [onT, wo_s], [acc])
            S.op("dve", lambda e, acc=acc, nn=nn, h=h: e.tensor_tensor(out=h[:, nn * 512:(nn + 1) * 512], in0=h[:, nn * 512:(nn + 1) * 512], in1=acc[:], op=ALU.add), [acc, h], [h])
        hf = hx
        k.rmsnorm(h, h[:], gf_bc, gf_bc[:], hf, hf[:], D)
        if moe:
            S.dma("sp", h_out[tt * 128:(tt + 1) * 128, :], h[:], [h], [h_out], h)
            S.dma("sp", hn_out[tt * 128:(tt + 1) * 128, :], hf[:], [hf], [hn_out], hf)
            k.rmsnorm(h, h[:], gf_bc, gf_bc[:], hf32, hf32[:], D)
            for c0 in range(0, KC, 4):
                tacc = k.acc.next()
                for c in range(c0, c0 + 4):
                    S.op("pe", lambda e, tacc=tacc, c=c, c0=c0: e.transpose(out=tacc[:, (c - c0) * 128:(c - c0 + 1) * 128], in_=hf32[:, c * 128:(c + 1) * 128], identity=ident_f[:]), [hf32, ident_f], [tacc])
                S.op("dve", lambda e, tacc=tacc, c0=c0: e.tensor_copy(out=hf32T[:, c0:c0 + 4, :].rearrange("p a b -> p (a b)"), in_=tacc[:]), [tacc], [hf32T])
            lacc = k.acc.next()
            for kc in range(KC):
                S.op("pe", lambda e, lacc=lacc, kc=kc: e.matmul(lacc[:, 0:8], lhsT=hf32T[:, kc, :], rhs=rt_s[:, kc, :], start=(kc == 0), stop=(kc == KC - 1)), [hf32T, rt_s], [lacc])
            S.op("dve", lambda e, lacc=lacc: e.tensor_copy(out=lg[:], in_=lacc[:, 0:8]), [lacc], [lg])
            S.op("dve", lambda e: e.max(out=m8[:], in_=lg[:]), [lg], [m8])
            S.op("dve", lambda e: e.tensor_scalar(out=gt[:], in0=lg[:], scalar1=m8[:, 1:2], scalar2=None, op0=ALU.is_ge), [lg, m8], [gt])
            S.op("dve", lambda e: e.tensor_scalar(out=lg[:], in0=lg[:], scalar1=m8[:, 0:1], scalar2=None, op0=ALU.subtract), [lg, m8], [lg])
            S.op("act", lambda e: e.activation(out=lg[:], in_=lg[:], func=AF.Exp), [lg], [lg])
            S.op("dve", lambda e: e.tensor_tensor(out=gt[:], in0=gt[:], in1=lg[:], op=ALU.mult), [gt, lg], [gt])
            S.op("dve", lambda e: e.reduce_sum(out=m8[:, 2:3], in_=gt[:], axis=AX.X), [gt], [m8])
            S.op("dve", lambda e: e.reciprocal(out=m8[:, 3:4], in_=m8[:, 2:3]), [m8], [m8])
            S.op("dve", lambda e: e.tensor_scalar(out=gt[:], in0=gt[:], scalar1=m8[:, 3:4], scalar2=None, op0=ALU.mult), [gt, m8], [gt])
            S.dma("sp", gate_out[tt * 128:(tt + 1) * 128, :], gt[:], [gt], [gate_out], gt)
        else:
            k.transpose_to(hf, lambda c, hf=hf: hf[:, c * 128:(c + 1) * 128], KC, hfT, lambda c0, c1, bt=bt: hfT[:, c0:c1, bt * 128:(bt + 1) * 128])
            S.op("pool", lambda e, h=h, bt=bt: e.tensor_copy(out=hkeep[:, bt, :], in_=h[:]), [h], [hkeep])
            if bt == NB - 1:
                t0 = tt - bt

                def out_cb(tq, n0, n1, acc, t0=t0):
                    S.op("dve", lambda e: e.tensor_tensor(out=hkeep[:, tq, n0:n1], in0=hkeep[:, tq, n0:n1], in1=acc[:, 0:n1 - n0], op=ALU.add), [acc, hkeep], [hkeep])
                    if n1 == D:
                        S.dma("sp", h_out[(t0 + tq) * 128:(t0 + tq + 1) * 128, :], hkeep[:, tq, :], [hkeep], [h_out], hkeep)
                sw.run(hfT, w1, w3, w2, out_cb)
    S.emit()
    return nc


def build_moe(T, H):
    nc = bass.Bass("TRN2", target_bir_lowering=False)
    k = K(nc)
    S = k.S
    MB = 512
    NB = MB // 128
    hn = S.dram("hn", [T, D], BF16, kind="ExternalInput")
    gate = S.dram("gate", [T, 1], F32, kind="ExternalInput")
    w1 = S.dram("w1", [D, H], F32, kind="ExternalInput")
    w3 = S.dram("w3", [D, H], F32, kind="ExternalInput")
    w2 = S.dram("w2", [H, D], F32, kind="ExternalInput")
    part = S.dram("part", [T, D], BF16, kind="ExternalOutput")
    sw = Swiglu(k, H, blk=MB, w2w=256, n_w2=2)
    hbuf = Rot([S.sb("hn_t", [128, D], BF16) for _ in range(2)])
    hT = S.sb("hT", [128, KC, MB], BF16)
    gts = S.sb("gts", [128, NB], F32)
    obuf = S.sb("obuf", [128, NB, D], BF16)
    for blk in range(T // MB):
        for bt in range(NB):
            t0 = blk * MB + bt * 128
            hb = hbuf.next()
            S.dma("sp", hb[:], hn[t0:t0 + 128, :], [hn], [hb], hb)
            S.dma("sp", gts[:, bt:bt + 1], gate[t0:t0 + 128, :], [gate], [gts], gts)
            k.transpose_to(hb, lambda c, hb=hb: hb[:, c * 128:(c + 1) * 128], KC, hT, lambda c0, c1, bt=bt: hT[:, c0:c1, bt * 128:(bt + 1) * 128])

        def out_cb(tq, n0, n1, acc, blk=blk):
            S.op("act", lambda e: e.activation(out=obuf[:, tq, n0:n1], in_=acc[:, 0:n1 - n0], func=AF.Copy, scale=gts[:, tq:tq + 1]), [acc, gts], [obuf])
            if n1 == D:
                t0 = blk * MB + tq * 128
                S.dma("sp", part[t0:t0 + 128, :], obuf[:, tq, :], [obuf], [part], obuf)
        sw.run(hT, w1, w3, w2, out_cb)
    S.emit()
    return nc


def build_fin(T, n_part):
    nc = bass.Bass("TRN2", target_bir_lowering=False)
    S = Sched(nc)
    xres = S.dram("xres", [T, D], F32, kind="ExternalInput")
    parts = S.dram("parts", [n_part, T, D], BF16, kind="ExternalInput")
    out = S.dram("out", [T, D], F32, kind="ExternalOutput")
    hbuf = Rot([S.sb("h_f", [128, D], F32) for _ in range(2)])
    pbuf = Rot([S.sb("p_b", [128, D], BF16) for _ in range(3)])
    for tt in range(T // 128):
        h = hbuf.next()
        S.dma("sp", h[:], xres[tt * 128:(tt + 1) * 128, :], [xres], [h], h)
        for p in range(n_part):
            pb = pbuf.next()
            S.dma("sp", pb[:], parts[p, tt * 128:(tt + 1) * 128, :], [parts], [pb], pb)
            S.op("dve", lambda e, h=h, pb=pb: e.tensor_tensor(out=h[:], in0=h[:], in1=pb[:], op=ALU.add), [h, pb], [h])
        S.dma("sp", out[tt * 128:(tt + 1) * 128, :], h[:], [h], [out], h)
    S.emit()
    return nc


RH, RC, RN = 16, 1024, 64
GN_EPS = 1e-5 * 64


def build_rwkv(T):
    nc = bass.Bass("TRN2", target_bir_lowering=False)
    k = K(nc, n_tp=1, n_acc=1)
    S = k.S
    NT = T // 128
    di = lambda n, s, dt=F32: S.dram(n, s, dt, kind="ExternalInput")
    h1 = di("h1", [T, D])
    g_n = di("g_n", [D])
    mu = di("mu", [6, D])
    w_r, w_k, w_v = di("w_r", [D, RC]), di("w_k", [D, RC]), di("w_v", [D, RC])
    w_o = di("w_o", [RC, D])
    vecs = {n: di(n, [RC]) for n in ["w0", "a0", "k_k", "k_a", "r_k", "ln_w", "ln_b"]}
    w1, w2 = di("w1", [D, 96]), di("w2", [96, RC])
    a1, a2 = di("a1", [D, 96]), di("a2", [96, RC])
    g1, g2 = di("g1", [D, 256]), di("g2", [256, RC])
    part = S.dram("part", [T, D], BF16, kind="ExternalOutput")
    HN = S.dram("HN", [T + 1, D], F32)
    P_r, P_k, P_v = S.dram("P_r", [T, RC], F32), S.dram("P_k", [T, RC], F32), S.dram("P_v", [T, RC], F32)
    P_w1, P_a1, P_g1 = S.dram("P_w1", [T, 96], F32), S.dram("P_a1", [T, 96], F32), S.dram("P_g1", [T, 256], F32)

    ident_f = S.sb("ident_f", [128, 128], F32)
    S.dma("sp", ident_f[:], k.ident_d[:], [k.ident_d], [ident_f], ident_f)

    S.phase_begin()
    gn_bc = k.gain_bc("gn", g_n, D)
    zrow = S.sb("zrow", [1, D], F32)
    S.op("pool", lambda e: e.memset(zrow[:], 0.0), [], [zrow])
    S.dma("sp", HN[0:1, :], zrow[:], [zrow], [HN], zrow)
    hb = Rot([S.sb("hb", [128, D], F32) for _ in range(2)])
    ho = Rot([S.sb("ho", [128, D], F32) for _ in range(2)])
    for tt in range(NT):
        h = hb.next()
        o = ho.next()
        S.dma("sp", h[:], h1[tt * 128:(tt + 1) * 128, :], [h1], [h], h)
        k.rmsnorm(h, h[:], gn_bc, gn_bc[:], o, o[:], D)
        S.dma("sp", HN[1 + tt * 128:1 + (tt + 1) * 128, :], o[:], [o], [HN], o)
    S.phase_end()

    TH = min(T, 1024)
    groups = []
    for (wd, j, Pd) in [(w_r, 0, P_r), (w_k, 2, P_k), (w_v, 3, P_v)]:
        for c0 in range(0, RC, 256):
            groups.append((wd, c0, 256, j, Pd, c0))
    groups += [(w1, 0, 96, 1, P_w1, 0), (a1, 0, 96, 4, P_a1, 0), (g1, 0, 256, 5, P_g1, 0)]
    for th in range(T // TH):
        S.phase_begin()
        XT = S.sb("XT", [128, 2 * KC, TH], BF16)
        muT = S.sb("muT", [128, 6, KC], F32)
        S.dma("sp", muT[:], mu.t.rearrange("j (kc p) -> p j kc", p=128), [mu], [muT], muT, allow_slow_non_contiguous=True)
        hnb = Rot([S.sb("hnb", [128, D], F32) for _ in range(2)])
        shb = Rot([S.sb("shb", [128, D], F32) for _ in range(2)])
        cb = Rot([S.sb("cb", [128, 2 * D], BF16) for _ in range(2)])
        for tl in range(TH // 128):
            t0 = th * TH + tl * 128
            hn, sh, c = hnb.next(), shb.next(), cb.next()
            S.dma("sp", hn[:], HN[1 + t0:1 + t0 + 128, :], [HN], [hn], hn)
            S.dma("sp", sh[:], HN[t0:t0 + 128, :], [HN], [sh], sh)
            S.op("act", lambda e, hn=hn, c=c: e.activation(out=c[:, 0:D], in_=hn[:], func=AF.Copy), [hn], [c])
            S.op("dve", lambda e, hn=hn, sh=sh, c=c: e.tensor_tensor(out=c[:, D:2 * D], in0=sh[:], in1=hn[:], op=ALU.subtract), [hn, sh], [c])
            k.transpose_to(c, lambda q, c=c: c[:, q * 128:(q + 1) * 128], 2 * KC, XT, lambda c0, c1, tl=tl: XT[:, c0:c1, tl * 128:(tl + 1) * 128])
        wgb = Rot([S.sb("wg", [128, KC, 256], BF16) for _ in range(2)])
        mwb = Rot([S.sb("mwg", [128, KC, 256], BF16) for _ in range(2)])
        stg = Rot([S.sb("stg", [128, 256], F32) for _ in range(3)])
        for (wd, c0, n, j, Pd, pc0) in groups:
            wg, mw = wgb.next(), mwb.next()
            S.dma("pool", wg[:, :, 0:n], wd.t.rearrange("(kc p) n -> p kc n", p=128)[:, :, c0:c0 + n], [wd], [wg], wg)
            S.op("dve", lambda e, wg=wg, mw=mw, j=j, n=n: e.tensor_tensor(out=mw[:, :, 0:n], in0=wg[:, :, 0:n], in1=muT[:, j, :].unsqueeze(2).to_broadcast([128, KC, n]), op=ALU.mult), [wg, muT], [mw])
            for tl in range(TH // 128):
                t0 = th * TH + tl * 128
                acc = k.acc.next()
                for kc in range(2 * KC):
                    src_w = wg if kc < KC else mw
                    S.op("pe", lambda e, acc=acc, kc=kc, src_w=src_w, tl=tl, n=n: e.matmul(acc[:, 0:n], lhsT=XT[:, kc, tl * 128:(tl + 1) * 128], rhs=src_w[:, kc % KC, 0:n], start=(kc == 0), stop=(kc == 2 * KC - 1)), [XT, src_w], [acc])
                st = stg.next()
                S.op("act", lambda e, acc=acc, st=st, n=n: e.activation(out=st[:, 0:n], in_=acc[:, 0:n], func=AF.Copy), [acc], [st])
                S.dma("sp", Pd[t0:t0 + 128, pc0:pc0 + n], st[:, 0:n], [st], [Pd], st)
        S.phase_end()

    S.phase_begin()
    vb = {}
    for n in vecs:
        vb[n] = k.gain_bc("v_" + n, vecs[n], RC)
    omka = S.sb("omka", [128, RC], F32)
    S.op("dve", lambda e: e.tensor_scalar(out=omka[:], in0=vb["k_a"][:], scalar1=-1.0, scalar2=1.0, op0=ALU.mult, op1=ALU.add), [vb["k_a"]], [omka])
    w2_s = S.sb("w2_s", [96, RC], BF16)
    a2_s = S.sb("a2_s", [96, RC], BF16)
    g2_s = S.sb("g2_s", [128, 2, RC], BF16)
    wo_s = S.sb("wo_s", [128, 8, D], BF16)
    S.dma("pool", w2_s[:], w2[:, :], [w2], [w2_s], w2_s)
    S.dma("pool", a2_s[:], a2[:, :], [a2], [a2_s], a2_s)
    S.dma("pool", g2_s[:], g2.t.rearrange("(c p) n -> p c n", p=128), [g2], [g2_s], g2_s)
    S.dma("pool", wo_s[:], w_o.t.rearrange("(c p) n -> p c n", p=128), [w_o], [wo_s], wo_s)
    St = S.sb("St", [RN, RC], F32)
    S.op("pool", lambda e: e.memset(St[:], 0.0), [], [St])
    bcp = Rot([S.ps("bcp", [128, 1024], F32) for _ in range(3)])
    pr, pk, pv = S.sb("pr", [128, RC], F32), S.sb("pk", [128, RC], F32), S.sb("pv", [128, RC], F32)
    pw1, pa1, pg1 = S.sb("pw1", [128, 96], F32), S.sb("pa1", [128, 96], F32), S.sb("pg1", [128, 256], F32)
    lb = S.sb("lb", [128, 256], BF16)
    lT = S.sb("lT", [128, 2, 128], BF16)
    wr, av, gv = S.sb("wr", [128, RC], F32), S.sb("av", [128, RC], F32), S.sb("gv", [128, RC], F32)
    tA, tW, tB, tK = S.sb("tA", [128, RC], F32), S.sb("tW", [128, RC], F32), S.sb("tB", [128, RC], F32), S.sb("tK", [128, RC], F32)
    tm1, tm2 = S.sb("tm1", [128, RC], F32), S.sb("tm2", [128, RC], F32)
    s16 = S.sb("s16", [128, 4, RH], F32)
    VT = S.sb("VT", [RN, RH, 128], F32)
    YT = S.sb("YT", [RN, RH, 128], F32)
    T1, T2 = S.sb("T1", [RN, RC], F32), S.sb("T2", [RN, RC], F32)
    sa = S.sb("sa", [RN, RH], F32)
    kbs = Rot([S.sb("kbs", [RN, RC], F32) for _ in range(2)])
    t3s = Rot([S.sb("t3s", [RN, RC], F32) for _ in range(2)])
    yb = S.sb("yb", [128, RC], BF16)
    ybT = S.sb("ybT", [128, 8, 128], BF16)
    ob = S.sb("ob", [128, D], BF16)
    v3 = lambda ap: ap.rearrange("p (h j) -> p h j", h=RH)
    bc3 = lambda ap, p=128: ap.unsqueeze(2).to_broadcast([p, RH, RN])

    def lora2(src, n, func, w_s, kparts, dst, bias):
        nch = (n + 127) // 128
        S.op("act", lambda e: e.activation(out=lb[:, 0:n], in_=src[:, 0:n], func=func), [src], [lb])
        k.transpose_to(lb, lambda c: lb[:, c * 128:(c + 1) * 128], nch, lT, lambda c0, c1: lT[:, c0:c1, :])
        for half in range(2):
            acc = k.acc.next()
            for c in range(nch):
                rows = min(n, 128)
                rhs = w_s[0:rows, half * 512:(half + 1) * 512] if kparts == 1 else w_s[:, c, half * 512:(half + 1) * 512]
                S.op("pe", lambda e, acc=acc, c=c, rhs=rhs, rows=rows: e.matmul(acc[:], lhsT=lT[0:rows, c, :], rhs=rhs, start=(c == 0), stop=(c == nch - 1)), [lT, w_s], [acc])
            if bias is not None:
                S.op("dve", lambda e, acc=acc, half=half: e.tensor_tensor(out=dst[:, half * 512:(half + 1) * 512], in0=acc[:], in1=bias[:, half * 512:(half + 1) * 512], op=ALU.add), [acc, bias], [dst])
            else:
                S.op("dve", lambda e, acc=acc, half=half: e.tensor_copy(out=dst[:, half * 512:(half + 1) * 512], in_=acc[:]), [acc], [dst])

    for tt in range(NT):
        t0 = tt * 128
        for (b_, P_, n) in [(pr, P_r, RC), (pk, P_k, RC), (pv, P_v, RC), (pw1, P_w1, 96), (pa1, P_a1, 96), (pg1, P_g1, 256)]:
            S.dma("sp", b_[:, 0:n], P_[t0:t0 + 128, :], [P_], [b_], b_)
        lora2(pw1, 96, AF.Tanh, w2_s, 1, wr, vb["w0"])
        lora2(pa1, 96, AF.Copy, a2_s, 1, av, vb["a0"])
        lora2(pg1, 256, AF.Sigmoid, g2_s, 2, gv, None)
        S.op("act", lambda e: e.activation(out=av[:], in_=av[:], func=AF.Sigmoid), [av], [av])
        S.op("act", lambda e: e.activation(out=tm1[:], in_=wr[:], func=AF.Exp, scale=-1.0), [wr], [tm1])
        S.op("act", lambda e: e.activation(out=tm1[:], in_=tm1[:], func=AF.Ln, bias=1.0), [tm1], [tm1])
        S.op("act", lambda e: e.activation(out=tm1[:], in_=tm1[:], func=AF.Exp, scale=-1.0, bias=-0.5), [tm1], [tm1])
        S.op("act", lambda e: e.activation(out=tW[:], in_=tm1[:], func=AF.Exp, scale=-1.0), [tm1], [tW])
        S.op("dve", lambda e: e.tensor_tensor(out=tm2[:], in0=pk[:], in1=vb["k_k"][:], op=ALU.mult), [pk, vb["k_k"]], [tm2])
        S.op("dve", lambda e: e.tensor_tensor(out=tm1[:], in0=tm2[:], in1=tm2[:], op=ALU.mult), [tm2], [tm1])
        S.op("dve", lambda e: e.tensor_reduce(out=s16[:, 0, :], in_=v3(tm1[:]), axis=AX.X, op=ALU.add), [tm1], [s16])
        S.op("act", lambda e: e.activation(out=s16[:, 1, :], in_=s16[:, 0, :], func=AF.Sqrt), [s16], [s16])
        S.op("dve", lambda e: e.tensor_scalar(out=s16[:, 1, :], in0=s16[:, 1, :], scalar1=1e-12, scalar2=None, op0=ALU.max), [s16], [s16])
        S.op("dve", lambda e: e.reciprocal(out=s16[:, 2, :], in_=s16[:, 1, :]), [s16], [s16])
        S.op("dve", lambda e: e.tensor_tensor(out=v3(tm2[:]), in0=v3(tm2[:]), in1=bc3(s16[:, 2, :]), op=ALU.mult), [tm2, s16], [tm2])
        S.op("dve", lambda e: e.tensor_scalar(out=tA[:], in0=tm2[:], scalar1=-1.0, scalar2=None, op0=ALU.mult), [tm2], [tA])
        S.op("dve", lambda e: e.tensor_tensor(out=tB[:], in0=tm2[:], in1=av[:], op=ALU.mult), [tm2, av], [tB])
        S.op("dve", lambda e: e.tensor_tensor(out=tm1[:], in0=av[:], in1=vb["k_a"][:], op=ALU.mult), [av, vb["k_a"]], [tm1])
        S.op("dve", lambda e: e.tensor_tensor(out=tm1[:], in0=tm1[:], in1=omka[:], op=ALU.add), [tm1, omka], [tm1])
        S.op("dve", lambda e: e.tensor_tensor(out=tK[:], in0=pk[:], in1=tm1[:], op=ALU.mult), [pk, tm1], [tK])
        S.op("dve", lambda e: e.tensor_tensor(out=tm1[:], in0=pr[:], in1=tK[:], op=ALU.mult), [pr, tK], [tm1])
        S.op("dve", lambda e: e.tensor_tensor(out=tm1[:], in0=tm1[:], in1=vb["r_k"][:], op=ALU.mult), [tm1, vb["r_k"]], [tm1])
        S.op("dve", lambda e: e.tensor_reduce(out=s16[:, 3, :], in_=v3(tm1[:]), axis=AX.X, op=ALU.add), [tm1], [s16])
        for h0 in range(0, RH, 8):
            bp = bcp.next()
            for h in range(h0, h0 + 8):
                S.op("pe", lambda e, bp=bp, h=h, h0=h0: e.transpose(out=bp[0:RN, (h - h0) * 128:(h - h0 + 1) * 128], in_=pv[:, h * RN:(h + 1) * RN], identity=ident_f[:]), [pv, ident_f], [bp])
            S.op("act", lambda e, bp=bp, h0=h0: e.activation(out=VT[:, h0:h0 + 8, :].rearrange("p a b -> p (a b)"), in_=bp[0:RN, :], func=AF.Copy), [bp], [VT])
        for t in range(128):
            sel = ident_f[:, t:t + 1].to_broadcast([128, RN])
            bq = {}
            for nm, src in (("A", tA), ("W", tW), ("B", tB), ("K", tK), ("R", pr)):
                bp = bcp.next()
                for half in range(2):
                    S.op("pe", lambda e, bp=bp, src=src, half=half, sel=sel: e.matmul(bp[0:RN, half * 512:(half + 1) * 512], lhsT=sel, rhs=src[:, half * 512:(half + 1) * 512], start=True, stop=True), [ident_f, src], [bp])
                bq[nm] = bp
                if nm == "A":
                    S.op("dve", lambda e, bp=bp: e.tensor_tensor(out=T1[:], in0=St[:], in1=bp[0:RN, :], op=ALU.mult), [St, bp], [T1])
                    S.op("dve", lambda e: e.tensor_reduce(out=sa[:], in_=v3(T1[:]), axis=AX.X, op=ALU.add), [T1], [sa])
                elif nm == "W":
                    S.op("dve", lambda e, bp=bp: e.tensor_tensor(out=St[:], in0=St[:], in1=bp[0:RN, :], op=ALU.mult), [St, bp], [St])
                elif nm == "B":
                    S.op("dve", lambda e, bp=bp: e.tensor_tensor(out=v3(T2[:]), in0=v3(bp[0:RN, :]), in1=bc3(sa[:], RN), op=ALU.mult), [bp, sa], [T2])
                    S.op("dve", lambda e: e.tensor_tensor(out=St[:], in0=St[:], in1=T2[:], op=ALU.add), [St, T2], [St])
                elif nm == "K":
                    kb_, t3_ = kbs.next(), t3s.next()
                    S.op("act", lambda e, bp=bp, kb_=kb_: e.activation(out=kb_[:], in_=bp[0:RN, :], func=AF.Copy), [bp], [kb_])
                    S.op("pool", lambda e, kb_=kb_, t3_=t3_, t=t: e.tensor_tensor(out=v3(t3_[:]), in0=v3(kb_[:]), in1=bc3(VT[:, :, t], RN), op=ALU.mult), [kb_, VT], [t3_])
                    S.op("dve", lambda e, t3_=t3_: e.tensor_tensor(out=St[:], in0=St[:], in1=t3_[:], op=ALU.add), [St, t3_], [St])
                else:
                    S.op("dve", lambda e, bp=bp: e.tensor_tensor(out=T1[:], in0=St[:], in1=bp[0:RN, :], op=ALU.mult), [St, bp], [T1])
                    S.op("dve", lambda e, t=t: e.tensor_reduce(out=YT[:, :, t], in_=v3(T1[:]), axis=AX.X, op=ALU.add), [T1], [YT])
        for h0 in range(0, RH, 8):
            bp = bcp.next()
            for h in range(h0, h0 + 8):
                S.op("pe", lambda e, bp=bp, h=h, h0=h0: e.transpose(out=bp[:, (h - h0) * RN:(h - h0 + 1) * RN], in_=YT[:, h, :], identity=ident_f[0:RN, 0:RN]), [YT, ident_f], [bp])
            S.op("act", lambda e, bp=bp, h0=h0: e.activation(out=tm1[:, h0 * RN:(h0 + 8) * RN], in_=bp[:, 0:8 * RN], func=AF.Copy), [bp], [tm1])
        y = tm1
        S.op("dve", lambda e: e.tensor_reduce(out=s16[:, 0, :], in_=v3(y[:]), axis=AX.X, op=ALU.add), [y], [s16])
        S.op("dve", lambda e: e.tensor_scalar(out=s16[:, 0, :], in0=s16[:, 0, :], scalar1=-1.0 / RN, scalar2=None, op0=ALU.mult), [s16], [s16])
        S.op("dve", lambda e: e.tensor_tensor(out=v3(y[:]), in0=v3(y[:]), in1=bc3(s16[:, 0, :]), op=ALU.add), [y, s16], [y])
        S.op("dve", lambda e: e.tensor_tensor(out=tm2[:], in0=y[:], in1=y[:], op=ALU.mult), [y], [tm2])
        S.op("dve", lambda e: e.tensor_reduce(out=s16[:, 1, :], in_=v3(tm2[:]), axis=AX.X, op=ALU.add), [tm2], [s16])
        S.op("dve", lambda e: e.tensor_scalar(out=s16[:, 1, :], in0=s16[:, 1, :], scalar1=1.0 / RN, scalar2=GN_EPS, op0=ALU.mult, op1=ALU.add), [s16], [s16])
        S.op("act", lambda e: e.activation(out=s16[:, 1, :], in_=s16[:, 1, :], func=AF.Sqrt), [s16], [s16])
        S.op("dve", lambda e: e.reciprocal(out=s16[:, 2, :], in_=s16[:, 1, :]), [s16], [s16])
        S.op("dve", lambda e: e.tensor_tensor(out=v3(y[:]), in0=v3(y[:]), in1=bc3(s16[:, 2, :]), op=ALU.mult), [y, s16], [y])
        S.op("dve", lambda e: e.tensor_tensor(out=y[:], in0=y[:], in1=vb["ln_w"][:], op=ALU.mult), [y, vb["ln_w"]], [y])
        S.op("dve", lambda e: e.tensor_tensor(out=y[:], in0=y[:], in1=vb["ln_b"][:], op=ALU.add), [y, vb["ln_b"]], [y])
        S.op("dve", lambda e: e.tensor_tensor(out=v3(tm2[:]), in0=v3(pv[:]), in1=bc3(s16[:, 3, :]), op=ALU.mult), [pv, s16], [tm2])
        S.op("dve", lambda e: e.tensor_tensor(out=y[:], in0=y[:], in1=tm2[:], op=ALU.add), [y, tm2], [y])
        S.op("dve", lambda e: e.tensor_tensor(out=yb[:], in0=y[:], in1=gv[:], op=ALU.mult), [y, gv], [yb])
        k.transpose_to(yb, lambda c: yb[:, c * 128:(c + 1) * 128], 8, ybT, lambda c0, c1: ybT[:, c0:c1, :])
        for nn in range(4):
            acc = k.acc.next()
            for c in range(8):
                S.op("pe", lambda e, acc=acc, c=c, nn=nn: e.matmul(acc[:], lhsT=ybT[:, c, :], rhs=wo_s[:, c, nn * 512:(nn + 1) * 512], start=(c == 0), stop=(c == 7)), [ybT, wo_s], [acc])
            S.op("act", lambda e, acc=acc, nn=nn: e.activation(out=ob[:, nn * 512:(nn + 1) * 512], in_=acc[:], func=AF.Copy), [acc], [ob])
        S.dma("sp", part[t0:t0 + 128, :], ob[:], [ob], [part], ob)
    S.phase_end()
    S.emit()
    return nc


def proj_phase(k, T, xd, gd, Wd, ncols, Pd, row_off, TH=1024):
    S = k.S
    TH = min(T, TH)
    groups = [(c0, min(256, ncols - c0)) for c0 in range(0, ncols, 256)]
    for th in range(T // TH):
        S.phase_begin()
        g_bc = k.gain_bc("pg", gd, D)
        XT = S.sb("XT", [128, KC, TH], BF16)
        hb = Rot([S.sb("hb", [128, D], F32) for _ in range(2)])
        hn = Rot([S.sb("hn", [128, D], BF16) for _ in range(2)])
        for tl in range(TH // 128):
            t0 = th * TH + tl * 128
            h, o = hb.next(), hn.next()
            S.dma("sp", h[:], xd[t0:t0 + 128, :], [xd], [h], h)
            k.rmsnorm(h, h[:], g_bc, g_bc[:], o, o[:], D)
            k.transpose_to(o, lambda q, o=o: o[:, q * 128:(q + 1) * 128], KC, XT, lambda c0, c1, tl=tl: XT[:, c0:c1, tl * 128:(tl + 1) * 128])
        wgb = Rot([S.sb("wg", [128, KC, 256], BF16) for _ in range(2)])
        stg = Rot([S.sb("stg", [128, 256], F32) for _ in range(3)])
        Wv = Wd.t.rearrange("(kc p) n -> p kc n", p=128)
        for (c0, n) in groups:
            wg = wgb.next()
            S.dma("pool", wg[:, :, 0:n], Wv[:, :, c0:c0 + n], [Wd], [wg], wg)
            for tl in range(TH // 128):
                t0 = th * TH + tl * 128
                acc = k.acc.next()
                for kc in range(KC):
                    S.op("pe", lambda e, acc=acc, kc=kc, wg=wg, tl=tl, n=n: e.matmul(acc[:, 0:n], lhsT=XT[:, kc, tl * 128:(tl + 1) * 128], rhs=wg[:, kc, 0:n], start=(kc == 0), stop=(kc == KC - 1)), [XT, wg], [acc])
                st = stg.next()
                S.op("act", lambda e, acc=acc, st=st, n=n: e.activation(out=st[:, 0:n], in_=acc[:, 0:n], func=AF.Copy), [acc], [st])
                S.dma("sp", Pd[row_off + t0:row_off + t0 + 128, c0:c0 + n], st[:, 0:n], [st], [Pd], st)
        S.phase_end()


SH, SP_, SN, SG = 16, 64, 128, 2
SC = SH * SP_
NCOL_SSM = 2 * SC + 2 * SG * SN + SH


def build_ssm(T):
    nc = bass.Bass("TRN2", target_bir_lowering=False)
    k = K(nc, n_tp=1, n_acc=2)
    S = k.S
    NT = T // 128
    di = lambda n, s, dt=F32: S.dram(n, s, dt, kind="ExternalInput")
    xb = di("xb", [T, D])
    g_n = di("g_n", [D])
    w_in = di("w_in", [D, NCOL_SSM])
    cw = di("cw", [4, 1536])
    cb = di("cb", [1536])
    dtb, alog, dsk = di("dtb", [SH]), di("alog", [SH]), di("dsk", [SH])
    nw = di("nw", [SC])
    w_o = di("w_o", [SC, D])
    tri_d, neg_d, ones_d = di("c_tri", [128, 128]), di("c_neg", [128, 128]), di("c_ones", [128, 128])
    part = S.dram("part", [T, D], BF16, kind="ExternalOutput")
    P = S.dram("P", [T + 3, NCOL_SSM], F32)

    zrow = S.sb("zrow", [3, NCOL_SSM], F32)
    S.op("pool", lambda e: e.memset(zrow[:], 0.0), [], [zrow])
    S.dma("sp", P[0:3, :], zrow[:], [zrow], [P], zrow)
    proj_phase(k, T, xb, g_n, w_in, NCOL_SSM, P, 3)

    S.phase_begin()
    ident_f = S.sb("ident_f", [128, 128], F32)
    tri, neg, ones = S.sb("tri", [128, 128], F32), S.sb("neg", [128, 128], F32), S.sb("ones", [128, 128], F32)
    for b_, d_ in ((ident_f, k.ident_d), (tri, tri_d), (neg, neg_d), (ones, ones_d)):
        S.dma("sp", b_[:], d_[:], [d_], [b_], b_)
    cwb = [k.gain_bc(f"cw{i}", cw.t[i], 1536) for i in range(4)]
    cbb = k.gain_bc("cbb", cb, 1536)
    nwb = k.gain_bc("nwb", nw, SC)
    dtbb, ab, dskb = k.gain_bc("dtbb", dtb, SH), k.gain_bc("ab", alog, SH), k.gain_bc("dskb", dsk, SH)
    S.op("act", lambda e: e.activation(out=ab[:], in_=ab[:], func=AF.Exp), [ab], [ab])
    S.op("dve", lambda e: e.tensor_scalar(out=ab[:], in0=ab[:], scalar1=-1.0, scalar2=None, op0=ALU.mult), [ab], [ab])
    wo_s = S.sb("wo_s", [128, 8, D], BF16)
    S.dma("pool", wo_s[:], w_o.t.rearrange("(c p) n -> p c n", p=128), [w_o], [wo_s], wo_s)
    dps = Rot([S.ps("dps", [128, 512], F32) for _ in range(2)])
    ydp, yop, stp = S.ps("ydp", [128, 512], F32), S.ps("yop", [128, 512], F32), S.ps("stp", [128, 512], F32)
    z = S.sb("z", [128, SC], F32)
    xk = [S.sb(f"xk{i}", [128, 1536], F32) for i in range(4)]
    tmpa, tmpb = S.sb("tmpa", [128, 1536], F32), S.sb("tmpb", [128, 1536], F32)
    cv = S.sb("cv", [128, 1536], F32)
    xa = S.sb("xa", [128, 1536], F32)
    s16 = S.sb("s16", [128, 12, SH], F32)
    cs = S.sb("cs", [128, 2 * SH], F32)
    xdt_b, xst_b = S.sb("xdt_b", [128, SC], BF16), S.sb("xst_b", [128, SC], BF16)
    bcb = S.sb("bcb", [128, 512], BF16)
    BCT = S.sb("BCT", [128, 4, 128], BF16)
    CBT = S.sb("CBT", [128, SG, 128], F32)
    DT = S.sb("DT", [128, SH, 128], F32)
    MT = S.sb("MT", [128, SH, 128], BF16)
    ysb, y = S.sb("ysb", [128, SC], F32), S.sb("y", [128, SC], F32)
    hs = S.sb("hs", [128, SG, 512], F32)
    hbf = S.sb("hbf", [128, SG, 512], BF16)
    S.op("pool", lambda e: e.memset(hs[:], 0.0), [], [hs])
    S.op("pool", lambda e: e.memset(hbf[:], 0.0), [], [hbf])
    yb = S.sb("yb", [128, SC], BF16)
    ybT = S.sb("ybT", [128, 8, 128], BF16)
    ob = S.sb("ob", [128, D], BF16)
    v3 = lambda ap: ap.rearrange("p (h j) -> p h j", j=SP_)
    bc3 = lambda ap, nh=SH: ap.unsqueeze(2).to_broadcast([128, nh, SP_])
    DTI, ADT, C_, NEGC, ECL, TOT, CD, DTE, DD = range(9)

    for tt in range(NT):
        t0 = tt * 128
        S.dma("sp", z[:], P[3 + t0:3 + t0 + 128, 0:SC], [P], [z], z)
        for i in range(4):
            S.dma("sp", xk[i][:], P[t0 + i:t0 + i + 128, SC:SC + 1536], [P], [xk[i]], xk[i])
        S.dma("sp", s16[:, DTI, :], P[3 + t0:3 + t0 + 128, SC + 1536:SC + 1536 + SH], [P], [s16], s16)
        S.op("pool", lambda e: e.tensor_tensor(out=cv[:], in0=xk[0][:], in1=cwb[0][:], op=ALU.mult), [xk[0], cwb[0]], [cv])
        S.op("pool", lambda e: e.tensor_tensor(out=tmpa[:], in0=xk[1][:], in1=cwb[1][:], op=ALU.mult), [xk[1], cwb[1]], [tmpa])
        S.op("dve", lambda e: e.tensor_tensor(out=cv[:], in0=cv[:], in1=tmpa[:], op=ALU.add), [cv, tmpa], [cv])
        S.op("pool", lambda e: e.tensor_tensor(out=tmpb[:], in0=xk[2][:], in1=cwb[2][:], op=ALU.mult), [xk[2], cwb[2]], [tmpb])
        S.op("dve", lambda e: e.tensor_tensor(out=cv[:], in0=cv[:], in1=tmpb[:], op=ALU.add), [cv, tmpb], [cv])
        S.op("pool", lambda e: e.tensor_tensor(out=tmpa[:], in0=xk[3][:], in1=cwb[3][:], op=ALU.mult), [xk[3], cwb[3]], [tmpa])
        S.op("dve", lambda e: e.tensor_tensor(out=cv[:], in0=cv[:], in1=tmpa[:], op=ALU.add), [cv, tmpa], [cv])
        S.op("dve", lambda e: e.tensor_tensor(out=cv[:], in0=cv[:], in1=cbb[:], op=ALU.add), [cv, cbb], [cv])
        S.op("act", lambda e: e.activation(out=xa[:], in_=cv[:], func=AF.Silu), [cv], [xa])
        S.op("dve", lambda e: e.tensor_tensor(out=s16[:, DTI, :], in0=s16[:, DTI, :], in1=dtbb[:], op=ALU.add), [s16, dtbb], [s16])
        S.op("act", lambda e: e.activation(out=s16[:, DTI, :], in_=s16[:, DTI, :], func=AF.Exp), [s16], [s16])
        S.op("act", lambda e: e.activation(out=s16[:, DTI, :], in_=s16[:, DTI, :], func=AF.Ln, bias=1.0), [s16], [s16])
        S.op("dve", lambda e: e.tensor_tensor(out=s16[:, ADT, :], in0=s16[:, DTI, :], in1=ab[:], op=ALU.mult), [s16, ab], [s16])
        acc = k.acc.next()
        S.op("pe", lambda e, acc=acc: e.matmul(acc[:, 0:SH], lhsT=tri[:], rhs=s16[:, ADT, :], start=True, stop=True), [tri, s16], [acc])
        S.op("pe", lambda e, acc=acc: e.matmul(acc[:, SH:2 * SH], lhsT=ones[:], rhs=s16[:, ADT, :], start=True, stop=True), [ones, s16], [acc])
        S.op("dve", lambda e, acc=acc: e.tensor_copy(out=cs[:], in_=acc[:, 0:2 * SH]), [acc], [cs])
        S.op("dve", lambda e: e.tensor_scalar(out=s16[:, NEGC, :], in0=cs[:, 0:SH], scalar1=-1.0, scalar2=None, op0=ALU.mult), [cs], [s16])
        S.op("act", lambda e: e.activation(out=s16[:, ECL, :], in_=cs[:, 0:SH], func=AF.Exp), [cs], [s16])
        S.op("act", lambda e: e.activation(out=s16[:, CD, :], in_=cs[:, SH:2 * SH], func=AF.Exp), [cs], [s16])
        S.op("dve", lambda e: e.tensor_tensor(out=s16[:, TOT, :], in0=cs[:, SH:2 * SH], in1=cs[:, 0:SH], op=ALU.subtract), [cs], [s16])
        S.op("act", lambda e: e.activation(out=s16[:, DTE, :], in_=s16[:, TOT, :], func=AF.Exp), [s16], [s16])
        S.op("dve", lambda e: e.tensor_tensor(out=s16[:, DD, :], in0=s16[:, DTE, :], in1=s16[:, DTI, :], op=ALU.mult), [s16], [s16])
        S.op("dve", lambda e: e.tensor_tensor(out=v3(xdt_b[:]), in0=v3(xa[:, 0:SC]), in1=bc3(s16[:, DTI, :]), op=ALU.mult), [xa, s16], [xdt_b])
        S.op("dve", lambda e: e.tensor_tensor(out=v3(xst_b[:]), in0=v3(xa[:, 0:SC]), in1=bc3(s16[:, DD, :]), op=ALU.mult), [xa, s16], [xst_b])
        S.op("act", lambda e: e.activation(out=bcb[:], in_=xa[:, SC:SC + 512], func=AF.Copy), [xa], [bcb])
        k.transpose_to(bcb, lambda c: bcb[:, c * 128:(c + 1) * 128], 4, BCT, lambda c0, c1: BCT[:, c0:c1, :])
        acc = k.acc.next()
        for g in range(SG):
            S.op("pe", lambda e, acc=acc, g=g: e.matmul(acc[:, g * 128:(g + 1) * 128], lhsT=BCT[:, g, :], rhs=BCT[:, 2 + g, :], start=True, stop=True), [BCT], [acc])
        S.op("act", lambda e, acc=acc: e.activation(out=CBT[:].rearrange("p a b -> p (a b)"), in_=acc[:, 0:256], func=AF.Copy), [acc], [CBT])
        for hq in range(4):
            dp = dps.next()
            for j in range(4):
                h = hq * 4 + j
                S.op("pe", lambda e, dp=dp, j=j, h=h: e.matmul(dp[:, j * 128:(j + 1) * 128], lhsT=cs[:, h:h + 1].to_broadcast([128, 128]), rhs=ident_f[:], start=True, stop=False), [cs, ident_f], [dp])
                S.op("pe", lambda e, dp=dp, j=j: e.matmul(dp[:, j * 128:(j + 1) * 128], lhsT=ident_f[:], rhs=neg[:], start=False, stop=True), [ident_f, neg], [dp])
            for j in range(4):
                h = hq * 4 + j
                S.op("act", lambda e, dp=dp, j=j, h=h: e.activation(out=DT[:, h, :], in_=dp[:, j * 128:(j + 1) * 128], func=AF.Exp, bias=s16[:, NEGC, h:h + 1]), [dp, s16], [DT])
        for g in range(SG):
            S.op("dve", lambda e, g=g: e.tensor_tensor(out=MT[:, g * 8:(g + 1) * 8, :], in0=DT[:, g * 8:(g + 1) * 8, :], in1=CBT[:, g, :].unsqueeze(1).to_broadcast([128, 8, 128]), op=ALU.mult), [DT, CBT], [MT])
        for g in range(SG):
            for j in range(8):
                h = g * 8 + j
                S.op("pe", lambda e, h=h, j=j: e.matmul(ydp[:, j * 64:(j + 1) * 64], lhsT=MT[:, h, :], rhs=xdt_b[:, h * 64:(h + 1) * 64], start=True, stop=True), [MT, xdt_b], [ydp])
            S.op("pe", lambda e, g=g: e.matmul(yop[:], lhsT=BCT[:, 2 + g, :], rhs=hbf[:, g, :], start=True, stop=True), [BCT, hbf], [yop])
            S.op("act", lambda e, g=g: e.activation(out=ysb[:, g * 512:(g + 1) * 512], in_=ydp[:], func=AF.Copy), [ydp], [ysb])
            S.op("dve", lambda e, g=g: e.tensor_tensor(out=v3(y[:, g * 512:(g + 1) * 512]), in0=v3(yop[:]), in1=bc3(s16[:, ECL, g * 8:(g + 1) * 8], 8), op=ALU.mult), [yop, s16], [y])
            S.op("pe", lambda e, g=g: e.matmul(stp[:], lhsT=bcb[:, g * 128:(g + 1) * 128], rhs=xst_b[:, g * 512:(g + 1) * 512], start=True, stop=True), [bcb, xst_b], [stp])
            S.op("dve", lambda e, g=g: e.tensor_tensor(out=v3(hs[:, g, :]), in0=v3(hs[:, g, :]), in1=bc3(s16[:, CD, g * 8:(g + 1) * 8], 8), op=ALU.mult), [hs, s16], [hs])
            S.op("dve", lambda e, g=g: e.tensor_tensor(out=hs[:, g, :], in0=hs[:, g, :], in1=stp[:], op=ALU.add), [hs, stp], [hs])
            S.op("act", lambda e, g=g: e.activation(out=hbf[:, g, :], in_=hs[:, g, :], func=AF.Copy), [hs], [hbf])
        S.op("dve", lambda e: e.tensor_tensor(out=y[:], in0=y[:], in1=ysb[:], op=ALU.add), [y, ysb], [y])
        S.op("dve", lambda e: e.tensor_tensor(out=v3(ysb[:]), in0=v3(xa[:, 0:SC]), in1=bc3(dskb[:]), op=ALU.mult), [xa, dskb], [ysb])
        S.op("dve", lambda e: e.tensor_tensor(out=y[:], in0=y[:], in1=ysb[:], op=ALU.add), [y, ysb], [y])
        S.op("act", lambda e: e.activation(out=ysb[:], in_=z[:], func=AF.Silu), [z], [ysb])
        S.op("dve", lambda e: e.tensor_tensor(out=y[:], in0=y[:], in1=ysb[:], op=ALU.mult), [y, ysb], [y])
        for g in range(SG):
            k.rmsnorm(y, y[:, g * 512:(g + 1) * 512], nwb, nwb[:, g * 512:(g + 1) * 512], yb, yb[:, g * 512:(g + 1) * 512], 512)
        k.transpose_to(yb, lambda c: yb[:, c * 128:(c + 1) * 128], 8, ybT, lambda c0, c1: ybT[:, c0:c1, :])
        for nn in range(4):
            acc = k.acc.next()
            for c in range(8):
                S.op("pe", lambda e, acc=acc, c=c, nn=nn: e.matmul(acc[:], lhsT=ybT[:, c, :], rhs=wo_s[:, c, nn * 512:(nn + 1) * 512], start=(c == 0), stop=(c == 7)), [ybT, wo_s], [acc])
            S.op("act", lambda e, acc=acc, nn=nn: e.activation(out=ob[:, nn * 512:(nn + 1) * 512], in_=acc[:], func=AF.Copy), [acc], [ob])
        S.dma("sp", part[t0:t0 + 128, :], ob[:], [ob], [part], ob)
    S.phase_end()
    S.emit()
    return nc


def _ssm_consts():
    s = np.arange(128)[:, None]
    l = np.arange(128)[None, :]
    return dict(c_tri=(s <= l).astype(np.float32), c_neg=np.where(s > l, -30000.0, 0.0).astype(np.float32), c_ones=np.ones((128, 128), np.float32))


def _ssm_cols(hh):
    z = np.arange(hh * 1024, hh * 1024 + 1024)
    xc = 2048 + np.arange(hh * 1024, hh * 1024 + 1024)
    Bc = 2048 + 2048 + np.arange(2 * hh * 128, 2 * hh * 128 + 256)
    Cc = 2048 + 2048 + 512 + np.arange(2 * hh * 128, 2 * hh * 128 + 256)
    dtc = 2048 + 3072 + np.arange(hh * 16, hh * 16 + 16)
    conv_rows = np.concatenate([xc, Bc, Cc]) - 2048
    return np.concatenate([z, xc, Bc, Cc, dtc]), conv_rows


def _ssm_inputs(inp, xb, hh):
    f32 = lambda a: np.ascontiguousarray(np.asarray(a), dtype=np.float32)
    cols, crow = _ssm_cols(hh)
    m = dict(c_ident=_ident(), xb=xb, g_n=f32(inp["norm_mix"][0]), w_in=f32(np.asarray(inp["ev_w_in"][0])[:, cols]),
             cw=f32(np.asarray(inp["ev_conv_w"][0])[crow].T), cb=f32(np.asarray(inp["ev_conv_b"][0])[crow]),
             dtb=f32(np.asarray(inp["ev_dt_bias"][0])[hh * 16:(hh + 1) * 16]), alog=f32(np.asarray(inp["ev_a_log"][0])[hh * 16:(hh + 1) * 16]),
             dsk=f32(np.asarray(inp["ev_d_skip"][0])[hh * 16:(hh + 1) * 16]), nw=f32(np.asarray(inp["ev_ssm_norm"][0])[hh * 1024:(hh + 1) * 1024]),
             w_o=f32(np.asarray(inp["ev_w_out"][0])[hh * 1024:(hh + 1) * 1024]))
    m.update(_ssm_consts())
    return m


NG, NR, HD = 2, 4, 128
NQH = NG * NR
NCOL_NSA = NQH * HD + 6 * NG * HD + NQH * 3
OQ, OKC, OVC, OKS, OVS, OKW, OVW, OGL = 0, 1024, 1280, 1536, 1792, 2048, 2304, 2560
VW_ = 132
CW_ = 196


def build_nsa(T, debug=False):
    nc = bass.Bass("TRN2", target_bir_lowering=False)
    k = K(nc, n_tp=1, n_acc=1)
    k.debug = debug
    S = k.S
    NT = T // 128
    NCMP = (T - 32) // 16 + 1
    NIT = (NCMP + 127) // 128
    di = lambda n, s, dt=F32: S.dram(n, s, dt, kind="ExternalInput")
    xb = di("xb", [T, D])
    g_n = di("g_n", [D])
    w_in = di("w_in", [D, NCOL_NSA])
    pos = di("pos", [T, 1], I32)
    invf = di("c_invf", [16])
    qg, kcg, ksg, kwg = di("qg", [HD]), di("kcg", [HD]), di("ksg", [HD]), di("kwg", [HD])
    pe_k, pe_v = di("pe_k", [32, HD]), di("pe_v", [32, HD])
    wk1, wv1 = di("wk1", [32 * HD, 256]), di("wv1", [32 * HD, 256])
    wk2, wv2 = di("wk2", [256, HD]), di("wv2", [256, HD])
    w_o = di("w_o", [NQH * HD, D])
    ov_d = di("c_ov", [NIT * 128, 64])
    efull_d = di("c_efull", [64, T])
    cmask_d = di("c_cmask", [16, 128, 128])
    diag_d = di("c_diag", [128, 128])
    fadd_d = di("c_fadd", [NT, 128, 64])
    part = S.dram("part", [T, D], BF16, kind="ExternalOutput")
    P = S.dram("P", [T, NCOL_NSA], F32)

    KsT = S.sb("KsT", [128, NG, T], BF16)
    KwT = S.sb("KwT", [128, NG, T], BF16)
    Vs = S.sb("Vs", [128, NT, NG, VW_], BF16)
    Vw = S.sb("Vw", [128, NT, NG, VW_], BF16)
    KcmpT = S.sb("KcmpT", [128, NG, NIT * 128], BF16)
    Vcmp = S.sb("Vcmp", [128, NG, NIT, CW_], BF16)
    cosb = S.sb("cosb", [128, NT, 16], F32)
    sinb = S.sb("sinb", [128, NT, 16], F32)
    S.op("pool", lambda e: e.memset(Vs[:], 1.0), [], [Vs])
    S.op("pool", lambda e: e.memset(Vw[:], 1.0), [], [Vw])
    S.op("pool", lambda e: e.memset(Vcmp[:], 0.0), [], [Vcmp])
    S.op("pool", lambda e: e.memset(KcmpT[:], 0.0), [], [KcmpT])

    proj_phase(k, T, xb, g_n, w_in, NCOL_NSA, P, 0)

    def rope(buf, ap3, nh, tt, t1, t2, t3, t4):
        cb_ = cosb[:, tt, :].unsqueeze(1).to_broadcast([128, nh, 16])
        sb_ = sinb[:, tt, :].unsqueeze(1).to_broadcast([128, nh, 16])
        x1, x2 = ap3[:, :, 0:16], ap3[:, :, 16:32]
        v = lambda b: b[:, 0:nh * 16].rearrange("p (h i) -> p h i", i=16)
        S.op("dve", lambda e: e.tensor_tensor(out=v(t1), in0=x1, in1=cb_, op=ALU.mult), [buf, cosb], [t1])
        S.op("dve", lambda e: e.tensor_tensor(out=v(t2), in0=x2, in1=sb_, op=ALU.mult), [buf, sinb], [t2])
        S.op("dve", lambda e: e.tensor_tensor(out=v(t3), in0=x2, in1=cb_, op=ALU.mult), [buf, cosb], [t3])
        S.op("dve", lambda e: e.tensor_tensor(out=v(t4), in0=x1, in1=sb_, op=ALU.mult), [buf, sinb], [t4])
        S.op("dve", lambda e: e.tensor_tensor(out=x1, in0=v(t1), in1=v(t2), op=ALU.subtract), [t1, t2], [buf])
        S.op("dve", lambda e: e.tensor_tensor(out=x2, in0=v(t3), in1=v(t4), op=ALU.add), [t3, t4], [buf])

    S.phase_begin()
    gb = {n: k.gain_bc("g_" + n, d_, HD) for n, d_ in (("kc", kcg), ("ks", ksg), ("kw", kwg))}
    invf_bc = k.gain_bc("invf", invf, 16)
    KcT = S.sb("KcT", [128, NG, T], BF16)
    VcT = S.sb("VcT", [128, NG, T], BF16)
    w1s = {"k": S.sb("w1k", [128, 32, 256], BF16), "v": S.sb("w1v", [128, 32, 256], BF16)}
    w2s = {"k": S.sb("w2k", [128, 2, HD], BF16), "v": S.sb("w2v", [128, 2, HD], BF16)}
    peT = {"k": S.sb("peTk", [128, 32], BF16), "v": S.sb("peTv", [128, 32], BF16)}
    for nm, w1d, w2d, ped in (("k", wk1, wk2, pe_k), ("v", wv1, wv2, pe_v)):
        S.dma("pool", w1s[nm][:], w1d.t.rearrange("(j d) n -> d j n", d=HD), [w1d], [w1s[nm]], w1s[nm])
        S.dma("pool", w2s[nm][:], w2d.t.rearrange("(c p) n -> p c n", p=128), [w2d], [w2s[nm]], w2s[nm])
        S.dma("pool", peT[nm][:], ped.t.rearrange("j d -> d j"), [ped], [peT[nm]], peT[nm], allow_slow_non_contiguous=True)
    ov_s = S.sb("ov_s", [128, NIT, 64], BF16)
    S.dma("pool", ov_s[:], ov_d.t.rearrange("(c p) n -> p c n", p=128), [ov_d], [ov_s], ov_s)
    kvl = S.sb("kvl", [128, 6 * NG * HD], F32)
    kvn = S.sb("kvn", [128, 4 * NG * HD], BF16)
    pos_i = S.sb("pos_i", [128, 1], I32)
    ang = S.sb("ang", [128, 16], F32)
    angi = S.sb("angi", [128, 16], I32)
    rt = [S.sb(f"rt{i}", [128, 128], F32) for i in range(4)]
    kT4 = S.sb("kT4", [128, 8, 128], BF16)
    for tt in range(NT):
        t0 = tt * 128
        S.dma("sp", kvl[:], P[t0:t0 + 128, OKC:OKC + 6 * NG * HD], [P], [kvl], kvl)
        S.dma("sp", pos_i[:], pos[t0:t0 + 128, :], [pos], [pos_i], pos_i)
        S.op("dve", lambda e: e.tensor_copy(out=ang[:, 0:1], in_=pos_i[:]), [pos_i], [ang])
        S.op("dve", lambda e: e.tensor_scalar(out=ang[:], in0=invf_bc[:], scalar1=ang[:, 0:1], scalar2=None, op0=ALU.mult), [ang, invf_bc], [ang])
        for ri, shift in ((0, 0.0), (1, 0.5 * math.pi)):
            r_ = rt[ri]
            S.op("dve", lambda e, r_=r_, shift=shift: e.tensor_scalar(out=r_[:, 0:16], in0=ang[:], scalar1=shift, scalar2=None, op0=ALU.add), [ang], [r_])
            S.op("dve", lambda e, r_=r_: e.tensor_scalar(out=r_[:, 16:32], in0=r_[:, 0:16], scalar1=1.0 / (2 * math.pi), scalar2=None, op0=ALU.mult), [r_], [r_])
            S.op("dve", lambda e, r_=r_: e.tensor_scalar(out=r_[:, 16:32], in0=r_[:, 16:32], scalar1=12582912.0, scalar2=None, op0=ALU.add), [r_], [r_])
            S.op("dve", lambda e, r_=r_: e.tensor_scalar(out=r_[:, 16:32], in0=r_[:, 16:32], scalar1=-12582912.0, scalar2=None, op0=ALU.add), [r_], [r_])
            S.op("dve", lambda e, r_=r_: e.scalar_tensor_tensor(out=r_[:, 0:16], in0=r_[:, 16:32], scalar=-2 * math.pi, in1=r_[:, 0:16], op0=ALU.mult, op1=ALU.add), [r_], [r_])
            S.op("dve", lambda e, r_=r_: e.tensor_scalar(out=r_[:, 16:32], in0=r_[:, 0:16], scalar1=math.pi, scalar2=None, op0=ALU.is_gt), [r_], [r_])
            S.op("dve", lambda e, r_=r_: e.scalar_tensor_tensor(out=r_[:, 0:16], in0=r_[:, 16:32], scalar=-2 * math.pi, in1=r_[:, 0:16], op0=ALU.mult, op1=ALU.add), [r_], [r_])
            S.op("dve", lambda e, r_=r_: e.tensor_scalar(out=r_[:, 16:32], in0=r_[:, 0:16], scalar1=-math.pi, scalar2=None, op0=ALU.is_lt), [r_], [r_])
            S.op("dve", lambda e, r_=r_: e.scalar_tensor_tensor(out=r_[:, 0:16], in0=r_[:, 16:32], scalar=2 * math.pi, in1=r_[:, 0:16], op0=ALU.mult, op1=ALU.add), [r_], [r_])
        if tt == NT - 1 and debug:
            dbgr = S.sb("dbgr", [128, 64], F32)
            S.op("dve", lambda e: e.tensor_copy(out=dbgr[:, 0:32], in_=rt[0][:, 0:32]), [rt[0]], [dbgr])
            S.op("dve", lambda e: e.tensor_copy(out=dbgr[:, 32:64], in_=rt[1][:, 0:32]), [rt[1]], [dbgr])
        S.op("act", lambda e, tt=tt: e.activation(out=sinb[:, tt, :], in_=rt[0][:, 0:16], func=AF.Sin), [rt[0]], [sinb])
        S.op("act", lambda e, tt=tt: e.activation(out=cosb[:, tt, :], in_=rt[1][:, 0:16], func=AF.Sin), [rt[1]], [cosb])
        for gi in range(NG):
            for (src_off, gname) in ((2 * NG * HD, "ks"), (4 * NG * HD, "kw")):
                ap = kvl[:, src_off + gi * HD:src_off + (gi + 1) * HD]
                st = k.rms_rstd(kvl, ap, HD)
                S.op("dve", lambda e, ap=ap, st=st, gname=gname: e.scalar_tensor_tensor(out=ap, in0=ap, scalar=st[:, 2:3], in1=gb[gname][:], op0=ALU.mult, op1=ALU.mult), [kvl, st, gb[gname]], [kvl])
        for src_off in (2 * NG * HD, 4 * NG * HD):
            rope(kvl, kvl[:, src_off:src_off + NG * HD].rearrange("p (h d) -> p h d", d=HD), NG, tt, *rt)
        S.op("act", lambda e: e.activation(out=kvn[:, 0:256], in_=kvl[:, 2 * NG * HD:3 * NG * HD], func=AF.Copy), [kvl], [kvn])
        S.op("act", lambda e: e.activation(out=kvn[:, 256:512], in_=kvl[:, 4 * NG * HD:5 * NG * HD], func=AF.Copy), [kvl], [kvn])
        S.op("act", lambda e: e.activation(out=kvn[:, 512:1024], in_=kvl[:, 0:2 * NG * HD], func=AF.Copy), [kvl], [kvn])
        k.transpose_to(kvn, lambda c: kvn[:, c * 128:(c + 1) * 128], 8, kT4, lambda c0, c1: kT4[:, c0:c1, :])
        for gi in range(NG):
            S.op("dve", lambda e, gi=gi, t0=t0: e.tensor_copy(out=KsT[:, gi, t0:t0 + 128], in_=kT4[:, gi, :]), [kT4], [KsT])
            S.op("dve", lambda e, gi=gi, t0=t0: e.tensor_copy(out=KwT[:, gi, t0:t0 + 128], in_=kT4[:, 2 + gi, :]), [kT4], [KwT])
            S.op("pool", lambda e, gi=gi, t0=t0: e.tensor_copy(out=KcT[:, gi, t0:t0 + 128], in_=kT4[:, 4 + gi, :]), [kT4], [KcT])
            S.op("pool", lambda e, gi=gi, t0=t0: e.tensor_copy(out=VcT[:, gi, t0:t0 + 128], in_=kT4[:, 6 + gi, :]), [kT4], [VcT])
        S.op("act", lambda e, tt=tt: e.activation(out=Vs[:, tt, :, 0:HD], in_=kvl[:, 3 * NG * HD:4 * NG * HD].rearrange("p (g d) -> p g d", d=HD), func=AF.Copy), [kvl], [Vs])
        S.op("act", lambda e, tt=tt: e.activation(out=Vw[:, tt, :, 0:HD], in_=kvl[:, 5 * NG * HD:6 * NG * HD].rearrange("p (g d) -> p g d", d=HD), func=AF.Copy), [kvl], [Vw])
    hT = S.sb("hT", [128, 2, NIT * 128], BF16)
    cbias = S.sb("cbias", [128, 2], F32)
    cm_f = S.sb("cm_f", [128, HD], F32)
    cm_b = S.sb("cm_b", [128, HD], BF16)
    for nm, UT in (("k", KcT), ("v", VcT)):
        for hc in range(2):
            acc = k.acc.next()
            for j in range(32):
                S.op("pe", lambda e, acc=acc, j=j, hc=hc, nm=nm: e.matmul(acc[:, 0:1], lhsT=w1s[nm][:, j, hc * 128:(hc + 1) * 128], rhs=peT[nm][:, j:j + 1], start=(j == 0), stop=(j == 31)), [w1s[nm], peT[nm]], [acc])
            S.op("dve", lambda e, acc=acc, hc=hc: e.tensor_copy(out=cbias[:, hc:hc + 1], in_=acc[:, 0:1]), [acc], [cbias])
        for gi in range(NG):
            for hc in range(2):
                acc = k.acc.next()
                for j in range(32):
                    S.op("pe", lambda e, acc=acc, j=j, hc=hc, nm=nm, gi=gi, UT=UT: e.matmul(acc[:, 0:NCMP], lhsT=w1s[nm][:, j, hc * 128:(hc + 1) * 128], rhs=UT[:, gi, j:j + 16 * (NCMP - 1) + 1:16], start=(j == 0), stop=(j == 31)), [w1s[nm], UT], [acc])
                S.op("act", lambda e, acc=acc, hc=hc: e.activation(out=hT[:, hc, 0:NCMP], in_=acc[:, 0:NCMP], func=AF.Silu, bias=cbias[:, hc:hc + 1]), [acc, cbias], [hT])
            for it in range(NIT):
                ni = min(128, NCMP - it * 128)
                acc = k.acc.next()
                for hc in range(2):
                    S.op("pe", lambda e, acc=acc, hc=hc, it=it, ni=ni, nm=nm: e.matmul(acc[0:ni, 0:HD], lhsT=hT[:, hc, it * 128:it * 128 + ni], rhs=w2s[nm][:, hc, :], start=(hc == 0), stop=(hc == 1)), [hT, w2s[nm]], [acc])
                if nm == "k":
                    S.op("pool", lambda e: e.memset(cm_f[:], 0.0), [], [cm_f])
                    S.op("act", lambda e, acc=acc, ni=ni: e.activation(out=cm_f[0:ni, :], in_=acc[0:ni, 0:HD], func=AF.Copy), [acc], [cm_f])
                    k.rmsnorm(cm_f, cm_f[:], gb["kc"], gb["kc"][:], cm_b, cm_b[:], HD)
                    k.transpose_to(cm_b, lambda c: cm_b[:], 1, KcmpT, lambda c0, c1, gi=gi, it=it: KcmpT[:, gi, it * 128:(it + 1) * 128])
                else:
                    S.op("act", lambda e, acc=acc, ni=ni, gi=gi, it=it: e.activation(out=Vcmp[0:ni, gi, it, 0:HD], in_=acc[0:ni, 0:HD], func=AF.Copy), [acc], [Vcmp])
                    S.op("dve", lambda e, ni=ni, gi=gi, it=it: e.tensor_copy(out=Vcmp[0:ni, gi, it, HD:HD + 64], in_=ov_s[0:ni, it, :]), [ov_s], [Vcmp])
                    S.op("pool", lambda e, ni=ni, gi=gi, it=it: e.memset(Vcmp[0:ni, gi, it, HD + 64:HD + 65], 1.0), [], [Vcmp])
    k.dump("ang", ang, ang[:], [128, 16])
    if debug:
        k.dump("rt0", dbgr, dbgr[:], [128, 64])
    k.dump("posi", pos_i, pos_i[:], [128, 1], I32)
    k.dump("invf", invf_bc, invf_bc[:], [128, 16])
    k.dump("cos", cosb, cosb[:], [128, NT, 16])
    k.dump("sin", sinb, sinb[:], [128, NT, 16])
    k.dump("KsT", KsT, KsT[:], [128, NG, T], BF16)
    k.dump("KwT", KwT, KwT[:], [128, NG, T], BF16)
    k.dump("KcmpT", KcmpT, KcmpT[:], [128, NG, NIT * 128], BF16)
    k.dump("Vcmp", Vcmp, Vcmp[:], [128, NG, NIT, CW_], BF16)
    S.phase_end()

    S.phase_begin()
    qg_bc = k.gain_bc("g_q", qg, HD)
    wo_s = S.sb("wo_s", [128, NQH, D], BF16)
    S.dma("pool", wo_s[:], w_o.t.rearrange("(c p) n -> p c n", p=128), [w_o], [wo_s], wo_s)
    efull = S.sb("efull", [64, T], BF16)
    S.dma("pool", efull[:], efull_d[:, :], [efull_d], [efull], efull)
    cmask = S.sb("cmask", [128, 16, 128], BF16)
    S.dma("pool", cmask[:], cmask_d.t.rearrange("c p q -> p c q"), [cmask_d], [cmask], cmask)
    diag = S.sb("diag", [128, 128], BF16)
    sup = S.sb("sup", [128, 128], BF16)
    S.dma("pool", diag[:], diag_d[:, :], [diag_d], [diag], diag)
    S.op("dve", lambda e: e.tensor_scalar(out=sup[:], in0=diag[:], scalar1=-1.0, scalar2=1.0, op0=ALU.mult, op1=ALU.add), [diag], [sup])
    fadd = S.sb("fadd", [128, NT, 64], F32)
    S.dma("sp", fadd[:], fadd_d.t.rearrange("t p j -> p t j"), [fadd_d], [fadd], fadd)
    stp_ = Rot([S.ps("sT", [128, 512], F32) for _ in range(1)])
    mxp = S.ps("mxp", [128, 128], F32)
    oacc = [S.ps("oacc", [128, 512], F32) for _ in range(4)]
    ql = S.sb("ql", [128, NQH * HD], F32)
    gl = S.sb("gl", [128, NQH * 3], F32)
    qn = S.sb("qn", [128, NQH * HD], BF16)
    QT = S.sb("QT", [128, NQH, 128], BF16)
    rq = [S.sb(f"rq{i}", [128, 128], F32) for i in range(4)]
    Eb = Rot([S.sb("Eb", [128, 512], F32) for _ in range(2)])
    Pb = Rot([S.sb("Pb", [128, 512], BF16) for _ in range(2)])
    y = S.sb("y", [128, NQH * HD], F32)
    yb = S.sb("yb", [128, NQH * HD], BF16)
    ybT = S.sb("ybT", [128, NQH, 128], BF16)
    ob = S.sb("ob", [128, D], BF16)
    rc = S.sb("rc", [128, 8], F32)
    imp = S.sb("imp", [128, 64], F32)
    sc2 = S.sb("sc2", [128, 64], F32)
    m8 = S.sb("m8", [128, 16], F32)
    selb = S.sb("selb", [128, 128], BF16)
    selT = S.sb("selT", [64, 128], BF16)
    cf = S.sb("cf", [128, NQH * 3], F32)
    scale = HD ** -0.5
    S.op("pool", lambda e: e.memset(selb[:], 0.0), [], [selb])
    q3 = lambda ap, w=128: ap.rearrange("p (r q) -> p r q", q=w)

    def branch_out(gi, br, first):
        for r in range(NR):
            oa = oacc[r]
            base = 0
            h = gi * NR + r
            S.op("dve", lambda e, oa=oa, base=base: e.reciprocal(out=rc[:, 0:1], in_=oa[:, base + HD:base + HD + 1]), [oa], [rc])
            S.op("dve", lambda e, h=h, br=br: e.tensor_tensor(out=rc[:, 1:2], in0=rc[:, 0:1], in1=gl[:, h * 3 + br:h * 3 + br + 1], op=ALU.mult), [rc, gl], [rc])
            if first:
                S.op("dve", lambda e, oa=oa, base=base, h=h: e.tensor_scalar(out=y[:, h * HD:(h + 1) * HD], in0=oa[:, base:base + HD], scalar1=rc[:, 1:2], scalar2=None, op0=ALU.mult), [oa, rc], [y])
            else:
                S.op("dve", lambda e, oa=oa, base=base, h=h: e.scalar_tensor_tensor(out=y[:, h * HD:(h + 1) * HD], in0=oa[:, base:base + HD], scalar=rc[:, 1:2], in1=y[:, h * HD:(h + 1) * HD], op0=ALU.mult, op1=ALU.add), [oa, rc, y], [y])

    for qt in range(NT):
        t0 = qt * 128
        S.dma("sp", ql[:], P[t0:t0 + 128, OQ:OQ + NQH * HD], [P], [ql], ql)
        S.dma("sp", gl[:], P[t0:t0 + 128, OGL:OGL + NQH * 3], [P], [gl], gl)
        S.op("act", lambda e: e.activation(out=gl[:], in_=gl[:], func=AF.Sigmoid), [gl], [gl])
        for h in range(NQH):
            ap = ql[:, h * HD:(h + 1) * HD]
            st = k.rms_rstd(ql, ap, HD)
            S.op("dve", lambda e, ap=ap, st=st: e.scalar_tensor_tensor(out=ap, in0=ap, scalar=st[:, 2:3], in1=qg_bc[:], op0=ALU.mult, op1=ALU.mult), [ql, st, qg_bc], [ql])
        rope(ql, ql[:].rearrange("p (h d) -> p h d", d=HD), NQH, qt, *rq)
        S.op("act", lambda e: e.activation(out=qn[:], in_=ql[:], func=AF.Copy), [ql], [qn])
        k.transpose_to(qn, lambda c: qn[:, c * 128:(c + 1) * 128], NQH, QT, lambda c0, c1: QT[:, c0:c1, :])
        if qt == 0:
            k.dump("QT", QT, QT[:], [128, NQH, 128], BF16)
        for gi in range(NG):
            qrhs = QT[:, gi * NR:(gi + 1) * NR, :].rearrange("p r q -> p (r q)")
            its = [it for it in range(NIT) if 16 * it * 128 + 31 <= t0 + 127]
            for ii, it in enumerate(its):
                sT = stp_.next()
                S.op("pe", lambda e, sT=sT, it=it, gi=gi, qrhs=qrhs: e.matmul(sT[:], lhsT=KcmpT[:, gi, it * 128:(it + 1) * 128], rhs=qrhs, start=True, stop=True), [KcmpT, QT], [sT])
                E = Eb.next()
                S.op("act", lambda e, sT=sT, E=E: e.activation(out=E[:], in_=sT[:], func=AF.Exp, scale=scale), [sT], [E])
                Pm = Pb.next()
                delta = qt - 16 * it
                if delta >= 16:
                    S.op("dve", lambda e, E=E, Pm=Pm: e.tensor_copy(out=Pm[:], in_=E[:]), [E], [Pm])
                else:
                    S.op("dve", lambda e, E=E, Pm=Pm, delta=delta: e.tensor_tensor(out=q3(Pm[:]), in0=q3(E[:]), in1=cmask[:, delta, :].unsqueeze(1).to_broadcast([128, NR, 128]), op=ALU.mult), [E, cmask], [Pm])
                for r in range(NR):
                    oa = oacc[r]
                    base = 0
                    S.op("pe", lambda e, oa=oa, base=base, Pm=Pm, r=r, it=it, gi=gi, ii=ii: e.matmul(oa[:, base:base + HD + 65], lhsT=Pm[:, r * 128:(r + 1) * 128], rhs=Vcmp[:, gi, it, 0:HD + 65], start=(ii == 0), stop=(ii == len(its) - 1)), [Pm, Vcmp], [oa])
            if its:
                for r in range(NR):
                    oa = oacc[r]
                    base = 0
                    S.op("dve", lambda e, oa=oa, base=base, r=r: e.tensor_scalar(out=rc[:, 2 + r:3 + r], in0=oa[:, base + HD + 64:base + HD + 65], scalar1=1e-30, scalar2=None, op0=ALU.max), [oa], [rc])
                    S.op("dve", lambda e, r=r: e.reciprocal(out=rc[:, 2 + r:3 + r], in_=rc[:, 2 + r:3 + r]), [rc], [rc])
                    if r == 0:
                        S.op("dve", lambda e, oa=oa, base=base, r=r: e.tensor_scalar(out=imp[:], in0=oa[:, base + HD:base + HD + 64], scalar1=rc[:, 2 + r:3 + r], scalar2=None, op0=ALU.mult), [oa, rc], [imp])
                    else:
                        S.op("dve", lambda e, oa=oa, base=base, r=r: e.scalar_tensor_tensor(out=imp[:], in0=oa[:, base + HD:base + HD + 64], scalar=rc[:, 2 + r:3 + r], in1=imp[:], op0=ALU.mult, op1=ALU.add), [oa, rc, imp], [imp])
                    h = gi * NR + r
                    S.op("dve", lambda e, r=r, h=h: e.tensor_tensor(out=rc[:, 1:2], in0=rc[:, 2 + r:3 + r], in1=gl[:, h * 3:h * 3 + 1], op=ALU.mult), [rc, gl], [rc])
                    S.op("dve", lambda e, oa=oa, base=base, h=h: e.tensor_scalar(out=y[:, h * HD:(h + 1) * HD], in0=oa[:, base:base + HD], scalar1=rc[:, 1:2], scalar2=None, op0=ALU.mult), [oa, rc], [y])
            else:
                S.op("pool", lambda e: e.memset(imp[:], 0.0), [], [imp])
                S.op("pool", lambda e, gi=gi: e.memset(y[:, gi * NR * HD:(gi + 1) * NR * HD], 0.0), [], [y])
            S.op("dve", lambda e, qt=qt: e.tensor_tensor(out=imp[:], in0=imp[:], in1=fadd[:, qt, :], op=ALU.add), [imp, fadd], [imp])
            S.op("dve", lambda e: e.max(out=m8[:, 0:8], in_=imp[:]), [imp], [m8])
            S.op("dve", lambda e: e.match_replace(out=sc2[:], in_to_replace=m8[:, 0:8], in_values=imp[:], imm_value=-3.0e9), [imp, m8], [sc2])
            S.op("dve", lambda e: e.max(out=m8[:, 8:16], in_=sc2[:]), [sc2], [m8])
            S.op("dve", lambda e: e.tensor_scalar(out=sc2[:], in0=imp[:], scalar1=m8[:, 15:16], scalar2=None, op0=ALU.is_ge), [imp, m8], [sc2])
            S.op("dve", lambda e: e.tensor_scalar(out=imp[:], in0=imp[:], scalar1=-1.0e8, scalar2=None, op0=ALU.is_gt), [imp], [imp])
            S.op("dve", lambda e: e.tensor_tensor(out=selb[:, 0:64], in0=sc2[:], in1=imp[:], op=ALU.mult), [sc2, imp], [selb])
            tp = k.tp.next()
            S.op("pe", lambda e, tp=tp: e.transpose(out=tp[:, 0:128], in_=selb[:], identity=k.ident[:]), [selb, k.ident], [tp])
            S.op("dve", lambda e, tp=tp: e.tensor_copy(out=selT[:], in_=tp[0:64, 0:128]), [tp], [selT])
            for kt in range(qt + 1):
                sT = stp_.next()
                S.op("pe", lambda e, sT=sT, kt=kt, gi=gi, qrhs=qrhs: e.matmul(sT[:], lhsT=KsT[:, gi, kt * 128:(kt + 1) * 128], rhs=qrhs, start=True, stop=True), [KsT, QT], [sT])
                S.op("pe", lambda e, kt=kt: e.matmul(mxp[:], lhsT=efull[:, kt * 128:(kt + 1) * 128], rhs=selT[:], start=True, stop=True), [efull, selT], [mxp])
                E = Eb.next()
                S.op("act", lambda e, sT=sT, E=E: e.activation(out=E[:], in_=sT[:], func=AF.Exp, scale=scale), [sT], [E])
                Pm = Pb.next()
                if kt == qt:
                    S.op("dve", lambda e, E=E: e.tensor_tensor(out=q3(E[:]), in0=q3(E[:]), in1=diag[:].unsqueeze(1).to_broadcast([128, NR, 128]), op=ALU.mult), [E, diag], [E])
                S.op("dve", lambda e, E=E, Pm=Pm: e.tensor_tensor(out=q3(Pm[:]), in0=q3(E[:]), in1=mxp[:].unsqueeze(1).to_broadcast([128, NR, 128]), op=ALU.mult), [E, mxp], [Pm])
                for r in range(NR):
                    oa = oacc[r]
                    base = 0
                    S.op("pe", lambda e, oa=oa, base=base, Pm=Pm, r=r, kt=kt, gi=gi, qt=qt: e.matmul(oa[:, base:base + HD + 1], lhsT=Pm[:, r * 128:(r + 1) * 128], rhs=Vs[:, kt, gi, 0:HD + 1], start=(kt == 0), stop=(kt == qt)), [Pm, Vs], [oa])
            branch_out(gi, 1, False)
            kts = [kt for kt in range(qt - 4, qt + 1) if kt >= 0]
            for kt in kts:
                sT = stp_.next()
                S.op("pe", lambda e, sT=sT, kt=kt, gi=gi, qrhs=qrhs: e.matmul(sT[:], lhsT=KwT[:, gi, kt * 128:(kt + 1) * 128], rhs=qrhs, start=True, stop=True), [KwT, QT], [sT])
                E = Eb.next()
                S.op("act", lambda e, sT=sT, E=E: e.activation(out=E[:], in_=sT[:], func=AF.Exp, scale=scale), [sT], [E])
                Pm = Pb.next()
                if kt == qt:
                    S.op("dve", lambda e, E=E, Pm=Pm: e.tensor_tensor(out=q3(Pm[:]), in0=q3(E[:]), in1=diag[:].unsqueeze(1).to_broadcast([128, NR, 128]), op=ALU.mult), [E, diag], [Pm])
                elif kt == qt - 4:
                    S.op("dve", lambda e, E=E, Pm=Pm: e.tensor_tensor(out=q3(Pm[:]), in0=q3(E[:]), in1=sup[:].unsqueeze(1).to_broadcast([128, NR, 128]), op=ALU.mult), [E, sup], [Pm])
                else:
                    S.op("dve", lambda e, E=E, Pm=Pm: e.tensor_copy(out=Pm[:], in_=E[:]), [E], [Pm])
                for r in range(NR):
                    oa = oacc[r]
                    base = 0
                    S.op("pe", lambda e, oa=oa, base=base, Pm=Pm, r=r, kt=kt, gi=gi, kts=kts: e.matmul(oa[:, base:base + HD + 1], lhsT=Pm[:, r * 128:(r + 1) * 128], rhs=Vw[:, kt, gi, 0:HD + 1], start=(kt == kts[0]), stop=(kt == kts[-1])), [Pm, Vw], [oa])
            branch_out(gi, 2, False)
        S.op("act", lambda e: e.activation(out=yb[:], in_=y[:], func=AF.Copy), [y], [yb])
        k.transpose_to(yb, lambda c: yb[:, c * 128:(c + 1) * 128], NQH, ybT, lambda c0, c1: ybT[:, c0:c1, :])
        for nn in range(4):
            acc = k.acc.next()
            for c in range(NQH):
                S.op("pe", lambda e, acc=acc, c=c, nn=nn: e.matmul(acc[:], lhsT=ybT[:, c, :], rhs=wo_s[:, c, nn * 512:(nn + 1) * 512], start=(c == 0), stop=(c == NQH - 1)), [ybT, wo_s], [acc])
            S.op("act", lambda e, acc=acc, nn=nn: e.activation(out=ob[:, nn * 512:(nn + 1) * 512], in_=acc[:], func=AF.Copy), [acc], [ob])
        S.dma("sp", part[t0:t0 + 128, :], ob[:], [ob], [part], ob)
    S.phase_end()
    S.emit()
    return nc


def _nsa_consts(T):
    NT = T // 128
    NCMP = (T - 32) // 16 + 1
    NIT = (NCMP + 127) // 128
    NBLK = T // 64
    c0 = np.arange(NIT * 128)[:, None] * 16
    s0 = np.arange(64)[None, :] * 64
    ov = np.clip(np.minimum(c0 + 32, s0 + 64) - np.maximum(c0, s0), 0, None) / 16.0
    ov[NCMP:, :] = 0.0
    ov[:, NBLK:] = 0.0
    efull = (np.arange(T)[None, :] // 64 == np.arange(64)[:, None]).astype(np.float32)
    ip = np.arange(128)[:, None]
    qp = np.arange(128)[None, :]
    cmask = np.stack([(16 * ip + 31 <= qp + 128 * d) for d in range(16)], 0).astype(np.float32)
    diag = (ip <= qp).astype(np.float32)
    t = np.arange(T)[:, None]
    blk = np.arange(64)[None, :]
    cur = t // 64
    causal = blk <= cur
    forced = ((blk == 0) | (blk >= cur - 1)) & causal
    fadd = np.where(forced, 1.0e9, np.where(causal, 0.0, -1.0e9)).astype(np.float32).reshape(NT, 128, 64)
    invf = np.exp(-math.log(500000.0) * np.arange(0, 32, 2, dtype=np.float32) / 32).astype(np.float32)
    return dict(c_ov=ov.astype(np.float32), c_efull=efull, c_cmask=cmask, c_diag=diag, c_fadd=fadd, c_invf=invf)


def _nsa_cols(gg):
    Q0 = 2048 + 3072 + 32
    q = Q0 + np.arange(8 * gg * 128, 8 * gg * 128 + 1024)
    parts = [q]
    off = Q0 + 2048
    for i in range(6):
        parts.append(off + i * 512 + np.arange(2 * gg * 128, 2 * gg * 128 + 256))
    gl = off + 6 * 512 + np.arange(8 * gg * 3, 8 * gg * 3 + 24)
    parts.append(gl)
    return np.concatenate(parts)


def _nsa_inputs(inp, xb, posb, gg, T):
    f32 = lambda a: np.ascontiguousarray(np.asarray(a), dtype=np.float32)
    m = dict(c_ident=_ident(), xb=xb, g_n=f32(inp["norm_mix"][0]), w_in=f32(np.asarray(inp["ev_w_in"][0])[:, _nsa_cols(gg)]),
             pos=np.ascontiguousarray(np.asarray(posb).astype(np.int32).reshape(T, 1)),
             qg=f32(inp["ev_q_gain"][0]), kcg=f32(inp["ev_kc_gain"][0]), ksg=f32(inp["ev_ks_gain"][0]), kwg=f32(inp["ev_kw_gain"][0]),
             pe_k=f32(inp["ev_pe_k"][0]), pe_v=f32(inp["ev_pe_v"][0]), wk1=f32(inp["ev_cmp_wk1"][0]), wv1=f32(inp["ev_cmp_wv1"][0]),
             wk2=f32(inp["ev_cmp_wk2"][0]), wv2=f32(inp["ev_cmp_wv2"][0]),
             w_o=f32(np.asarray(inp["ev_w_out"][0])[2048 + gg * 1024:2048 + (gg + 1) * 1024]))
    m.update(_nsa_consts(T))
    return m


NCORES = 8
SEQ = 4096
BATCH = 4
TSH = 2048
_cache = {}


def _run(key, builder, in_maps):
    if key not in _cache:
        _cache[key] = builder()
    res = run_bass_kernel_spmd(_cache[key], in_maps, core_ids=list(range(NCORES)))
    return res.results


def _rwkv_inputs(inp, h1b, hh):
    f32 = lambda a: np.ascontiguousarray(np.asarray(a), dtype=np.float32)
    cols = slice(hh * RC, (hh + 1) * RC)
    g = lambda n: np.asarray(inp[n][0])
    return dict(c_ident=_ident(), h1=h1b, g_n=f32(inp["norm_mix"][1]), mu=f32(g("od_mu")),
                w_r=f32(g("od_w_r")[:, cols]), w_k=f32(g("od_w_k")[:, cols]), w_v=f32(g("od_w_v")[:, cols]), w_o=f32(g("od_w_o")[cols, :]),
                w0=f32(g("od_w0")[cols]), a0=f32(g("od_a0")[cols]), k_k=f32(g("od_k_k")[cols]), k_a=f32(g("od_k_a")[cols]),
                r_k=f32(g("od_r_k").reshape(-1)[cols]), ln_w=f32(g("od_ln_w")[cols]), ln_b=f32(g("od_ln_b")[cols]),
                w1=f32(g("od_w1")), w2=f32(g("od_w2")[:, cols]), a1=f32(g("od_a1")), a2=f32(g("od_a2")[:, cols]),
                g1=f32(g("od_g1")), g2=f32(g("od_g2")[:, cols]))


def kernel(**inp):
    f32 = lambda a: np.ascontiguousarray(np.asarray(a), dtype=np.float32)
    x = f32(inp["x"])
    mem = f32(inp["mem"])
    positions = np.asarray(inp["positions"])
    ident = _ident()

    def tok_shard(arr, c):
        b, half = c // 2, c % 2
        return np.ascontiguousarray(arr[b, half * TSH:(half + 1) * TSH])

    def xattn_w(layer, c):
        return dict(mem=mem[c // 2], g_x=f32(inp["norm_xattn"][layer]), g_m=f32(inp["norm_mem"][layer]),
                    g_f=f32(inp["norm_ffn"][layer]), wq=f32(inp["xattn_wq"][layer]), wkv=f32(inp["xattn_wkv"][layer]),
                    wo=f32(inp["xattn_wo"][layer]), qg=f32(inp["xattn_q_gain"][layer]), kg=f32(inp["xattn_k_gain"][layer]))

    r_ssm = _run("ssm", lambda: build_ssm(SEQ), [_ssm_inputs(inp, x[c // 2], c % 2) for c in range(NCORES)])
    p_ssm = [r_ssm[c]["part"] for c in range(NCORES)]
    r_nsa = _run("nsa", lambda: build_nsa(SEQ), [_nsa_inputs(inp, x[c // 2], positions[c // 2], c % 2, SEQ) for c in range(NCORES)])
    p_nsa = [r_nsa[c]["part"] for c in range(NCORES)]
    maps = []
    for c in range(NCORES):
        b, half = c // 2, c % 2
        sl = slice(half * TSH, (half + 1) * TSH)
        parts = np.stack([p_ssm[2 * b][sl], p_ssm[2 * b + 1][sl], p_nsa[2 * b][sl], p_nsa[2 * b + 1][sl]], axis=0)
        m = dict(c_ident=ident, xres=tok_shard(x, c), parts=parts, w1=f32(inp["ev_ffn_w1"][0]), w3=f32(inp["ev_ffn_w3"][0]), w2=f32(inp["ev_ffn_w2"][0]))
        m.update(xattn_w(0, c))
        maps.append(m)
    r = _run("mid0", lambda: build_mid(TSH, 4, 5632, False), maps)
    h1 = [r[c]["h_out"] for c in range(NCORES)]
    maps = [_rwkv_inputs(inp, np.concatenate([h1[2 * (c // 2)], h1[2 * (c // 2) + 1]], axis=0), c % 2) for c in range(NCORES)]
    r_rw = _run("rwkv", lambda: build_rwkv(SEQ), maps)
    p_rw = [r_rw[c]["part"] for c in range(NCORES)]
    maps = []
    for c in range(NCORES):
        b, half = c // 2, c % 2
        sl = slice(half * TSH, (half + 1) * TSH)
        parts = np.stack([p_rw[2 * b][sl], p_rw[2 * b + 1][sl]], axis=0)
        m = dict(c_ident=ident, xres=h1[c], parts=parts, router=f32(inp["od_router"][0]))
        m.update(xattn_w(1, c))
        maps.append(m)
    r = _run("mid1", lambda: build_mid(TSH, 2, 0, True), maps)
    h2 = [r[c]["h_out"] for c in range(NCORES)]
    hn_all = np.concatenate([r[c]["hn_out"] for c in range(NCORES)], axis=0)
    gates = np.concatenate([r[c]["gate_out"] for c in range(NCORES)], axis=0)
    maps = []
    for e in range(NCORES):
        maps.append(dict(c_ident=ident, hn=hn_all, gate=np.ascontiguousarray(gates[:, e:e + 1]),
                         w1=f32(inp["od_moe_w1"][0, e]), w3=f32(inp["od_moe_w3"][0, e]), w2=f32(inp["od_moe_w2"][0, e])))
    r = _run("moe", lambda: build_moe(BATCH * SEQ, 7168), maps)
    maps = []
    for c in range(NCORES):
        parts = np.stack([r[e]["part"][c * TSH:(c + 1) * TSH] for e in range(NCORES)], axis=0)
        maps.append(dict(xres=h2[c], parts=parts))
    r = _run("fin", lambda: build_fin(TSH, NCORES), maps)
    out = np.stack([np.concatenate([r[2 * b]["out"], r[2 * b + 1]["out"]], axis=0) for b in range(BATCH)], axis=0)
    return out.astype(np.float32)
```

```python
import math
import numpy as np
import ml_dtypes
import concourse.bass as bass
import concourse.mybir as mybir
from concourse.bass_utils import run_bass_kernel_spmd
from contextlib import ExitStack

F32 = mybir.dt.float32
BF16 = mybir.dt.bfloat16
I32 = mybir.dt.int32
ALU = mybir.AluOpType
AF = mybir.ActivationFunctionType
AX = mybir.AxisListType

D = 2048
KC = D // 128
NORM_EPS = 1e-6


class Buf:
    __slots__ = ("name", "t", "last_w", "readers")

    def __init__(self, name, t):
        self.name = name
        self.t = t
        self.last_w = None
        self.readers = []

    def __getitem__(self, k):
        return self.t[k]


class Sched:
    def __init__(self, nc):
        self.nc = nc
        self.ops = []
        self.es = ExitStack()
        self.uid = 0

    def sb(self, name, shape, dt):
        self.uid += 1
        nm = f"{name}_{self.uid}"
        es = self.phase_es if getattr(self, "phase_es", None) is not None else self.es
        return Buf(nm, es.enter_context(self.nc.sbuf_tensor(nm, list(shape), dt)))

    def phase_begin(self):
        self.phase_es = ExitStack()

    def phase_end(self):
        self.ops.append(dict(barrier=True))
        self.phase_es.close()
        self.phase_es = None

    def ps(self, name, shape, dt=F32):
        self.uid += 1
        nm = f"{name}_{self.uid}"
        return Buf(nm, self.es.enter_context(self.nc.psum_tensor(nm, list(shape), dt)))

    def dram(self, name, shape, dt, kind="Internal"):
        return Buf(name, self.nc.dram_tensor(name, list(shape), dt, kind=kind).ap())

    def op(self, eng, fn, reads=(), writes=(), dma=False, owner=None):
        self.ops.append(dict(eng=eng, fn=fn, reads=list(reads), writes=list(writes), dma=dma, owner=owner))

    def dma(self, q, out_ap, in_ap, reads, writes, owner, **kw):
        self.op(q, lambda e: e.dma_start(out=out_ap, in_=in_ap, **kw), reads, writes, dma=True, owner=owner)

    def emit(self):
        nc = self.nc
        ops = self.ops
        n = len(ops)
        deps = [None] * n
        needed = [False] * n
        last_on = {}
        bar_deps = []
        pending = set()
        for i, o in enumerate(ops):
            if o.get("barrier"):
                bar_deps = list(last_on.values())
                pending = set(["pe", "act", "dve", "pool", "sp"])
                deps[i] = []
                o.update(eng=None, dma=False, reads=[], writes=[])
                continue
            d = set()
            if bar_deps and o["eng"] in pending:
                d.update(bar_deps)
                pending.discard(o["eng"])
            last_on[("dma", o["owner"].name) if o["dma"] else (o["eng"],)] = i
            for r in o["reads"]:
                if r.last_w is not None:
                    d.add(r.last_w)
            for w in o["writes"]:
                if w.last_w is not None:
                    d.add(w.last_w)
                d.update(w.readers)
            d.discard(i)
            dd = []
            for j in d:
                oj = ops[j]
                if (not oj["dma"]) and (not o["dma"]) and oj["eng"] == o["eng"] == "pe":
                    continue
                dd.append(j)
            deps[i] = dd
            for j in dd:
                needed[j] = True
            for r in o["reads"]:
                r.readers.append(i)
            for w in o["writes"]:
                w.last_w = i
                w.readers = []
        engs = ["pe", "act", "dve", "pool", "sp"]
        esem = {e: self.es.enter_context(nc.semaphore("s_" + e)) for e in engs}
        ecount = {e: 0 for e in engs}
        dsem = {}
        dcount = {}
        tok = [None] * n
        for i, o in enumerate(ops):
            if o.get("barrier"):
                continue
            if o["dma"]:
                ow = o["owner"]
                if ow not in dsem:
                    dsem[ow] = self.es.enter_context(nc.semaphore("d_" + ow.name))
                    dcount[ow] = 0
                dcount[ow] += 16
                tok[i] = (dsem[ow], dcount[ow], 16)
            elif needed[i]:
                ecount[o["eng"]] += 1
                tok[i] = (esem[o["eng"]], ecount[o["eng"]], 1)
        streams = {e: [] for e in engs}
        seen = {e: {} for e in engs}
        for i, o in enumerate(ops):
            if o.get("barrier"):
                continue
            e = o["eng"]
            waits = {}
            for j in deps[i]:
                s, v, _ = tok[j]
                if seen[e].get(id(s), 0) >= v:
                    continue
                if waits.get(id(s), (None, 0))[1] < v:
                    waits[id(s)] = (s, v)
            for k, (s, v) in waits.items():
                seen[e][k] = v
            streams[e].append((list(waits.values()), o["fn"], tok[i]))

        def run_stream(engine, lst):
            for waits, fn, t in lst:
                for s, v in waits:
                    engine.wait_ge(s, v)
                ins = fn(engine)
                if t is not None:
                    ins.then_inc(t[0], t[2])

        with nc.Block() as block:
            @block.tensor
            def _(eng):
                run_stream(eng, streams["pe"])

            @block.scalar
            def _(eng):
                run_stream(eng, streams["act"])

            @block.vector
            def _(eng):
                run_stream(eng, streams["dve"])

            @block.gpsimd
            def _(eng):
                run_stream(eng, streams["pool"])

            @block.sync
            def _(eng):
                run_stream(eng, streams["sp"])
                for ow, s in dsem.items():
                    eng.wait_ge(s, dcount[ow])
        self.es.close()


class Rot:
    def __init__(self, bufs):
        self.bufs = bufs
        self.i = 0

    def next(self):
        b = self.bufs[self.i % len(self.bufs)]
        self.i += 1
        return b


class K:
    def __init__(self, nc, n_tp=2, n_acc=4):
        self.nc = nc
        self.S = Sched(nc)
        S = self.S
        self.ident_d = S.dram("c_ident", [128, 128], F32, kind="ExternalInput")
        self.ident = S.sb("ident", [128, 128], BF16)
        S.dma("pool", self.ident[:], self.ident_d[:], [self.ident_d], [self.ident], self.ident)
        self.tp = Rot([S.ps("tp", [128, 1024], BF16) for _ in range(n_tp)])
        self.acc = Rot([S.ps("acc", [128, 512], F32) for _ in range(n_acc)])
        self.junk = S.sb("junk", [128, 2048], BF16)
        self.stat = Rot([S.sb("stat", [128, 8], F32) for _ in range(6)])
        self.wq = 0

    def dq(self):
        return "sp"

    def dump(self, name, buf, ap, shape, dt=F32):
        if not getattr(self, "debug", False):
            return
        S = self.S
        d = S.dram("dbg_" + name, list(shape), dt, kind="ExternalOutput")
        S.dma("sp", d[:], ap, [buf], [d], d)

    def gain_bc(self, name, ap_row, n):
        S = self.S
        b = S.sb(name, [128, n], F32)
        src = ap_row.t if isinstance(ap_row, Buf) else ap_row
        S.dma("sp", b[:], src.partition_broadcast(128), [], [b], b)
        return b

    def rms_rstd(self, x, xap, n, eps=NORM_EPS):
        S = self.S
        st = self.stat.next()
        junk = self.junk
        S.op("act", lambda e: e.activation(out=junk[:, 0:n], in_=xap, func=AF.Square, accum_out=st[:, 0:1]), [x], [junk, st])
        S.op("dve", lambda e: e.tensor_scalar(out=st[:, 1:2], in0=st[:, 0:1], scalar1=1.0 / n, scalar2=eps, op0=ALU.mult, op1=ALU.add), [st], [st])
        S.op("act", lambda e: e.activation(out=st[:, 3:4], in_=st[:, 1:2], func=AF.Sqrt), [st], [st])
        S.op("dve", lambda e: e.reciprocal(out=st[:, 2:3], in_=st[:, 3:4]), [st], [st])
        return st

    def rmsnorm(self, x, xap, gain, gap, out, oap, n, eps=NORM_EPS):
        S = self.S
        st = self.rms_rstd(x, xap, n, eps)
        S.op("dve", lambda e: e.scalar_tensor_tensor(out=oap, in0=xap, scalar=st[:, 2:3], in1=gap, op0=ALU.mult, op1=ALU.mult), [x, st, gain], [out])

    def transpose_to(self, src, src_ap_fn, nchunks, dst, dst_ap_fn, eng_rot=("dve", "act")):
        S = self.S
        for c0 in range(0, nchunks, 8):
            c1 = min(nchunks, c0 + 8)
            tp = self.tp.next()
            for c in range(c0, c1):
                S.op("pe", lambda e, c=c, tp=tp, c0=c0: e.transpose(out=tp[:, (c - c0) * 128:(c - c0 + 1) * 128], in_=src_ap_fn(c), identity=self.ident[:]), [src, self.ident], [tp])
            eng = eng_rot[(c0 // 8) % len(eng_rot)]
            if eng == "dve":
                S.op("dve", lambda e, tp=tp, c0=c0, c1=c1: e.tensor_copy(out=dst_ap_fn(c0, c1), in_=tp[:, 0:(c1 - c0) * 128]), [tp], [dst])
            else:
                S.op("act", lambda e, tp=tp, c0=c0, c1=c1: e.activation(out=dst_ap_fn(c0, c1), in_=tp[:, 0:(c1 - c0) * 128], func=AF.Copy), [tp], [dst])


def _ident():
    return np.eye(128, dtype=np.float32)


BLK = 256
W2W = 128


class Swiglu:
    def __init__(self, k, H, blk=None, w2w=None, n_w2=1):
        S = k.S
        self.k = k
        self.H = H
        self.HC = H // 128
        self.blk = blk or BLK
        self.w2w = w2w or W2W
        self.w1g = Rot([S.sb("w1g", [128, KC, 256], BF16) for _ in range(2)])
        self.w3g = Rot([S.sb("w3g", [128, KC, 256], BF16) for _ in range(2)])
        self.w2n = Rot([S.sb("w2n", [128, self.HC, self.w2w], BF16) for _ in range(n_w2)])
        self.actT = S.sb("actT", [128, self.HC, self.blk], BF16)
        self.sg = Rot([S.sb("sg", [128, self.blk], F32) for _ in range(2)])

    def run(self, xT, w1d, w3d, w2d, out_cb, gate=None):
        k, S, HC, blk, w2w = self.k, self.k.S, self.HC, self.blk, self.w2w
        w1v = w1d.t.rearrange("(kc p) n -> p kc n", p=128)
        w3v = w3d.t.rearrange("(kc p) n -> p kc n", p=128)
        w2v = w2d.t.rearrange("(c p) n -> p c n", p=128)
        actT = self.actT
        for hg in range(self.H // 256):
            w1g = self.w1g.next()
            w3g = self.w3g.next()
            S.dma("pool", w1g[:], w1v[:, :, hg * 256:(hg + 1) * 256], [w1d], [w1g], w1g)
            S.dma("pool", w3g[:], w3v[:, :, hg * 256:(hg + 1) * 256], [w3d], [w3g], w3g)
            for c in range(2):
                ga = k.acc.next()
                ua = k.acc.next()
                for kc in range(KC):
                    S.op("pe", lambda e, ga=ga, w1g=w1g, kc=kc, c=c: e.matmul(ga[:, 0:blk], lhsT=w1g[:, kc, c * 128:(c + 1) * 128], rhs=xT[:, kc, :], start=(kc == 0), stop=(kc == KC - 1)), [w1g, xT], [ga])
                for kc in range(KC):
                    S.op("pe", lambda e, ua=ua, w3g=w3g, kc=kc, c=c: e.matmul(ua[:, 0:blk], lhsT=w3g[:, kc, c * 128:(c + 1) * 128], rhs=xT[:, kc, :], start=(kc == 0), stop=(kc == KC - 1)), [w3g, xT], [ua])
                sg = self.sg.next()
                S.op("act", lambda e, sg=sg, ga=ga: e.activation(out=sg[:], in_=ga[:, 0:blk], func=AF.Silu), [ga], [sg])
                hc = hg * 2 + c
                S.op("dve", lambda e, sg=sg, ua=ua, hc=hc: e.tensor_tensor(out=actT[:, hc, :], in0=sg[:], in1=ua[:, 0:blk], op=ALU.mult), [sg, ua], [actT])
        for nn in range(D // w2w):
            w2n = self.w2n.next()
            S.dma("pool", w2n[:], w2v[:, :, nn * w2w:(nn + 1) * w2w], [w2d], [w2n], w2n)
            for tt in range(blk // 128):
                acc = k.acc.next()
                for c in range(HC):
                    S.op("pe", lambda e, acc=acc, w2n=w2n, c=c, tt=tt: e.matmul(acc[:, 0:w2w], lhsT=actT[:, c, tt * 128:(tt + 1) * 128], rhs=w2n[:, c, :], start=(c == 0), stop=(c == HC - 1)), [actT, w2n], [acc])
                out_cb(tt, nn * w2w, (nn + 1) * w2w, acc)


XH, XD, MEM = 4, 128, 256


def build_mid(T, n_part, H, moe, debug=False):
    nc = bass.Bass("TRN2", target_bir_lowering=False)
    k = K(nc)
    k.debug = debug
    S = k.S
    NT = T // 128
    NB = BLK // 128
    xres = S.dram("xres", [T, D], F32, kind="ExternalInput")
    parts = S.dram("parts", [n_part, T, D], BF16, kind="ExternalInput") if n_part else None
    mem = S.dram("mem", [MEM, D], F32, kind="ExternalInput")
    g_x = S.dram("g_x", [D], F32, kind="ExternalInput")
    g_m = S.dram("g_m", [D], F32, kind="ExternalInput")
    g_f = S.dram("g_f", [D], F32, kind="ExternalInput")
    wq = S.dram("wq", [D, 512], F32, kind="ExternalInput")
    wkv = S.dram("wkv", [D, 1024], F32, kind="ExternalInput")
    wo = S.dram("wo", [512, D], F32, kind="ExternalInput")
    qg = S.dram("qg", [128], F32, kind="ExternalInput")
    kg = S.dram("kg", [128], F32, kind="ExternalInput")
    h_out = S.dram("h_out", [T, D], F32, kind="ExternalOutput")
    if moe:
        router = S.dram("router", [D, 8], F32, kind="ExternalInput")
        hn_out = S.dram("hn_out", [T, D], BF16, kind="ExternalOutput")
        gate_out = S.dram("gate_out", [T, 8], F32, kind="ExternalOutput")
    else:
        w1 = S.dram("w1", [D, H], F32, kind="ExternalInput")
        w3 = S.dram("w3", [D, H], F32, kind="ExternalInput")
        w2 = S.dram("w2", [H, D], F32, kind="ExternalInput")

    gx_bc = k.gain_bc("gx", g_x, D)
    gf_bc = k.gain_bc("gf", g_f, D)
    qg_bc = k.gain_bc("qg", qg, 128)
    kg_bc = k.gain_bc("kg", kg, 128)
    hkeep = S.sb("hkeep", [128, NB, D], F32)
    gm_ap = hkeep[:, 0, :]
    S.dma("sp", gm_ap, g_m.t.partition_broadcast(128), [], [hkeep], hkeep)
    wq_s = S.sb("wq_s", [128, KC, 512], BF16)
    S.dma("pool", wq_s[:], wq.t.rearrange("(kc p) n -> p kc n", p=128), [wq], [wq_s], wq_s)
    wo_s = S.sb("wo_s", [128, 4, D], BF16)
    S.dma("pool", wo_s[:], wo.t.rearrange("(kc p) n -> p kc n", p=128), [wo], [wo_s], wo_s)
    if moe:
        wkvbuf = Rot([S.sb("wkvg", [128, KC, 256], BF16) for _ in range(2)])
    else:
        sw = Swiglu(k, H)
        wkvbuf = sw.w1g
    hbuf = Rot([S.sb("h_f", [128, D], F32) for _ in range(2)])
    pbuf = Rot([S.sb("p_b", [128, D], BF16) for _ in range(2)]) if n_part else None
    hxbuf = Rot([S.sb("hx", [128, D], BF16) for _ in range(2)])
    hxTbuf = Rot([S.sb("hxT", [128, KC, 128], BF16) for _ in range(2)])
    hfT = S.sb("hfT", [128, KC, BLK], BF16)
    kv = S.sb("kv_f", [128, 1024], F32)
    kn = S.sb("k_n", [128, 512], BF16)
    KT = S.sb("KT", [128, XH, MEM], BF16)
    Vx = S.sb("Vx", [128, 2, XH, 132], BF16)
    qf = S.sb("q_f", [128, 512], F32)
    qn = S.sb("q_n", [128, 512], BF16)
    qT = S.sb("qT", [128, XH, 128], BF16)
    pT = S.sb("pT", [128, XH, 2, 128], BF16)
    on = S.sb("o_n", [128, 512], BF16)
    onT = S.sb("o_nT", [128, 4, 128], BF16)
    rs = S.sb("rs", [128, XH], F32)

    S.op("pool", lambda e: e.memset(Vx[:], 1.0), [], [Vx])
    mTs = []
    for mt in range(2):
        mt_f = hbuf.next()
        S.dma("sp", mt_f[:], mem[mt * 128:(mt + 1) * 128, :], [mem], [mt_f], mt_f)
        mn = hxbuf.next()
        k.rmsnorm(mt_f, mt_f[:], hkeep, gm_ap, mn, mn[:], D)
        mT = hxTbuf.next()
        k.transpose_to(mn, lambda c, mn=mn: mn[:, c * 128:(c + 1) * 128], KC, mT, lambda c0, c1, mT=mT: mT[:, c0:c1, :])
        mTs.append(mT)
    wkv_r = wkv.t.rearrange("(kc p) n -> p kc n", p=128)
    kvs = [kv, S.sb("kv_f2", [128, 1024], F32)]
    for nn in range(4):
        wg = wkvbuf.next()
        S.dma("pool", wg[:], wkv_r[:, :, nn * 256:(nn + 1) * 256], [wkv], [wg], wg)
        for mt in range(2):
            acc = k.acc.next()
            for kc in range(KC):
                S.op("pe", lambda e, acc=acc, kc=kc, mt=mt, wg=wg: e.matmul(acc[:, 0:256], lhsT=mTs[mt][:, kc, :], rhs=wg[:, kc, :], start=(kc == 0), stop=(kc == KC - 1)), [mTs[mt], wg], [acc])
            S.op("act", lambda e, acc=acc, nn=nn, mt=mt: e.activation(out=kvs[mt][:, nn * 256:(nn + 1) * 256], in_=acc[:, 0:256], func=AF.Copy), [acc], [kvs[mt]])
    for mt in range(2):
        kvm = kvs[mt]
        for h in range(XH):
            k.rmsnorm(kvm, kvm[:, h * 128:(h + 1) * 128], kg_bc, kg_bc[:], kn, kn[:, h * 128:(h + 1) * 128], 128)
        k.transpose_to(kn, lambda c: kn[:, c * 128:(c + 1) * 128], XH, KT, lambda c0, c1, mt=mt: KT[:, c0:c1, mt * 128:(mt + 1) * 128])
        S.op("dve", lambda e, mt=mt, kvm=kvm: e.tensor_copy(out=Vx[:, mt, :, 0:128], in_=kvm[:, 512:1024].rearrange("p (h d) -> p h d", h=XH)), [kvm], [Vx])

    if moe:
        hf32 = S.sb("hf32", [128, D], F32)
        rt_s = S.sb("rt_s", [128, KC, 8], F32)
        S.dma("sp", rt_s[:], router.t.rearrange("(kc p) e -> p kc e", p=128), [router], [rt_s], rt_s)
        ident_f = S.sb("ident_f", [128, 128], F32)
        S.dma("sp", ident_f[:], k.ident_d[:], [k.ident_d], [ident_f], ident_f)
        hf32T = S.sb("hf32T", [128, KC, 128], F32)
        lg = S.sb("lg", [128, 8], F32)
        m8 = S.sb("m8", [128, 8], F32)
        gt = S.sb("gt", [128, 8], F32)

    scale = XD ** -0.5
    for tt in range(NT):
        bt = tt % NB
        h = hbuf.next()
        S.dma("sp", h[:], xres[tt * 128:(tt + 1) * 128, :], [xres], [h], h)
        for p in range(n_part):
            pb = pbuf.next()
            S.dma("sp", pb[:], parts[p, tt * 128:(tt + 1) * 128, :], [parts], [pb], pb)
            S.op("dve", lambda e, h=h, pb=pb: e.tensor_tensor(out=h[:], in0=h[:], in1=pb[:], op=ALU.add), [h, pb], [h])
        if tt == 0:
            k.dump("h0", h, h[:], [128, D])
        hx = hxbuf.next()
        k.rmsnorm(h, h[:], gx_bc, gx_bc[:], hx, hx[:], D)
        if tt == 0:
            k.dump("hx", hx, hx[:], [128, D], BF16)
        hxT = hxTbuf.next()
        k.transpose_to(hx, lambda c, hx=hx: hx[:, c * 128:(c + 1) * 128], KC, hxT, lambda c0, c1, hxT=hxT: hxT[:, c0:c1, :])
        acc = k.acc.next()
        for kc in range(KC):
            S.op("pe", lambda e, acc=acc, kc=kc, hxT=hxT: e.matmul(acc[:], lhsT=hxT[:, kc, :], rhs=wq_s[:, kc, :], start=(kc == 0), stop=(kc == KC - 1)), [hxT, wq_s], [acc])
        S.op("act", lambda e, acc=acc: e.activation(out=qf[:], in_=acc[:], func=AF.Copy), [acc], [qf])
        if tt == 0:
            k.dump("qf", qf, qf[:], [128, 512])
        for hh in range(XH):
            k.rmsnorm(qf, qf[:, hh * 128:(hh + 1) * 128], qg_bc, qg_bc[:], qn, qn[:, hh * 128:(hh + 1) * 128], 128)
        if tt == 0:
            k.dump("qn", qn, qn[:], [128, 512], BF16)
            k.dump("KT", KT, KT[:], [128, XH, MEM], BF16)
            k.dump("Vx", Vx, Vx[:], [128, 2, XH, 132], BF16)
        k.transpose_to(qn, lambda c: qn[:, c * 128:(c + 1) * 128], XH, qT, lambda c0, c1: qT[:, c0:c1, :])
        for half in range(2):
            sacc = k.acc.next()
            for j in range(4):
                hh, mt = (half * 4 + j) // 2, (half * 4 + j) % 2
                S.op("pe", lambda e, sacc=sacc, j=j, hh=hh, mt=mt: e.matmul(sacc[:, j * 128:(j + 1) * 128], lhsT=KT[:, hh, mt * 128:(mt + 1) * 128], rhs=qT[:, hh, :], start=True, stop=True), [KT, qT], [sacc])
            S.op("act", lambda e, sacc=sacc, half=half: e.activation(out=pT[:, half * 2:(half + 1) * 2, :, :].rearrange("p a b q -> p (a b q)"), in_=sacc[:], func=AF.Exp, scale=scale), [sacc], [pT])
        oacc = k.acc.next()
        for hh in range(XH):
            for mt in range(2):
                S.op("pe", lambda e, oacc=oacc, hh=hh, mt=mt: e.matmul(oacc[:, hh * 128:hh * 128 + 128], lhsT=pT[:, hh, mt, :], rhs=Vx[:, mt, hh, 0:128], start=(mt == 0), stop=(mt == 1)), [pT, Vx], [oacc])
        racc = k.acc.next()
        for hh in range(XH):
            for mt in range(2):
                S.op("pe", lambda e, racc=racc, hh=hh, mt=mt: e.matmul(racc[:, hh:hh + 1], lhsT=pT[:, hh, mt, :], rhs=Vx[:, mt, hh, 128:129], start=(mt == 0), stop=(mt == 1)), [pT, Vx], [racc])
        S.op("dve", lambda e, racc=racc: e.reciprocal(out=rs[:], in_=racc[:, 0:XH]), [racc], [rs])
        for hh in range(XH):
            S.op("dve", lambda e, oacc=oacc, hh=hh: e.tensor_scalar(out=on[:, hh * 128:(hh + 1) * 128], in0=oacc[:, hh * 128:(hh + 1) * 128], scalar1=rs[:, hh:hh + 1], scalar2=None, op0=ALU.mult), [oacc, rs], [on])
        if tt == 0:
            k.dump("pT", pT, pT[:], [128, XH, 2, 128], BF16)
            k.dump("on", on, on[:], [128, 512], BF16)
        k.transpose_to(on, lambda c: on[:, c * 128:(c + 1) * 128], 4, onT, lambda c0, c1: onT[:, c0:c1, :])
        for nn in range(4):
            acc = k.acc.next()
            for kc in range(4):
                S.op("pe", lambda e, acc=acc, kc=kc, nn=nn: e.matmul(acc[:], lhsT=onT[:, kc, :], rhs=wo_s[:, kc, nn * 512:(nn + 1) * 512], start=(kc == 0), stop=(kc == 3)), [onT, wo_s], [acc])
            S.op("dve", lambda e, acc=acc, nn=nn, h=h: e.tensor_tensor(out=h[:, nn * 512:(nn + 1) * 512], in0=h[:, nn * 512:(nn + 1) * 512], in1=acc[:], op=ALU.add), [acc, h], [h])
        hf = hx
        k.rmsnorm(h, h[:], gf_bc, gf_bc[:], hf, hf[:], D)
        if moe:
            S.dma("sp", h_out[tt * 128:(tt + 1) * 128, :], h[:], [h], [h_out], h)
            S.dma("sp", hn_out[tt * 128:(tt + 1) * 128, :], hf[:], [hf], [hn_out], hf)
            k.rmsnorm(h, h[:], gf_bc, gf_bc[:], hf32, hf32[:], D)
            for c0 in range(0, KC, 4):
                tacc = k.acc.next()
                for c in range(c0, c0 + 4):
                    S.op("pe", lambda e, tacc=tacc, c=c, c0=c0: e.transpose(out=tacc[:, (c - c0) * 128:(c - c0 + 1) * 128], in_=hf32[:, c * 128:(c + 1) * 128], identity=ident_f[:]), [hf32, ident_f], [tacc])
                S.op("dve", lambda e, tacc=tacc, c0=c0: e.tensor_copy(out=hf32T[:, c0:c0 + 4, :].rearrange("p a b -> p (a b)"), in_=tacc[:]), [tacc], [hf32T])
            lacc = k.acc.next()
            for kc in range(KC):
                S.op("pe", lambda e, lacc=lacc, kc=kc: e.matmul(lacc[:, 0:8], lhsT=hf32T[:, kc, :], rhs=rt_s[:, kc, :], start=(kc == 0), stop=(kc == KC - 1)), [hf32T, rt_s], [lacc])
            S.op("dve", lambda e, lacc=lacc: e.tensor_copy(out=lg[:], in_=lacc[:, 0:8]), [lacc], [lg])
            S.op("dve", lambda e: e.max(out=m8[:], in_=lg[:]), [lg], [m8])
            S.op("dve", lambda e: e.tensor_scalar(out=gt[:], in0=lg[:], scalar1=m8[:, 1:2], scalar2=None, op0=ALU.is_ge), [lg, m8], [gt])
            S.op("dve", lambda e: e.tensor_scalar(out=lg[:], in0=lg[:], scalar1=m8[:, 0:1], scalar2=None, op0=ALU.subtract), [lg, m8], [lg])
            S.op("act", lambda e: e.activation(out=lg[:], in_=lg[:], func=AF.Exp), [lg], [lg])
            S.op("dve", lambda e: e.tensor_tensor(out=gt[:], in0=gt[:], in1=lg[:], op=ALU.mult), [gt, lg], [gt])
            S.op("dve", lambda e: e.reduce_sum(out=m8[:, 2:3], in_=gt[:], axis=AX.X), [gt], [m8])
            S.op("dve", lambda e: e.reciprocal(out=m8[:, 3:4], in_=m8[:, 2:3]), [m8], [m8])
            S.op("dve", lambda e: e.tensor_scalar(out=gt[:], in0=gt[:], scalar1=m8[:, 3:4], scalar2=None, op0=ALU.mult), [gt, m8], [gt])
            S.dma("sp", gate_out[tt * 128:(tt + 1) * 128, :], gt[:], [gt], [gate_out], gt)
        else:
            k.transpose_to(hf, lambda c, hf=hf: hf[:, c * 128:(c + 1) * 128], KC, hfT, lambda c0, c1, bt=bt: hfT[:, c0:c1, bt * 128:(bt + 1) * 128])
            S.op("pool", lambda e, h=h, bt=bt: e.tensor_copy(out=hkeep[:, bt, :], in_=h[:]), [h], [hkeep])
            if bt == NB - 1:
                t0 = tt - bt

                def out_cb(tq, n0, n1, acc, t0=t0):
                    S.op("dve", lambda e: e.tensor_tensor(out=hkeep[:, tq, n0:n1], in0=hkeep[:, tq, n0:n1], in1=acc[:, 0:n1 - n0], op=ALU.add), [acc, hkeep], [hkeep])
                    if n1 == D:
                        S.dma("sp", h_out[(t0 + tq) * 128:(t0 + tq + 1) * 128, :], hkeep[:, tq, :], [hkeep], [h_out], hkeep)
                sw.run(hfT, w1, w3, w2, out_cb)
    S.emit()
    return nc


def build_moe(T, H):
    nc = bass.Bass("TRN2", target_bir_lowering=False)
    k = K(nc)
    S = k.S
    MB = 512
    NB = MB // 128
    hn = S.dram("hn", [T, D], BF16, kind="ExternalInput")
    gate = S.dram("gate", [T, 1], F32, kind="ExternalInput")
    w1 = S.dram("w1", [D, H], F32, kind="ExternalInput")
    w3 = S.dram("w3", [D, H], F32, kind="ExternalInput")
    w2 = S.dram("w2", [H, D], F32, kind="ExternalInput")
    part = S.dram("part", [T, D], BF16, kind="ExternalOutput")
    sw = Swiglu(k, H, blk=MB, w2w=256, n_w2=2)
    hbuf = Rot([S.sb("hn_t", [128, D], BF16) for _ in range(2)])
    hT = S.sb("hT", [128, KC, MB], BF16)
    gts = S.sb("gts", [128, NB], F32)
    obuf = S.sb("obuf", [128, NB, D], BF16)
    for blk in range(T // MB):
        for bt in range(NB):
            t0 = blk * MB + bt * 128
            hb = hbuf.next()
            S.dma("sp", hb[:], hn[t0:t0 + 128, :], [hn], [hb], hb)
            S.dma("sp", gts[:, bt:bt + 1], gate[t0:t0 + 128, :], [gate], [gts], gts)
            k.transpose_to(hb, lambda c, hb=hb: hb[:, c * 128:(c + 1) * 128], KC, hT, lambda c0, c1, bt=bt: hT[:, c0:c1, bt * 128:(bt + 1) * 128])

        def out_cb(tq, n0, n1, acc, blk=blk):
            S.op("act", lambda e: e.activation(out=obuf[:, tq, n0:n1], in_=acc[:, 0:n1 - n0], func=AF.Copy, scale=gts[:, tq:tq + 1]), [acc, gts], [obuf])
            if n1 == D:
                t0 = blk * MB + tq * 128
                S.dma("sp", part[t0:t0 + 128, :], obuf[:, tq, :], [obuf], [part], obuf)
        sw.run(hT, w1, w3, w2, out_cb)
    S.emit()
    return nc


def build_fin(T, n_part):
    nc = bass.Bass("TRN2", target_bir_lowering=False)
    S = Sched(nc)
    xres = S.dram("xres", [T, D], F32, kind="ExternalInput")
    parts = S.dram("parts", [n_part, T, D], BF16, kind="ExternalInput")
    out = S.dram("out", [T, D], F32, kind="ExternalOutput")
    hbuf = Rot([S.sb("h_f", [128, D], F32) for _ in range(2)])
    pbuf = Rot([S.sb("p_b", [128, D], BF16) for _ in range(3)])
    for tt in range(T // 128):
        h = hbuf.next()
        S.dma("sp", h[:], xres[tt * 128:(tt + 1) * 128, :], [xres], [h], h)
        for p in range(n_part):
            pb = pbuf.next()
            S.dma("sp", pb[:], parts[p, tt * 128:(tt + 1) * 128, :], [parts], [pb], pb)
            S.op("dve", lambda e, h=h, pb=pb: e.tensor_tensor(out=h[:], in0=h[:], in1=pb[:], op=ALU.add), [h, pb], [h])
        S.dma("sp", out[tt * 128:(tt + 1) * 128, :], h[:], [h], [out], h)
    S.emit()
    return nc


RH, RC, RN = 16, 1024, 64
GN_EPS = 1e-5 * 64


def build_rwkv(T):
    nc = bass.Bass("TRN2", target_bir_lowering=False)
    k = K(nc, n_tp=1, n_acc=1)
    S = k.S
    NT = T // 128
    di = lambda n, s, dt=F32: S.dram(n, s, dt, kind="ExternalInput")
    h1 = di("h1", [T, D])
    g_n = di("g_n", [D])
    mu = di("mu", [6, D])
    w_r, w_k, w_v = di("w_r", [D, RC]), di("w_k", [D, RC]), di("w_v", [D, RC])
    w_o = di("w_o", [RC, D])
    vecs = {n: di(n, [RC]) for n in ["w0", "a0", "k_k", "k_a", "r_k", "ln_w", "ln_b"]}
    w1, w2 = di("w1", [D, 96]), di("w2", [96, RC])
    a1, a2 = di("a1", [D, 96]), di("a2", [96, RC])
    g1, g2 = di("g1", [D, 256]), di("g2", [256, RC])
    part = S.dram("part", [T, D], BF16, kind="ExternalOutput")
    HN = S.dram("HN", [T + 1, D], F32)
    P_r, P_k, P_v = S.dram("P_r", [T, RC], F32), S.dram("P_k", [T, RC], F32), S.dram("P_v", [T, RC], F32)
    P_w1, P_a1, P_g1 = S.dram("P_w1", [T, 96], F32), S.dram("P_a1", [T, 96], F32), S.dram("P_g1", [T, 256], F32)

    ident_f = S.sb("ident_f", [128, 128], F32)
    S.dma("sp", ident_f[:], k.ident_d[:], [k.ident_d], [ident_f], ident_f)

    S.phase_begin()
    gn_bc = k.gain_bc("gn", g_n, D)
    zrow = S.sb("zrow", [1, D], F32)
    S.op("pool", lambda e: e.memset(zrow[:], 0.0), [], [zrow])
    S.dma("sp", HN[0:1, :], zrow[:], [zrow], [HN], zrow)
    hb = Rot([S.sb("hb", [128, D], F32) for _ in range(2)])
    ho = Rot([S.sb("ho", [128, D], F32) for _ in range(2)])
    for tt in range(NT):
        h = hb.next()
        o = ho.next()
        S.dma("sp", h[:], h1[tt * 128:(tt + 1) * 128, :], [h1], [h], h)
        k.rmsnorm(h, h[:], gn_bc, gn_bc[:], o, o[:], D)
        S.dma("sp", HN[1 + tt * 128:1 + (tt + 1) * 128, :], o[:], [o], [HN], o)
    S.phase_end()

    TH = min(T, 1024)
    groups = []
    for (wd, j, Pd) in [(w_r, 0, P_r), (w_k, 2, P_k), (w_v, 3, P_v)]:
        for c0 in range(0, RC, 256):
            groups.append((wd, c0, 256, j, Pd, c0))
    groups += [(w1, 0, 96, 1, P_w1, 0), (a1, 0, 96, 4, P_a1, 0), (g1, 0, 256, 5, P_g1, 0)]
    for th in range(T // TH):
        S.phase_begin()
        XT = S.sb("XT", [128, 2 * KC, TH], BF16)
        muT = S.sb("muT", [128, 6, KC], F32)
        S.dma("sp", muT[:], mu.t.rearrange("j (kc p) -> p j kc", p=128), [mu], [muT], muT, allow_slow_non_contiguous=True)
        hnb = Rot([S.sb("hnb", [128, D], F32) for _ in range(2)])
        shb = Rot([S.sb("shb", [128, D], F32) for _ in range(2)])
        cb = Rot([S.sb("cb", [128, 2 * D], BF16) for _ in range(2)])
        for tl in range(TH // 128):
            t0 = th * TH + tl * 128
            hn, sh, c = hnb.next(), shb.next(), cb.next()
            S.dma("sp", hn[:], HN[1 + t0:1 + t0 + 128, :], [HN], [hn], hn)
            S.dma("sp", sh[:], HN[t0:t0 + 128, :], [HN], [sh], sh)
            S.op("act", lambda e, hn=hn, c=c: e.activation(out=c[:, 0:D], in_=hn[:], func=AF.Copy), [hn], [c])
            S.op("dve", lambda e, hn=hn, sh=sh, c=c: e.tensor_tensor(out=c[:, D:2 * D], in0=sh[:], in1=hn[:], op=ALU.subtract), [hn, sh], [c])
            k.transpose_to(c, lambda q, c=c: c[:, q * 128:(q + 1) * 128], 2 * KC, XT, lambda c0, c1, tl=tl: XT[:, c0:c1, tl * 128:(tl + 1) * 128])
        wgb = Rot([S.sb("wg", [128, KC, 256], BF16) for _ in range(2)])
        mwb = Rot([S.sb("mwg", [128, KC, 256], BF16) for _ in range(2)])
        stg = Rot([S.sb("stg", [128, 256], F32) for _ in range(3)])
        for (wd, c0, n, j, Pd, pc0) in groups:
            wg, mw = wgb.next(), mwb.next()
            S.dma("pool", wg[:, :, 0:n], wd.t.rearrange("(kc p) n -> p kc n", p=128)[:, :, c0:c0 + n], [wd], [wg], wg)
            S.op("dve", lambda e, wg=wg, mw=mw, j=j, n=n: e.tensor_tensor(out=mw[:, :, 0:n], in0=wg[:, :, 0:n], in1=muT[:, j, :].unsqueeze(2).to_broadcast([128, KC, n]), op=ALU.mult), [wg, muT], [mw])
            for tl in range(TH // 128):
                t0 = th * TH + tl * 128
                acc = k.acc.next()
                for kc in range(2 * KC):
                    src_w = wg if kc < KC else mw
                    S.op("pe", lambda e, acc=acc, kc=kc, src_w=src_w, tl=tl, n=n: e.matmul(acc[:, 0:n], lhsT=XT[:, kc, tl * 128:(tl + 1) * 128], rhs=src_w[:, kc % KC, 0:n], start=(kc == 0), stop=(kc == 2 * KC - 1)), [XT, src_w], [acc])
                st = stg.next()
                S.op("act", lambda e, acc=acc, st=st, n=n: e.activation(out=st[:, 0:n], in_=acc[:, 0:n], func=AF.Copy), [acc], [st])
                S.dma("sp", Pd[t0:t0 + 128, pc0:pc0 + n], st[:, 0:n], [st], [Pd], st)
        S.phase_end()

    S.phase_begin()
    vb = {}
    for n in vecs:
        vb[n] = k.gain_bc("v_" + n, vecs[n], RC)
    omka = S.sb("omka", [128, RC], F32)
    S.op("dve", lambda e: e.tensor_scalar(out=omka[:], in0=vb["k_a"][:], scalar1=-1.0, scalar2=1.0, op0=ALU.mult, op1=ALU.add), [vb["k_a"]], [omka])
    w2_s = S.sb("w2_s", [96, RC], BF16)
    a2_s = S.sb("a2_s", [96, RC], BF16)
    g2_s = S.sb("g2_s", [128, 2, RC], BF16)
    wo_s = S.sb("wo_s", [128, 8, D], BF16)
    S.dma("pool", w2_s[:], w2[:, :], [w2], [w2_s], w2_s)
    S.dma("pool", a2_s[:], a2[:, :], [a2], [a2_s], a2_s)
    S.dma("pool", g2_s[:], g2.t.rearrange("(c p) n -> p c n", p=128), [g2], [g2_s], g2_s)
    S.dma("pool", wo_s[:], w_o.t.rearrange("(c p) n -> p c n", p=128), [w_o], [wo_s], wo_s)
    St = S.sb("St", [RN, RC], F32)
    S.op("pool", lambda e: e.memset(St[:], 0.0), [], [St])
    bcp = Rot([S.ps("bcp", [128, 1024], F32) for _ in range(3)])
    pr, pk, pv = S.sb("pr", [128, RC], F32), S.sb("pk", [128, RC], F32), S.sb("pv", [128, RC], F32)
    pw1, pa1, pg1 = S.sb("pw1", [128, 96], F32), S.sb("pa1", [128, 96], F32), S.sb("pg1", [128, 256], F32)
    lb = S.sb("lb", [128, 256], BF16)
    lT = S.sb("lT", [128, 2, 128], BF16)
    wr, av, gv = S.sb("wr", [128, RC], F32), S.sb("av", [128, RC], F32), S.sb("gv", [128, RC], F32)
    tA, tW, tB, tK = S.sb("tA", [128, RC], F32), S.sb("tW", [128, RC], F32), S.sb("tB", [128, RC], F32), S.sb("tK", [128, RC], F32)
    tm1, tm2 = S.sb("tm1", [128, RC], F32), S.sb("tm2", [128, RC], F32)
    s16 = S.sb("s16", [128, 4, RH], F32)
    VT = S.sb("VT", [RN, RH, 128], F32)
    YT = S.sb("YT", [RN, RH, 128], F32)
    T1, T2 = S.sb("T1", [RN, RC], F32), S.sb("T2", [RN, RC], F32)
    sa = S.sb("sa", [RN, RH], F32)
    hil = {nm: (S.sb("hi" + nm, [128, RC], BF16), S.sb("lo" + nm, [128, RC], BF16)) for nm in ("A", "B", "K", "R")}
    kbs = Rot([S.sb("kbs", [RN, RC], F32) for _ in range(2)])
    t3s = Rot([S.sb("t3s", [RN, RC], F32) for _ in range(2)])
    yb = S.sb("yb", [128, RC], BF16)
    ybT = S.sb("ybT", [128, 8, 128], BF16)
    ob = S.sb("ob", [128, D], BF16)
    v3 = lambda ap: ap.rearrange("p (h j) -> p h j", h=RH)
    bc3 = lambda ap, p=128: ap.unsqueeze(2).to_broadcast([p, RH, RN])

    def lora2(src, n, func, w_s, kparts, dst, bias):
        nch = (n + 127) // 128
        S.op("act", lambda e: e.activation(out=lb[:, 0:n], in_=src[:, 0:n], func=func), [src], [lb])
        k.transpose_to(lb, lambda c: lb[:, c * 128:(c + 1) * 128], nch, lT, lambda c0, c1: lT[:, c0:c1, :])
        for half in range(2):
            acc = k.acc.next()
            for c in range(nch):
                rows = min(n, 128)
                rhs = w_s[0:rows, half * 512:(half + 1) * 512] if kparts == 1 else w_s[:, c, half * 512:(half + 1) * 512]
                S.op("pe", lambda e, acc=acc, c=c, rhs=rhs, rows=rows: e.matmul(acc[:], lhsT=lT[0:rows, c, :], rhs=rhs, start=(c == 0), stop=(c == nch - 1)), [lT, w_s], [acc])
            if bias is not None:
                S.op("dve", lambda e, acc=acc, half=half: e.tensor_tensor(out=dst[:, half * 512:(half + 1) * 512], in0=acc[:], in1=bias[:, half * 512:(half + 1) * 512], op=ALU.add), [acc, bias], [dst])
            else:
                S.op("dve", lambda e, acc=acc, half=half: e.tensor_copy(out=dst[:, half * 512:(half + 1) * 512], in_=acc[:]), [acc], [dst])

    for tt in range(NT):
        t0 = tt * 128
        for (b_, P_, n) in [(pr, P_r, RC), (pk, P_k, RC), (pv, P_v, RC), (pw1, P_w1, 96), (pa1, P_a1, 96), (pg1, P_g1, 256)]:
            S.dma("sp", b_[:, 0:n], P_[t0:t0 + 128, :], [P_], [b_], b_)
        lora2(pw1, 96, AF.Tanh, w2_s, 1, wr, vb["w0"])
        lora2(pa1, 96, AF.Copy, a2_s, 1, av, vb["a0"])
        lora2(pg1, 256, AF.Sigmoid, g2_s, 2, gv, None)
        S.op("act", lambda e: e.activation(out=av[:], in_=av[:], func=AF.Sigmoid), [av], [av])
        S.op("act", lambda e: e.activation(out=tm1[:], in_=wr[:], func=AF.Exp, scale=-1.0), [wr], [tm1])
        S.op("act", lambda e: e.activation(out=tm1[:], in_=tm1[:], func=AF.Ln, bias=1.0), [tm1], [tm1])
        S.op("act", lambda e: e.activation(out=tm1[:], in_=tm1[:], func=AF.Exp, scale=-1.0, bias=-0.5), [tm1], [tm1])
        S.op("act", lambda e: e.activation(out=tW[:], in_=tm1[:], func=AF.Exp, scale=-1.0), [tm1], [tW])
        S.op("dve", lambda e: e.tensor_tensor(out=tm2[:], in0=pk[:], in1=vb["k_k"][:], op=ALU.mult), [pk, vb["k_k"]], [tm2])
        S.op("dve", lambda e: e.tensor_tensor(out=tm1[:], in0=tm2[:], in1=tm2[:], op=ALU.mult), [tm2], [tm1])
        S.op("dve", lambda e: e.tensor_reduce(out=s16[:, 0, :], in_=v3(tm1[:]), axis=AX.X, op=ALU.add), [tm1], [s16])
        S.op("act", lambda e: e.activation(out=s16[:, 1, :], in_=s16[:, 0, :], func=AF.Sqrt), [s16], [s16])
        S.op("dve", lambda e: e.tensor_scalar(out=s16[:, 1, :], in0=s16[:, 1, :], scalar1=1e-12, scalar2=None, op0=ALU.max), [s16], [s16])
        S.op("dve", lambda e: e.reciprocal(out=s16[:, 2, :], in_=s16[:, 1, :]), [s16], [s16])
        S.op("dve", lambda e: e.tensor_tensor(out=v3(tm2[:]), in0=v3(tm2[:]), in1=bc3(s16[:, 2, :]), op=ALU.mult), [tm2, s16], [tm2])
        S.op("dve", lambda e: e.tensor_scalar(out=tA[:], in0=tm2[:], scalar1=-1.0, scalar2=None, op0=ALU.mult), [tm2], [tA])
        S.op("dve", lambda e: e.tensor_tensor(out=tB[:], in0=tm2[:], in1=av[:], op=ALU.mult), [tm2, av], [tB])
        S.op("dve", lambda e: e.tensor_tensor(out=tm1[:], in0=av[:], in1=vb["k_a"][:], op=ALU.mult), [av, vb["k_a"]], [tm1])
        S.op("dve", lambda e: e.tensor_tensor(out=tm1[:], in0=tm1[:], in1=omka[:], op=ALU.add), [tm1, omka], [tm1])
        S.op("dve", lambda e: e.tensor_tensor(out=tK[:], in0=pk[:], in1=tm1[:], op=ALU.mult), [pk, tm1], [tK])
        S.op("dve", lambda e: e.tensor_tensor(out=tm1[:], in0=pr[:], in1=tK[:], op=ALU.mult), [pr, tK], [tm1])
        S.op("dve", lambda e: e.tensor_tensor(out=tm1[:], in0=tm1[:], in1=vb["r_k"][:], op=ALU.mult), [tm1, vb["r_k"]], [tm1])
        S.op("dve", lambda e: e.tensor_reduce(out=s16[:, 3, :], in_=v3(tm1[:]), axis=AX.X, op=ALU.add), [tm1], [s16])
        for h0 in range(0, RH, 8):
            bp = bcp.next()
            for h in range(h0, h0 + 8):
                S.op("pe", lambda e, bp=bp, h=h, h0=h0: e.transpose(out=bp[0:RN, (h - h0) * 128:(h - h0 + 1) * 128], in_=pv[:, h * RN:(h + 1) * RN], identity=ident_f[:]), [pv, ident_f], [bp])
            S.op("act", lambda e, bp=bp, h0=h0: e.activation(out=VT[:, h0:h0 + 8, :].rearrange("p a b -> p (a b)"), in_=bp[0:RN, :], func=AF.Copy), [bp], [VT])
        for nm, src_ in (("A", tA), ("B", tB), ("K", tK), ("R", pr)):
            hi_, lo_ = hil[nm]
            S.op("act", lambda e, hi_=hi_, src_=src_: e.activation(out=hi_[:], in_=src_[:], func=AF.Copy), [src_], [hi_])
            S.op("pool", lambda e, hi_=hi_, lo_=lo_, src_=src_: e.tensor_tensor(out=lo_[:], in0=src_[:], in1=hi_[:], op=ALU.subtract), [src_, hi_], [lo_])
        for t in range(128):
            sel = ident_f[:, t:t + 1].to_broadcast([128, RN])
            selb = k.ident[:, t:t + 1].to_broadcast([128, RN])
            bq = {}
            for nm, src in (("A", tA), ("W", tW), ("B", tB), ("K", tK), ("R", pr)):
                bp = bcp.next()
                for half in range(2):
                    if nm == "W":
                        S.op("pe", lambda e, bp=bp, src=src, half=half, sel=sel: e.matmul(bp[0:RN, half * 512:(half + 1) * 512], lhsT=sel, rhs=src[:, half * 512:(half + 1) * 512], start=True, stop=True), [ident_f, src], [bp])
                    else:
                        hi_, lo_ = hil[nm]
                        S.op("pe", lambda e, bp=bp, hi_=hi_, half=half, selb=selb: e.matmul(bp[0:RN, half * 512:(half + 1) * 512], lhsT=selb, rhs=hi_[:, half * 512:(half + 1) * 512], start=True, stop=False), [k.ident, hi_], [bp])
                        S.op("pe", lambda e, bp=bp, lo_=lo_, half=half, selb=selb: e.matmul(bp[0:RN, half * 512:(half + 1) * 512], lhsT=selb, rhs=lo_[:, half * 512:(half + 1) * 512], start=False, stop=True), [k.ident, lo_], [bp])
                bq[nm] = bp
                if nm == "A":
                    S.op("dve", lambda e, bp=bp: e.tensor_tensor(out=T1[:], in0=St[:], in1=bp[0:RN, :], op=ALU.mult), [St, bp], [T1])
                    S.op("dve", lambda e: e.tensor_reduce(out=sa[:], in_=v3(T1[:]), axis=AX.X, op=ALU.add), [T1], [sa])
                elif nm == "W":
                    S.op("dve", lambda e, bp=bp: e.tensor_tensor(out=St[:], in0=St[:], in1=bp[0:RN, :], op=ALU.mult), [St, bp], [St])
                elif nm == "B":
                    S.op("dve", lambda e, bp=bp: e.tensor_tensor(out=v3(T2[:]), in0=v3(bp[0:RN, :]), in1=bc3(sa[:], RN), op=ALU.mult), [bp, sa], [T2])
                    S.op("dve", lambda e: e.tensor_tensor(out=St[:], in0=St[:], in1=T2[:], op=ALU.add), [St, T2], [St])
                elif nm == "K":
                    kb_, t3_ = kbs.next(), t3s.next()
                    S.op("act", lambda e, bp=bp, kb_=kb_: e.activation(out=kb_[:], in_=bp[0:RN, :], func=AF.Copy), [bp], [kb_])
                    S.op("pool", lambda e, kb_=kb_, t3_=t3_, t=t: e.tensor_tensor(out=v3(t3_[:]), in0=v3(kb_[:]), in1=bc3(VT[:, :, t], RN), op=ALU.mult), [kb_, VT], [t3_])
                    S.op("dve", lambda e, t3_=t3_: e.tensor_tensor(out=St[:], in0=St[:], in1=t3_[:], op=ALU.add), [St, t3_], [St])
                else:
                    S.op("dve", lambda e, bp=bp: e.tensor_tensor(out=T1[:], in0=St[:], in1=bp[0:RN, :], op=ALU.mult), [St, bp], [T1])
                    S.op("dve", lambda e, t=t: e.tensor_reduce(out=YT[:, :, t], in_=v3(T1[:]), axis=AX.X, op=ALU.add), [T1], [YT])
        for h0 in range(0, RH, 8):
            bp = bcp.next()
            for h in range(h0, h0 + 8):
                S.op("pe", lambda e, bp=bp, h=h, h0=h0: e.transpose(out=bp[:, (h - h0) * RN:(h - h0 + 1) * RN], in_=YT[:, h, :], identity=ident_f[0:RN, 0:RN]), [YT, ident_f], [bp])
            S.op("act", lambda e, bp=bp, h0=h0: e.activation(out=tm1[:, h0 * RN:(h0 + 8) * RN], in_=bp[:, 0:8 * RN], func=AF.Copy), [bp], [tm1])
        y = tm1
        S.op("dve", lambda e: e.tensor_reduce(out=s16[:, 0, :], in_=v3(y[:]), axis=AX.X, op=ALU.add), [y], [s16])
        S.op("dve", lambda e: e.tensor_scalar(out=s16[:, 0, :], in0=s16[:, 0, :], scalar1=-1.0 / RN, scalar2=None, op0=ALU.mult), [s16], [s16])
        S.op("dve", lambda e: e.tensor_tensor(out=v3(y[:]), in0=v3(y[:]), in1=bc3(s16[:, 0, :]), op=ALU.add), [y, s16], [y])
        S.op("dve", lambda e: e.tensor_tensor(out=tm2[:], in0=y[:], in1=y[:], op=ALU.mult), [y], [tm2])
        S.op("dve", lambda e: e.tensor_reduce(out=s16[:, 1, :], in_=v3(tm2[:]), axis=AX.X, op=ALU.add), [tm2], [s16])
        S.op("dve", lambda e: e.tensor_scalar(out=s16[:, 1, :], in0=s16[:, 1, :], scalar1=1.0 / RN, scalar2=GN_EPS, op0=ALU.mult, op1=ALU.add), [s16], [s16])
        S.op("act", lambda e: e.activation(out=s16[:, 1, :], in_=s16[:, 1, :], func=AF.Sqrt), [s16], [s16])
        S.op("dve", lambda e: e.reciprocal(out=s16[:, 2, :], in_=s16[:, 1, :]), [s16], [s16])
        S.op("dve", lambda e: e.tensor_tensor(out=v3(y[:]), in0=v3(y[:]), in1=bc3(s16[:, 2, :]), op=ALU.mult), [y, s16], [y])
        S.op("dve", lambda e: e.tensor_tensor(out=y[:], in0=y[:], in1=vb["ln_w"][:], op=ALU.mult), [y, vb["ln_w"]], [y])
        S.op("dve", lambda e: e.tensor_tensor(out=y[:], in0=y[:], in1=vb["ln_b"][:], op=ALU.add), [y, vb["ln_b"]], [y])
        S.op("dve", lambda e: e.tensor_tensor(out=v3(tm2[:]), in0=v3(pv[:]), in1=bc3(s16[:, 3, :]), op=ALU.mult), [pv, s16], [tm2])
        S.op("dve", lambda e: e.tensor_tensor(out=y[:], in0=y[:], in1=tm2[:], op=ALU.add), [y, tm2], [y])
        S.op("dve", lambda e: e.tensor_tensor(out=yb[:], in0=y[:], in1=gv[:], op=ALU.mult), [y, gv], [yb])
        k.transpose_to(yb, lambda c: yb[:, c * 128:(c + 1) * 128], 8, ybT, lambda c0, c1: ybT[:, c0:c1, :])
        for nn in range(4):
            acc = k.acc.next()
            for c in range(8):
                S.op("pe", lambda e, acc=acc, c=c, nn=nn: e.matmul(acc[:], lhsT=ybT[:, c, :], rhs=wo_s[:, c, nn * 512:(nn + 1) * 512], start=(c == 0), stop=(c == 7)), [ybT, wo_s], [acc])
            S.op("act", lambda e, acc=acc, nn=nn: e.activation(out=ob[:, nn * 512:(nn + 1) * 512], in_=acc[:], func=AF.Copy), [acc], [ob])
        S.dma("sp", part[t0:t0 + 128, :], ob[:], [ob], [part], ob)
    S.phase_end()
    S.emit()
    return nc


def proj_phase(k, T, xd, gd, Wd, ncols, Pd, row_off, TH=1024):
    S = k.S
    TH = min(T, TH)
    groups = [(c0, min(256, ncols - c0)) for c0 in range(0, ncols, 256)]
    for th in range(T // TH):
        S.phase_begin()
        g_bc = k.gain_bc("pg", gd, D)
        XT = S.sb("XT", [128, KC, TH], BF16)
        hb = Rot([S.sb("hb", [128, D], F32) for _ in range(2)])
        hn = Rot([S.sb("hn", [128, D], BF16) for _ in range(2)])
        for tl in range(TH // 128):
            t0 = th * TH + tl * 128
            h, o = hb.next(), hn.next()
            S.dma("sp", h[:], xd[t0:t0 + 128, :], [xd], [h], h)
            k.rmsnorm(h, h[:], g_bc, g_bc[:], o, o[:], D)
            k.transpose_to(o, lambda q, o=o: o[:, q * 128:(q + 1) * 128], KC, XT, lambda c0, c1, tl=tl: XT[:, c0:c1, tl * 128:(tl + 1) * 128])
        wgb = Rot([S.sb("wg", [128, KC, 256], BF16) for _ in range(2)])
        stg = Rot([S.sb("stg", [128, 256], F32) for _ in range(3)])
        Wv = Wd.t.rearrange("(kc p) n -> p kc n", p=128)
        for (c0, n) in groups:
            wg = wgb.next()
            S.dma("pool", wg[:, :, 0:n], Wv[:, :, c0:c0 + n], [Wd], [wg], wg)
            for tl in range(TH // 128):
                t0 = th * TH + tl * 128
                acc = k.acc.next()
                for kc in range(KC):
                    S.op("pe", lambda e, acc=acc, kc=kc, wg=wg, tl=tl, n=n: e.matmul(acc[:, 0:n], lhsT=XT[:, kc, tl * 128:(tl + 1) * 128], rhs=wg[:, kc, 0:n], start=(kc == 0), stop=(kc == KC - 1)), [XT, wg], [acc])
                st = stg.next()
                S.op("act", lambda e, acc=acc, st=st, n=n: e.activation(out=st[:, 0:n], in_=acc[:, 0:n], func=AF.Copy), [acc], [st])
                S.dma("sp", Pd[row_off + t0:row_off + t0 + 128, c0:c0 + n], st[:, 0:n], [st], [Pd], st)
        S.phase_end()


SH, SP_, SN, SG = 16, 64, 128, 2
SC = SH * SP_
NCOL_SSM = 2 * SC + 2 * SG * SN + SH


def build_ssm(T):
    nc = bass.Bass("TRN2", target_bir_lowering=False)
    k = K(nc, n_tp=1, n_acc=2)
    S = k.S
    NT = T // 128
    di = lambda n, s, dt=F32: S.dram(n, s, dt, kind="ExternalInput")
    xb = di("xb", [T, D])
    g_n = di("g_n", [D])
    w_in = di("w_in", [D, NCOL_SSM])
    cw = di("cw", [4, 1536])
    cb = di("cb", [1536])
    dtb, alog, dsk = di("dtb", [SH]), di("alog", [SH]), di("dsk", [SH])
    nw = di("nw", [SC])
    w_o = di("w_o", [SC, D])
    tri_d, neg_d, ones_d = di("c_tri", [128, 128]), di("c_neg", [128, 128]), di("c_ones", [128, 128])
    part = S.dram("part", [T, D], BF16, kind="ExternalOutput")
    P = S.dram("P", [T + 3, NCOL_SSM], F32)

    zrow = S.sb("zrow", [3, NCOL_SSM], F32)
    S.op("pool", lambda e: e.memset(zrow[:], 0.0), [], [zrow])
    S.dma("sp", P[0:3, :], zrow[:], [zrow], [P], zrow)
    proj_phase(k, T, xb, g_n, w_in, NCOL_SSM, P, 3)

    S.phase_begin()
    ident_f = S.sb("ident_f", [128, 128], F32)
    tri, neg, ones = S.sb("tri", [128, 128], F32), S.sb("neg", [128, 128], F32), S.sb("ones", [128, 128], F32)
    for b_, d_ in ((ident_f, k.ident_d), (tri, tri_d), (neg, neg_d), (ones, ones_d)):
        S.dma("sp", b_[:], d_[:], [d_], [b_], b_)
    cwb = [k.gain_bc(f"cw{i}", cw.t[i], 1536) for i in range(4)]
    cbb = k.gain_bc("cbb", cb, 1536)
    nwb = k.gain_bc("nwb", nw, SC)
    dtbb, ab, dskb = k.gain_bc("dtbb", dtb, SH), k.gain_bc("ab", alog, SH), k.gain_bc("dskb", dsk, SH)
    S.op("act", lambda e: e.activation(out=ab[:], in_=ab[:], func=AF.Exp), [ab], [ab])
    S.op("dve", lambda e: e.tensor_scalar(out=ab[:], in0=ab[:], scalar1=-1.0, scalar2=None, op0=ALU.mult), [ab], [ab])
    wo_s = S.sb("wo_s", [128, 8, D], BF16)
    S.dma("pool", wo_s[:], w_o.t.rearrange("(c p) n -> p c n", p=128), [w_o], [wo_s], wo_s)
    dps = Rot([S.ps("dps", [128, 512], F32) for _ in range(2)])
    ydp, yop, stp = S.ps("ydp", [128, 512], F32), S.ps("yop", [128, 512], F32), S.ps("stp", [128, 512], F32)
    z = S.sb("z", [128, SC], F32)
    xk = [S.sb(f"xk{i}", [128, 1536], F32) for i in range(4)]
    tmpa, tmpb = S.sb("tmpa", [128, 1536], F32), S.sb("tmpb", [128, 1536], F32)
    cv = S.sb("cv", [128, 1536], F32)
    xa = S.sb("xa", [128, 1536], F32)
    s16 = S.sb("s16", [128, 12, SH], F32)
    cs = S.sb("cs", [128, 2 * SH], F32)
    xdt_b, xst_b = S.sb("xdt_b", [128, SC], BF16), S.sb("xst_b", [128, SC], BF16)
    bcb = S.sb("bcb", [128, 512], BF16)
    BCT = S.sb("BCT", [128, 4, 128], BF16)
    CBT = S.sb("CBT", [128, SG, 128], F32)
    DT = S.sb("DT", [128, SH, 128], F32)
    MT = S.sb("MT", [128, SH, 128], BF16)
    ysb, y = S.sb("ysb", [128, SC], F32), S.sb("y", [128, SC], F32)
    hs = S.sb("hs", [128, SG, 512], F32)
    hbf = S.sb("hbf", [128, SG, 512], BF16)
    S.op("pool", lambda e: e.memset(hs[:], 0.0), [], [hs])
    S.op("pool", lambda e: e.memset(hbf[:], 0.0), [], [hbf])
    yb = S.sb("yb", [128, SC], BF16)
    ybT = S.sb("ybT", [128, 8, 128], BF16)
    ob = S.sb("ob", [128, D], BF16)
    v3 = lambda ap: ap.rearrange("p (h j) -> p h j", j=SP_)
    bc3 = lambda ap, nh=SH: ap.unsqueeze(2).to_broadcast([128, nh, SP_])
    DTI, ADT, C_, NEGC, ECL, TOT, CD, DTE, DD = range(9)

    for tt in range(NT):
        t0 = tt * 128
        S.dma("sp", z[:], P[3 + t0:3 + t0 + 128, 0:SC], [P], [z], z)
        for i in range(4):
            S.dma("sp", xk[i][:], P[t0 + i:t0 + i + 128, SC:SC + 1536], [P], [xk[i]], xk[i])
        S.dma("sp", s16[:, DTI, :], P[3 + t0:3 + t0 + 128, SC + 1536:SC + 1536 + SH], [P], [s16], s16)
        S.op("pool", lambda e: e.tensor_tensor(out=cv[:], in0=xk[0][:], in1=cwb[0][:], op=ALU.mult), [xk[0], cwb[0]], [cv])
        S.op("pool", lambda e: e.tensor_tensor(out=tmpa[:], in0=xk[1][:], in1=cwb[1][:], op=ALU.mult), [xk[1], cwb[1]], [tmpa])
        S.op("dve", lambda e: e.tensor_tensor(out=cv[:], in0=cv[:], in1=tmpa[:], op=ALU.add), [cv, tmpa], [cv])
        S.op("pool", lambda e: e.tensor_tensor(out=tmpb[:], in0=xk[2][:], in1=cwb[2][:], op=ALU.mult), [xk[2], cwb[2]], [tmpb])
        S.op("dve", lambda e: e.tensor_tensor(out=cv[:], in0=cv[:], in1=tmpb[:], op=ALU.add), [cv, tmpb], [cv])
        S.op("pool", lambda e: e.tensor_tensor(out=tmpa[:], in0=xk[3][:], in1=cwb[3][:], op=ALU.mult), [xk[3], cwb[3]], [tmpa])
        S.op("dve", lambda e: e.tensor_tensor(out=cv[:], in0=cv[:], in1=tmpa[:], op=ALU.add), [cv, tmpa], [cv])
        S.op("dve", lambda e: e.tensor_tensor(out=cv[:], in0=cv[:], in1=cbb[:], op=ALU.add), [cv, cbb], [cv])
        S.op("act", lambda e: e.activation(out=xa[:], in_=cv[:], func=AF.Silu), [cv], [xa])
        S.op("dve", lambda e: e.tensor_tensor(out=s16[:, DTI, :], in0=s16[:, DTI, :], in1=dtbb[:], op=ALU.add), [s16, dtbb], [s16])
        S.op("act", lambda e: e.activation(out=s16[:, DTI, :], in_=s16[:, DTI, :], func=AF.Exp), [s16], [s16])
        S.op("act", lambda e: e.activation(out=s16[:, DTI, :], in_=s16[:, DTI, :], func=AF.Ln, bias=1.0), [s16], [s16])
        S.op("dve", lambda e: e.tensor_tensor(out=s16[:, ADT, :], in0=s16[:, DTI, :], in1=ab[:], op=ALU.mult), [s16, ab], [s16])
        acc = k.acc.next()
        S.op("pe", lambda e, acc=acc: e.matmul(acc[:, 0:SH], lhsT=tri[:], rhs=s16[:, ADT, :], start=True, stop=True), [tri, s16], [acc])
        S.op("pe", lambda e, acc=acc: e.matmul(acc[:, SH:2 * SH], lhsT=ones[:], rhs=s16[:, ADT, :], start=True, stop=True), [ones, s16], [acc])
        S.op("dve", lambda e, acc=acc: e.tensor_copy(out=cs[:], in_=acc[:, 0:2 * SH]), [acc], [cs])
        S.op("dve", lambda e: e.tensor_scalar(out=s16[:, NEGC, :], in0=cs[:, 0:SH], scalar1=-1.0, scalar2=None, op0=ALU.mult), [cs], [s16])
        S.op("act", lambda e: e.activation(out=s16[:, ECL, :], in_=cs[:, 0:SH], func=AF.Exp), [cs], [s16])
        S.op("act", lambda e: e.activation(out=s16[:, CD, :], in_=cs[:, SH:2 * SH], func=AF.Exp), [cs], [s16])
        S.op("dve", lambda e: e.tensor_tensor(out=s16[:, TOT, :], in0=cs[:, SH:2 * SH], in1=cs[:, 0:SH], op=ALU.subtract), [cs], [s16])
        S.op("act", lambda e: e.activation(out=s16[:, DTE, :], in_=s16[:, TOT, :], func=AF.Exp), [s16], [s16])
        S.op("dve", lambda e: e.tensor_tensor(out=s16[:, DD, :], in0=s16[:, DTE, :], in1=s16[:, DTI, :], op=ALU.mult), [s16], [s16])
        S.op("dve", lambda e: e.tensor_tensor(out=v3(xdt_b[:]), in0=v3(xa[:, 0:SC]), in1=bc3(s16[:, DTI, :]), op=ALU.mult), [xa, s16], [xdt_b])
        S.op("dve", lambda e: e.tensor_tensor(out=v3(xst_b[:]), in0=v3(xa[:, 0:SC]), in1=bc3(s16[:, DD, :]), op=ALU.mult), [xa, s16], [xst_b])
        S.op("act", lambda e: e.activation(out=bcb[:], in_=xa[:, SC:SC + 512], func=AF.Copy), [xa], [bcb])
        k.transpose_to(bcb, lambda c: bcb[:, c * 128:(c + 1) * 128], 4, BCT, lambda c0, c1: BCT[:, c0:c1, :])
        acc = k.acc.next()
        for g in range(SG):
            S.op("pe", lambda e, acc=acc, g=g: e.matmul(acc[:, g * 128:(g + 1) * 128], lhsT=BCT[:, g, :], rhs=BCT[:, 2 + g, :], start=True, stop=True), [BCT], [acc])
        S.op("act", lambda e, acc=acc: e.activation(out=CBT[:].rearrange("p a b -> p (a b)"), in_=acc[:, 0:256], func=AF.Copy), [acc], [CBT])
        for hq in range(4):
            dp = dps.next()
            for j in range(4):
                h = hq * 4 + j
                S.op("pe", lambda e, dp=dp, j=j, h=h: e.matmul(dp[:, j * 128:(j + 1) * 128], lhsT=cs[:, h:h + 1].to_broadcast([128, 128]), rhs=ident_f[:], start=True, stop=False), [cs, ident_f], [dp])
                S.op("pe", lambda e, dp=dp, j=j: e.matmul(dp[:, j * 128:(j + 1) * 128], lhsT=ident_f[:], rhs=neg[:], start=False, stop=True), [ident_f, neg], [dp])
            for j in range(4):
                h = hq * 4 + j
                S.op("act", lambda e, dp=dp, j=j, h=h: e.activation(out=DT[:, h, :], in_=dp[:, j * 128:(j + 1) * 128], func=AF.Exp, bias=s16[:, NEGC, h:h + 1]), [dp, s16], [DT])
        for g in range(SG):
            S.op("dve", lambda e, g=g: e.tensor_tensor(out=MT[:, g * 8:(g + 1) * 8, :], in0=DT[:, g * 8:(g + 1) * 8, :], in1=CBT[:, g, :].unsqueeze(1).to_broadcast([128, 8, 128]), op=ALU.mult), [DT, CBT], [MT])
        for g in range(SG):
            for j in range(8):
                h = g * 8 + j
                S.op("pe", lambda e, h=h, j=j: e.matmul(ydp[:, j * 64:(j + 1) * 64], lhsT=MT[:, h, :], rhs=xdt_b[:, h * 64:(h + 1) * 64], start=True, stop=True), [MT, xdt_b], [ydp])
            S.op("pe", lambda e, g=g: e.matmul(yop[:], lhsT=BCT[:, 2 + g, :], rhs=hbf[:, g, :], start=True, stop=True), [BCT, hbf], [yop])
            S.op("act", lambda e, g=g: e.activation(out=ysb[:, g * 512:(g + 1) * 512], in_=ydp[:], func=AF.Copy), [ydp], [ysb])
            S.op("dve", lambda e, g=g: e.tensor_tensor(out=v3(y[:, g * 512:(g + 1) * 512]), in0=v3(yop[:]), in1=bc3(s16[:, ECL, g * 8:(g + 1) * 8], 8), op=ALU.mult), [yop, s16], [y])
            S.op("pe", lambda e, g=g: e.matmul(stp[:], lhsT=bcb[:, g * 128:(g + 1) * 128], rhs=xst_b[:, g * 512:(g + 1) * 512], start=True, stop=True), [bcb, xst_b], [stp])
            S.op("dve", lambda e, g=g: e.tensor_tensor(out=v3(hs[:, g, :]), in0=v3(hs[:, g, :]), in1=bc3(s16[:, CD, g * 8:(g + 1) * 8], 8), op=ALU.mult), [hs, s16], [hs])
            S.op("dve", lambda e, g=g: e.tensor_tensor(out=hs[:, g, :], in0=hs[:, g, :], in1=stp[:], op=ALU.add), [hs, stp], [hs])
            S.op("act", lambda e, g=g: e.activation(out=hbf[:, g, :], in_=hs[:, g, :], func=AF.Copy), [hs], [hbf])
        S.op("dve", lambda e: e.tensor_tensor(out=y[:], in0=y[:], in1=ysb[:], op=ALU.add), [y, ysb], [y])
        S.op("dve", lambda e: e.tensor_tensor(out=v3(ysb[:]), in0=v3(xa[:, 0:SC]), in1=bc3(dskb[:]), op=ALU.mult), [xa, dskb], [ysb])
        S.op("dve", lambda e: e.tensor_tensor(out=y[:], in0=y[:], in1=ysb[:], op=ALU.add), [y, ysb], [y])
        S.op("act", lambda e: e.activation(out=ysb[:], in_=z[:], func=AF.Silu), [z], [ysb])
        S.op("dve", lambda e: e.tensor_tensor(out=y[:], in0=y[:], in1=ysb[:], op=ALU.mult), [y, ysb], [y])
        for g in range(SG):
            k.rmsnorm(y, y[:, g * 512:(g + 1) * 512], nwb, nwb[:, g * 512:(g + 1) * 512], yb, yb[:, g * 512:(g + 1) * 512], 512)
        k.transpose_to(yb, lambda c: yb[:, c * 128:(c + 1) * 128], 8, ybT, lambda c0, c1: ybT[:, c0:c1, :])
        for nn in range(4):
            acc = k.acc.next()
            for c in range(8):
                S.op("pe", lambda e, acc=acc, c=c, nn=nn: e.matmul(acc[:], lhsT=ybT[:, c, :], rhs=wo_s[:, c, nn * 512:(nn + 1) * 512], start=(c == 0), stop=(c == 7)), [ybT, wo_s], [acc])
            S.op("act", lambda e, acc=acc, nn=nn: e.activation(out=ob[:, nn * 512:(nn + 1) * 512], in_=acc[:], func=AF.Copy), [acc], [ob])
        S.dma("sp", part[t0:t0 + 128, :], ob[:], [ob], [part], ob)
    S.phase_end()
    S.emit()
    return nc


def _ssm_consts():
    s = np.arange(128)[:, None]
    l = np.arange(128)[None, :]
    return dict(c_tri=(s <= l).astype(np.float32), c_neg=np.where(s > l, -30000.0, 0.0).astype(np.float32), c_ones=np.ones((128, 128), np.float32))


def _ssm_cols(hh):
    z = np.arange(hh * 1024, hh * 1024 + 1024)
    xc = 2048 + np.arange(hh * 1024, hh * 1024 + 1024)
    Bc = 2048 + 2048 + np.arange(2 * hh * 128, 2 * hh * 128 + 256)
    Cc = 2048 + 2048 + 512 + np.arange(2 * hh * 128, 2 * hh * 128 + 256)
    dtc = 2048 + 3072 + np.arange(hh * 16, hh * 16 + 16)
    conv_rows = np.concatenate([xc, Bc, Cc]) - 2048
    return np.concatenate([z, xc, Bc, Cc, dtc]), conv_rows


def _ssm_inputs(inp, xb, hh):
    f32 = lambda a: np.ascontiguousarray(np.asarray(a), dtype=np.float32)
    cols, crow = _ssm_cols(hh)
    m = dict(c_ident=_ident(), xb=xb, g_n=f32(inp["norm_mix"][0]), w_in=f32(np.asarray(inp["ev_w_in"][0])[:, cols]),
             cw=f32(np.asarray(inp["ev_conv_w"][0])[crow].T), cb=f32(np.asarray(inp["ev_conv_b"][0])[crow]),
             dtb=f32(np.asarray(inp["ev_dt_bias"][0])[hh * 16:(hh + 1) * 16]), alog=f32(np.asarray(inp["ev_a_log"][0])[hh * 16:(hh + 1) * 16]),
             dsk=f32(np.asarray(inp["ev_d_skip"][0])[hh * 16:(hh + 1) * 16]), nw=f32(np.asarray(inp["ev_ssm_norm"][0])[hh * 1024:(hh + 1) * 1024]),
             w_o=f32(np.asarray(inp["ev_w_out"][0])[hh * 1024:(hh + 1) * 1024]))
    m.update(_ssm_consts())
    return m


NG, NR, HD = 2, 4, 128
NQH = NG * NR
NCOL_NSA = NQH * HD + 6 * NG * HD + NQH * 3
OQ, OKC, OVC, OKS, OVS, OKW, OVW, OGL = 0, 1024, 1280, 1536, 1792, 2048, 2304, 2560
VW_ = 132
CW_ = 196


def build_nsa(T, debug=False):
    nc = bass.Bass("TRN2", target_bir_lowering=False)
    k = K(nc, n_tp=1, n_acc=1)
    k.debug = debug
    S = k.S
    NT = T // 128
    NCMP = (T - 32) // 16 + 1
    NIT = (NCMP + 127) // 128
    di = lambda n, s, dt=F32: S.dram(n, s, dt, kind="ExternalInput")
    xb = di("xb", [T, D])
    g_n = di("g_n", [D])
    w_in = di("w_in", [D, NCOL_NSA])
    pos = di("pos", [T, 1], I32)
    invf = di("c_invf", [16])
    qg, kcg, ksg, kwg = di("qg", [HD]), di("kcg", [HD]), di("ksg", [HD]), di("kwg", [HD])
    pe_k, pe_v = di("pe_k", [32, HD]), di("pe_v", [32, HD])
    wk1, wv1 = di("wk1", [32 * HD, 256]), di("wv1", [32 * HD, 256])
    wk2, wv2 = di("wk2", [256, HD]), di("wv2", [256, HD])
    w_o = di("w_o", [NQH * HD, D])
    ov_d = di("c_ov", [NIT * 128, 64])
    efull_d = di("c_efull", [64, T])
    cmask_d = di("c_cmask", [16, 128, 128])
    diag_d = di("c_diag", [128, 128])
    fadd_d = di("c_fadd", [NT, 128, 64])
    part = S.dram("part", [T, D], BF16, kind="ExternalOutput")
    P = S.dram("P", [T, NCOL_NSA], F32)

    KsT = S.sb("KsT", [128, NG, T], BF16)
    KwT = S.sb("KwT", [128, NG, T], BF16)
    Vs = S.sb("Vs", [128, NT, NG, VW_], BF16)
    Vw = S.sb("Vw", [128, NT, NG, VW_], BF16)
    KcmpT = S.sb("KcmpT", [128, NG, NIT * 128], BF16)
    Vcmp = S.sb("Vcmp", [128, NG, NIT, CW_], BF16)
    cosb = S.sb("cosb", [128, NT, 16], F32)
    sinb = S.sb("sinb", [128, NT, 16], F32)
    S.op("pool", lambda e: e.memset(Vs[:], 1.0), [], [Vs])
    S.op("pool", lambda e: e.memset(Vw[:], 1.0), [], [Vw])
    S.op("pool", lambda e: e.memset(Vcmp[:], 0.0), [], [Vcmp])
    S.op("pool", lambda e: e.memset(KcmpT[:], 0.0), [], [KcmpT])

    proj_phase(k, T, xb, g_n, w_in, NCOL_NSA, P, 0)

    def rope(buf, ap3, nh, tt, t1, t2, t3, t4):
        cb_ = cosb[:, tt, :].unsqueeze(1).to_broadcast([128, nh, 16])
        sb_ = sinb[:, tt, :].unsqueeze(1).to_broadcast([128, nh, 16])
        x1, x2 = ap3[:, :, 0:16], ap3[:, :, 16:32]
        v = lambda b: b[:, 0:nh * 16].rearrange("p (h i) -> p h i", i=16)
        S.op("dve", lambda e: e.tensor_tensor(out=v(t1), in0=x1, in1=cb_, op=ALU.mult), [buf, cosb], [t1])
        S.op("dve", lambda e: e.tensor_tensor(out=v(t2), in0=x2, in1=sb_, op=ALU.mult), [buf, sinb], [t2])
        S.op("dve", lambda e: e.tensor_tensor(out=v(t3), in0=x2, in1=cb_, op=ALU.mult), [buf, cosb], [t3])
        S.op("dve", lambda e: e.tensor_tensor(out=v(t4), in0=x1, in1=sb_, op=ALU.mult), [buf, sinb], [t4])
        S.op("dve", lambda e: e.tensor_tensor(out=x1, in0=v(t1), in1=v(t2), op=ALU.subtract), [t1, t2], [buf])
        S.op("dve", lambda e: e.tensor_tensor(out=x2, in0=v(t3), in1=v(t4), op=ALU.add), [t3, t4], [buf])

    S.phase_begin()
    gb = {n: k.gain_bc("g_" + n, d_, HD) for n, d_ in (("kc", kcg), ("ks", ksg), ("kw", kwg))}
    invf_bc = k.gain_bc("invf", invf, 16)
    KcT = S.sb("KcT", [128, NG, T], BF16)
    VcT = S.sb("VcT", [128, NG, T], BF16)
    w1s = {"k": S.sb("w1k", [128, 32, 256], BF16), "v": S.sb("w1v", [128, 32, 256], BF16)}
    w2s = {"k": S.sb("w2k", [128, 2, HD], BF16), "v": S.sb("w2v", [128, 2, HD], BF16)}
    peT = {"k": S.sb("peTk", [128, 32], BF16), "v": S.sb("peTv", [128, 32], BF16)}
    for nm, w1d, w2d, ped in (("k", wk1, wk2, pe_k), ("v", wv1, wv2, pe_v)):
        S.dma("pool", w1s[nm][:], w1d.t.rearrange("(j d) n -> d j n", d=HD), [w1d], [w1s[nm]], w1s[nm])
        S.dma("pool", w2s[nm][:], w2d.t.rearrange("(c p) n -> p c n", p=128), [w2d], [w2s[nm]], w2s[nm])
        S.dma("pool", peT[nm][:], ped.t.rearrange("j d -> d j"), [ped], [peT[nm]], peT[nm], allow_slow_non_contiguous=True)
    ov_s = S.sb("ov_s", [128, NIT, 64], BF16)
    S.dma("pool", ov_s[:], ov_d.t.rearrange("(c p) n -> p c n", p=128), [ov_d], [ov_s], ov_s)
    kvl = S.sb("kvl", [128, 6 * NG * HD], F32)
    kvn = S.sb("kvn", [128, 4 * NG * HD], BF16)
    pos_i = S.sb("pos_i", [128, 1], I32)
    ang = S.sb("ang", [128, 16], F32)
    angi = S.sb("angi", [128, 16], I32)
    rt = [S.sb(f"rt{i}", [128, 128], F32) for i in range(4)]
    kT4 = S.sb("kT4", [128, 8, 128], BF16)
    for tt in range(NT):
        t0 = tt * 128
        S.dma("sp", kvl[:], P[t0:t0 + 128, OKC:OKC + 6 * NG * HD], [P], [kvl], kvl)
        S.dma("sp", pos_i[:], pos[t0:t0 + 128, :], [pos], [pos_i], pos_i)
        S.op("dve", lambda e: e.tensor_copy(out=ang[:, 0:1], in_=pos_i[:]), [pos_i], [ang])
        S.op("dve", lambda e: e.tensor_scalar(out=ang[:], in0=invf_bc[:], scalar1=ang[:, 0:1], scalar2=None, op0=ALU.mult), [ang, invf_bc], [ang])
        for ri, shift in ((0, 0.0), (1, 0.5 * math.pi)):
            r_ = rt[ri]
            S.op("dve", lambda e, r_=r_, shift=shift: e.tensor_scalar(out=r_[:, 0:16], in0=ang[:], scalar1=shift, scalar2=None, op0=ALU.add), [ang], [r_])
            S.op("dve", lambda e, r_=r_: e.tensor_scalar(out=r_[:, 16:32], in0=r_[:, 0:16], scalar1=1.0 / (2 * math.pi), scalar2=None, op0=ALU.mult), [r_], [r_])
            S.op("dve", lambda e, r_=r_: e.tensor_scalar(out=r_[:, 16:32], in0=r_[:, 16:32], scalar1=12582912.0, scalar2=None, op0=ALU.add), [r_], [r_])
            S.op("dve", lambda e, r_=r_: e.tensor_scalar(out=r_[:, 16:32], in0=r_[:, 16:32], scalar1=-12582912.0, scalar2=None, op0=ALU.add), [r_], [r_])
            S.op("dve", lambda e, r_=r_: e.scalar_tensor_tensor(out=r_[:, 0:16], in0=r_[:, 16:32], scalar=-2 * math.pi, in1=r_[:, 0:16], op0=ALU.mult, op1=ALU.add), [r_], [r_])
            S.op("dve", lambda e, r_=r_: e.tensor_scalar(out=r_[:, 16:32], in0=r_[:, 0:16], scalar1=math.pi, scalar2=None, op0=ALU.is_gt), [r_], [r_])
            S.op("dve", lambda e, r_=r_: e.scalar_tensor_tensor(out=r_[:, 0:16], in0=r_[:, 16:32], scalar=-2 * math.pi, in1=r_[:, 0:16], op0=ALU.mult, op1=ALU.add), [r_], [r_])
            S.op("dve", lambda e, r_=r_: e.tensor_scalar(out=r_[:, 16:32], in0=r_[:, 0:16], scalar1=-math.pi, scalar2=None, op0=ALU.is_lt), [r_], [r_])
            S.op("dve", lambda e, r_=r_: e.scalar_tensor_tensor(out=r_[:, 0:16], in0=r_[:, 16:32], scalar=2 * math.pi, in1=r_[:, 0:16], op0=ALU.mult, op1=ALU.add), [r_], [r_])
        if tt == NT - 1 and debug:
            dbgr = S.sb("dbgr", [128, 64], F32)
            S.op("dve", lambda e: e.tensor_copy(out=dbgr[:, 0:32], in_=rt[0][:, 0:32]), [rt[0]], [dbgr])
            S.op("dve", lambda e: e.tensor_copy(out=dbgr[:, 32:64], in_=rt[1][:, 0:32]), [rt[1]], [dbgr])
        S.op("act", lambda e, tt=tt: e.activation(out=sinb[:, tt, :], in_=rt[0][:, 0:16], func=AF.Sin), [rt[0]], [sinb])
        S.op("act", lambda e, tt=tt: e.activation(out=cosb[:, tt, :], in_=rt[1][:, 0:16], func=AF.Sin), [rt[1]], [cosb])
        for gi in range(NG):
            for (src_off, gname) in ((2 * NG * HD, "ks"), (4 * NG * HD, "kw")):
                ap = kvl[:, src_off + gi * HD:src_off + (gi + 1) * HD]
                st = k.rms_rstd(kvl, ap, HD)
                S.op("dve", lambda e, ap=ap, st=st, gname=gname: e.scalar_tensor_tensor(out=ap, in0=ap, scalar=st[:, 2:3], in1=gb[gname][:], op0=ALU.mult, op1=ALU.mult), [kvl, st, gb[gname]], [kvl])
        for src_off in (2 * NG * HD, 4 * NG * HD):
            rope(kvl, kvl[:, src_off:src_off + NG * HD].rearrange("p (h d) -> p h d", d=HD), NG, tt, *rt)
        S.op("act", lambda e: e.activation(out=kvn[:, 0:256], in_=kvl[:, 2 * NG * HD:3 * NG * HD], func=AF.Copy), [kvl], [kvn])
        S.op("act", lambda e: e.activation(out=kvn[:, 256:512], in_=kvl[:, 4 * NG * HD:5 * NG * HD], func=AF.Copy), [kvl], [kvn])
        S.op("act", lambda e: e.activation(out=kvn[:, 512:1024], in_=kvl[:, 0:2 * NG * HD], func=AF.Copy), [kvl], [kvn])
        k.transpose_to(kvn, lambda c: kvn[:, c * 128:(c + 1) * 128], 8, kT4, lambda c0, c1: kT4[:, c0:c1, :])
        for gi in range(NG):
            S.op("dve", lambda e, gi=gi, t0=t0: e.tensor_copy(out=KsT[:, gi, t0:t0 + 128], in_=kT4[:, gi, :]), [kT4], [KsT])
            S.op("dve", lambda e, gi=gi, t0=t0: e.tensor_copy(out=KwT[:, gi, t0:t0 + 128], in_=kT4[:, 2 + gi, :]), [kT4], [KwT])
            S.op("pool", lambda e, gi=gi, t0=t0: e.tensor_copy(out=KcT[:, gi, t0:t0 + 128], in_=kT4[:, 4 + gi, :]), [kT4], [KcT])
            S.op("pool", lambda e, gi=gi, t0=t0: e.tensor_copy(out=VcT[:, gi, t0:t0 + 128], in_=kT4[:, 6 + gi, :]), [kT4], [VcT])
        S.op("act", lambda e, tt=tt: e.activation(out=Vs[:, tt, :, 0:HD], in_=kvl[:, 3 * NG * HD:4 * NG * HD].rearrange("p (g d) -> p g d", d=HD), func=AF.Copy), [kvl], [Vs])
        S.op("act", lambda e, tt=tt: e.activation(out=Vw[:, tt, :, 0:HD], in_=kvl[:, 5 * NG * HD:6 * NG * HD].rearrange("p (g d) -> p g d", d=HD), func=AF.Copy), [kvl], [Vw])
    hT = S.sb("hT", [128, 2, NIT * 128], BF16)
    cbias = S.sb("cbias", [128, 2], F32)
    cm_f = S.sb("cm_f", [128, HD], F32)
    cm_b = S.sb("cm_b", [128, HD], BF16)
    for nm, UT in (("k", KcT), ("v", VcT)):
        for hc in range(2):
            acc = k.acc.next()
            for j in range(32):
                S.op("pe", lambda e, acc=acc, j=j, hc=hc, nm=nm: e.matmul(acc[:, 0:1], lhsT=w1s[nm][:, j, hc * 128:(hc + 1) * 128], rhs=peT[nm][:, j:j + 1], start=(j == 0), stop=(j == 31)), [w1s[nm], peT[nm]], [acc])
            S.op("dve", lambda e, acc=acc, hc=hc: e.tensor_copy(out=cbias[:, hc:hc + 1], in_=acc[:, 0:1]), [acc], [cbias])
        for gi in range(NG):
            for hc in range(2):
                acc = k.acc.next()
                for j in range(32):
                    S.op("pe", lambda e, acc=acc, j=j, hc=hc, nm=nm, gi=gi, UT=UT: e.matmul(acc[:, 0:NCMP], lhsT=w1s[nm][:, j, hc * 128:(hc + 1) * 128], rhs=UT[:, gi, j:j + 16 * (NCMP - 1) + 1:16], start=(j == 0), stop=(j == 31)), [w1s[nm], UT], [acc])
                S.op("act", lambda e, acc=acc, hc=hc: e.activation(out=hT[:, hc, 0:NCMP], in_=acc[:, 0:NCMP], func=AF.Silu, bias=cbias[:, hc:hc + 1]), [acc, cbias], [hT])
            for it in range(NIT):
                ni = min(128, NCMP - it * 128)
                acc = k.acc.next()
                for hc in range(2):
                    S.op("pe", lambda e, acc=acc, hc=hc, it=it, ni=ni, nm=nm: e.matmul(acc[0:ni, 0:HD], lhsT=hT[:, hc, it * 128:it * 128 + ni], rhs=w2s[nm][:, hc, :], start=(hc == 0), stop=(hc == 1)), [hT, w2s[nm]], [acc])
                if nm == "k":
                    S.op("pool", lambda e: e.memset(cm_f[:], 0.0), [], [cm_f])
                    S.op("act", lambda e, acc=acc, ni=ni: e.activation(out=cm_f[0:ni, :], in_=acc[0:ni, 0:HD], func=AF.Copy), [acc], [cm_f])
                    k.rmsnorm(cm_f, cm_f[:], gb["kc"], gb["kc"][:], cm_b, cm_b[:], HD)
                    k.transpose_to(cm_b, lambda c: cm_b[:], 1, KcmpT, lambda c0, c1, gi=gi, it=it: KcmpT[:, gi, it * 128:(it + 1) * 128])
                else:
                    S.op("act", lambda e, acc=acc, ni=ni, gi=gi, it=it: e.activation(out=Vcmp[0:ni, gi, it, 0:HD], in_=acc[0:ni, 0:HD], func=AF.Copy), [acc], [Vcmp])
                    S.op("dve", lambda e, ni=ni, gi=gi, it=it: e.tensor_copy(out=Vcmp[0:ni, gi, it, HD:HD + 64], in_=ov_s[0:ni, it, :]), [ov_s], [Vcmp])
                    S.op("pool", lambda e, ni=ni, gi=gi, it=it: e.memset(Vcmp[0:ni, gi, it, HD + 64:HD + 65], 1.0), [], [Vcmp])
    k.dump("ang", ang, ang[:], [128, 16])
    if debug:
        k.dump("rt0", dbgr, dbgr[:], [128, 64])
    k.dump("posi", pos_i, pos_i[:], [128, 1], I32)
    k.dump("invf", invf_bc, invf_bc[:], [128, 16])
    k.dump("cos", cosb, cosb[:], [128, NT, 16])
    k.dump("sin", sinb, sinb[:], [128, NT, 16])
    k.dump("KsT", KsT, KsT[:], [128, NG, T], BF16)
    k.dump("KwT", KwT, KwT[:], [128, NG, T], BF16)
    k.dump("KcmpT", KcmpT, KcmpT[:], [128, NG, NIT * 128], BF16)
    k.dump("Vcmp", Vcmp, Vcmp[:], [128, NG, NIT, CW_], BF16)
    S.phase_end()

    S.phase_begin()
    qg_bc = k.gain_bc("g_q", qg, HD)
    wo_s = S.sb("wo_s", [128, NQH, D], BF16)
    S.dma("pool", wo_s[:], w_o.t.rearrange("(c p) n -> p c n", p=128), [w_o], [wo_s], wo_s)
    efull = S.sb("efull", [64, T], BF16)
    S.dma("pool", efull[:], efull_d[:, :], [efull_d], [efull], efull)
    cmask = S.sb("cmask", [128, 16, 128], BF16)
    S.dma("pool", cmask[:], cmask_d.t.rearrange("c p q -> p c q"), [cmask_d], [cmask], cmask)
    diag = S.sb("diag", [128, 128], BF16)
    sup = S.sb("sup", [128, 128], BF16)
    S.dma("pool", diag[:], diag_d[:, :], [diag_d], [diag], diag)
    S.op("dve", lambda e: e.tensor_scalar(out=sup[:], in0=diag[:], scalar1=-1.0, scalar2=1.0, op0=ALU.mult, op1=ALU.add), [diag], [sup])
    fadd = S.sb("fadd", [128, NT, 64], F32)
    S.dma("sp", fadd[:], fadd_d.t.rearrange("t p j -> p t j"), [fadd_d], [fadd], fadd)
    stp_ = Rot([S.ps("sT", [128, 512], F32) for _ in range(1)])
    mxp = S.ps("mxp", [128, 128], F32)
    oacc = [S.ps("oacc", [128, 512], F32) for _ in range(4)]
    ql = S.sb("ql", [128, NQH * HD], F32)
    gl = S.sb("gl", [128, NQH * 3], F32)
    qn = S.sb("qn", [128, NQH * HD], BF16)
    QT = S.sb("QT", [128, NQH, 128], BF16)
    rq = [S.sb(f"rq{i}", [128, 128], F32) for i in range(4)]
    Eb = Rot([S.sb("Eb", [128, 512], F32) for _ in range(2)])
    Pb = Rot([S.sb("Pb", [128, 512], BF16) for _ in range(2)])
    y = S.sb("y", [128, NQH * HD], F32)
    yb = S.sb("yb", [128, NQH * HD], BF16)
    ybT = S.sb("ybT", [128, NQH, 128], BF16)
    ob = S.sb("ob", [128, D], BF16)
    rc = S.sb("rc", [128, 8], F32)
    imp = S.sb("imp", [128, 64], F32)
    sc2 = S.sb("sc2", [128, 64], F32)
    m8 = S.sb("m8", [128, 16], F32)
    selb = S.sb("selb", [128, 128], BF16)
    selT = S.sb("selT", [64, 128], BF16)
    cf = S.sb("cf", [128, NQH * 3], F32)
    scale = HD ** -0.5
    S.op("pool", lambda e: e.memset(selb[:], 0.0), [], [selb])
    q3 = lambda ap, w=128: ap.rearrange("p (r q) -> p r q", q=w)

    def branch_out(gi, br, first):
        for r in range(NR):
            oa = oacc[r]
            base = 0
            h = gi * NR + r
            S.op("dve", lambda e, oa=oa, base=base: e.reciprocal(out=rc[:, 0:1], in_=oa[:, base + HD:base + HD + 1]), [oa], [rc])
            S.op("dve", lambda e, h=h, br=br: e.tensor_tensor(out=rc[:, 1:2], in0=rc[:, 0:1], in1=gl[:, h * 3 + br:h * 3 + br + 1], op=ALU.mult), [rc, gl], [rc])
            if first:
                S.op("dve", lambda e, oa=oa, base=base, h=h: e.tensor_scalar(out=y[:, h * HD:(h + 1) * HD], in0=oa[:, base:base + HD], scalar1=rc[:, 1:2], scalar2=None, op0=ALU.mult), [oa, rc], [y])
            else:
                S.op("dve", lambda e, oa=oa, base=base, h=h: e.scalar_tensor_tensor(out=y[:, h * HD:(h + 1) * HD], in0=oa[:, base:base + HD], scalar=rc[:, 1:2], in1=y[:, h * HD:(h + 1) * HD], op0=ALU.mult, op1=ALU.add), [oa, rc, y], [y])

    for qt in range(NT):
        t0 = qt * 128
        S.dma("sp", ql[:], P[t0:t0 + 128, OQ:OQ + NQH * HD], [P], [ql], ql)
        S.dma("sp", gl[:], P[t0:t0 + 128, OGL:OGL + NQH * 3], [P], [gl], gl)
        S.op("act", lambda e: e.activation(out=gl[:], in_=gl[:], func=AF.Sigmoid), [gl], [gl])
        for h in range(NQH):
            ap = ql[:, h * HD:(h + 1) * HD]
            st = k.rms_rstd(ql, ap, HD)
            S.op("dve", lambda e, ap=ap, st=st: e.scalar_tensor_tensor(out=ap, in0=ap, scalar=st[:, 2:3], in1=qg_bc[:], op0=ALU.mult, op1=ALU.mult), [ql, st, qg_bc], [ql])
        rope(ql, ql[:].rearrange("p (h d) -> p h d", d=HD), NQH, qt, *rq)
        S.op("act", lambda e: e.activation(out=qn[:], in_=ql[:], func=AF.Copy), [ql], [qn])
        k.transpose_to(qn, lambda c: qn[:, c * 128:(c + 1) * 128], NQH, QT, lambda c0, c1: QT[:, c0:c1, :])
        if qt == 0:
            k.dump("QT", QT, QT[:], [128, NQH, 128], BF16)
        for gi in range(NG):
            qrhs = QT[:, gi * NR:(gi + 1) * NR, :].rearrange("p r q -> p (r q)")
            its = [it for it in range(NIT) if 16 * it * 128 + 31 <= t0 + 127]
            for ii, it in enumerate(its):
                sT = stp_.next()
                S.op("pe", lambda e, sT=sT, it=it, gi=gi, qrhs=qrhs: e.matmul(sT[:], lhsT=KcmpT[:, gi, it * 128:(it + 1) * 128], rhs=qrhs, start=True, stop=True), [KcmpT, QT], [sT])
                E = Eb.next()
                S.op("act", lambda e, sT=sT, E=E: e.activation(out=E[:], in_=sT[:], func=AF.Exp, scale=scale), [sT], [E])
                Pm = Pb.next()
                delta = qt - 16 * it
                if delta >= 16:
                    S.op("dve", lambda e, E=E, Pm=Pm: e.tensor_copy(out=Pm[:], in_=E[:]), [E], [Pm])
                else:
                    S.op("dve", lambda e, E=E, Pm=Pm, delta=delta: e.tensor_tensor(out=q3(Pm[:]), in0=q3(E[:]), in1=cmask[:, delta, :].unsqueeze(1).to_broadcast([128, NR, 128]), op=ALU.mult), [E, cmask], [Pm])
                for r in range(NR):
                    oa = oacc[r]
                    base = 0
                    S.op("pe", lambda e, oa=oa, base=base, Pm=Pm, r=r, it=it, gi=gi, ii=ii: e.matmul(oa[:, base:base + HD + 65], lhsT=Pm[:, r * 128:(r + 1) * 128], rhs=Vcmp[:, gi, it, 0:HD + 65], start=(ii == 0), stop=(ii == len(its) - 1)), [Pm, Vcmp], [oa])
            if its:
                for r in range(NR):
                    oa = oacc[r]
                    base = 0
                    S.op("dve", lambda e, oa=oa, base=base, r=r: e.tensor_scalar(out=rc[:, 2 + r:3 + r], in0=oa[:, base + HD + 64:base + HD + 65], scalar1=1e-30, scalar2=None, op0=ALU.max), [oa], [rc])
                    S.op("dve", lambda e, r=r: e.reciprocal(out=rc[:, 2 + r:3 + r], in_=rc[:, 2 + r:3 + r]), [rc], [rc])
                    if r == 0:
                        S.op("dve", lambda e, oa=oa, base=base, r=r: e.tensor_scalar(out=imp[:], in0=oa[:, base + HD:base + HD + 64], scalar1=rc[:, 2 + r:3 + r], scalar2=None, op0=ALU.mult), [oa, rc], [imp])
                    else:
                        S.op("dve", lambda e, oa=oa, base=base, r=r: e.scalar_tensor_tensor(out=imp[:], in0=oa[:, base + HD:base + HD + 64], scalar=rc[:, 2 + r:3 + r], in1=imp[:], op0=ALU.mult, op1=ALU.add), [oa, rc, imp], [imp])
                    h = gi * NR + r
                    S.op("dve", lambda e, r=r, h=h: e.tensor_tensor(out=rc[:, 1:2], in0=rc[:, 2 + r:3 + r], in1=gl[:, h * 3:h * 3 + 1], op=ALU.mult), [rc, gl], [rc])
                    S.op("dve", lambda e, oa=oa, base=base, h=h: e.tensor_scalar(out=y[:, h * HD:(h + 1) * HD], in0=oa[:, base:base + HD], scalar1=rc[:, 1:2], scalar2=None, op0=ALU.mult), [oa, rc], [y])
            else:
                S.op("pool", lambda e: e.memset(imp[:], 0.0), [], [imp])
                S.op("pool", lambda e, gi=gi: e.memset(y[:, gi * NR * HD:(gi + 1) * NR * HD], 0.0), [], [y])
            S.op("dve", lambda e, qt=qt: e.tensor_tensor(out=imp[:], in0=imp[:], in1=fadd[:, qt, :], op=ALU.add), [imp, fadd], [imp])
            S.op("dve", lambda e: e.max(out=m8[:, 0:8], in_=imp[:]), [imp], [m8])
            S.op("dve", lambda e: e.match_replace(out=sc2[:], in_to_replace=m8[:, 0:8], in_values=imp[:], imm_value=-3.0e9), [imp, m8], [sc2])
            S.op("dve", lambda e: e.max(out=m8[:, 8:16], in_=sc2[:]), [sc2], [m8])
            S.op("dve", lambda e: e.tensor_scalar(out=sc2[:], in0=imp[:], scalar1=m8[:, 15:16], scalar2=None, op0=ALU.is_ge), [imp, m8], [sc2])
            S.op("dve", lambda e: e.tensor_scalar(out=imp[:], in0=imp[:], scalar1=-1.0e8, scalar2=None, op0=ALU.is_gt), [imp], [imp])
            S.op("dve", lambda e: e.tensor_tensor(out=selb[:, 0:64], in0=sc2[:], in1=imp[:], op=ALU.mult), [sc2, imp], [selb])
            tp = k.tp.next()
            S.op("pe", lambda e, tp=tp: e.transpose(out=tp[:, 0:128], in_=selb[:], identity=k.ident[:]), [selb, k.ident], [tp])
            S.op("dve", lambda e, tp=tp: e.tensor_copy(out=selT[:], in_=tp[0:64, 0:128]), [tp], [selT])
            for kt in range(qt + 1):
                sT = stp_.next()
                S.op("pe", lambda e, sT=sT, kt=kt, gi=gi, qrhs=qrhs: e.matmul(sT[:], lhsT=KsT[:, gi, kt * 128:(kt + 1) * 128], rhs=qrhs, start=True, stop=True), [KsT, QT], [sT])
                S.op("pe", lambda e, kt=kt: e.matmul(mxp[:], lhsT=efull[:, kt * 128:(kt + 1) * 128], rhs=selT[:], start=True, stop=True), [efull, selT], [mxp])
                E = Eb.next()
                S.op("act", lambda e, sT=sT, E=E: e.activation(out=E[:], in_=sT[:], func=AF.Exp, scale=scale), [sT], [E])
                Pm = Pb.next()
                if kt == qt:
                    S.op("dve", lambda e, E=E: e.tensor_tensor(out=q3(E[:]), in0=q3(E[:]), in1=diag[:].unsqueeze(1).to_broadcast([128, NR, 128]), op=ALU.mult), [E, diag], [E])
                S.op("dve", lambda e, E=E, Pm=Pm: e.tensor_tensor(out=q3(Pm[:]), in0=q3(E[:]), in1=mxp[:].unsqueeze(1).to_broadcast([128, NR, 128]), op=ALU.mult), [E, mxp], [Pm])
                for r in range(NR):
                    oa = oacc[r]
                    base = 0
                    S.op("pe", lambda e, oa=oa, base=base, Pm=Pm, r=r, kt=kt, gi=gi, qt=qt: e.matmul(oa[:, base:base + HD + 1], lhsT=Pm[:, r * 128:(r + 1) * 128], rhs=Vs[:, kt, gi, 0:HD + 1], start=(kt == 0), stop=(kt == qt)), [Pm, Vs], [oa])
            branch_out(gi, 1, False)
            kts = [kt for kt in range(qt - 4, qt + 1) if kt >= 0]
            for kt in kts:
                sT = stp_.next()
                S.op("pe", lambda e, sT=sT, kt=kt, gi=gi, qrhs=qrhs: e.matmul(sT[:], lhsT=KwT[:, gi, kt * 128:(kt + 1) * 128], rhs=qrhs, start=True, stop=True), [KwT, QT], [sT])
                E = Eb.next()
                S.op("act", lambda e, sT=sT, E=E: e.activation(out=E[:], in_=sT[:], func=AF.Exp, scale=scale), [sT], [E])
                Pm = Pb.next()
                if kt == qt:
                    S.op("dve", lambda e, E=E, Pm=Pm: e.tensor_tensor(out=q3(Pm[:]), in0=q3(E[:]), in1=diag[:].unsqueeze(1).to_broadcast([128, NR, 128]), op=ALU.mult), [E, diag], [Pm])
                elif kt == qt - 4:
                    S.op("dve", lambda e, E=E, Pm=Pm: e.tensor_tensor(out=q3(Pm[:]), in0=q3(E[:]), in1=sup[:].unsqueeze(1).to_broadcast([128, NR, 128]), op=ALU.mult), [E, sup], [Pm])
                else:
                    S.op("dve", lambda e, E=E, Pm=Pm: e.tensor_copy(out=Pm[:], in_=E[:]), [E], [Pm])
                for r in range(NR):
                    oa = oacc[r]
                    base = 0
                    S.op("pe", lambda e, oa=oa, base=base, Pm=Pm, r=r, kt=kt, gi=gi, kts=kts: e.matmul(oa[:, base:base + HD + 1], lhsT=Pm[:, r * 128:(r + 1) * 128], rhs=Vw[:, kt, gi, 0:HD + 1], start=(kt == kts[0]), stop=(kt == kts[-1])), [Pm, Vw], [oa])
            branch_out(gi, 2, False)
        S.op("act", lambda e: e.activation(out=yb[:], in_=y[:], func=AF.Copy), [y], [yb])
        k.transpose_to(yb, lambda c: yb[:, c * 128:(c + 1) * 128], NQH, ybT, lambda c0, c1: ybT[:, c0:c1, :])
        for nn in range(4):
            acc = k.acc.next()
            for c in range(NQH):
                S.op("pe", lambda e, acc=acc, c=c, nn=nn: e.matmul(acc[:], lhsT=ybT[:, c, :], rhs=wo_s[:, c, nn * 512:(nn + 1) * 512], start=(c == 0), stop=(c == NQH - 1)), [ybT, wo_s], [acc])
            S.op("act", lambda e, acc=acc, nn=nn: e.activation(out=ob[:, nn * 512:(nn + 1) * 512], in_=acc[:], func=AF.Copy), [acc], [ob])
        S.dma("sp", part[t0:t0 + 128, :], ob[:], [ob], [part], ob)
    S.phase_end()
    S.emit()
    return nc


def _nsa_consts(T):
    NT = T // 128
    NCMP = (T - 32) // 16 + 1
    NIT = (NCMP + 127) // 128
    NBLK = T // 64
    c0 = np.arange(NIT * 128)[:, None] * 16
    s0 = np.arange(64)[None, :] * 64
    ov = np.clip(np.minimum(c0 + 32, s0 + 64) - np.maximum(c0, s0), 0, None) / 16.0
    ov[NCMP:, :] = 0.0
    ov[:, NBLK:] = 0.0
    efull = (np.arange(T)[None, :] // 64 == np.arange(64)[:, None]).astype(np.float32)
    ip = np.arange(128)[:, None]
    qp = np.arange(128)[None, :]
    cmask = np.stack([(16 * ip + 31 <= qp + 128 * d) for d in range(16)], 0).astype(np.float32)
    diag = (ip <= qp).astype(np.float32)
    t = np.arange(T)[:, None]
    blk = np.arange(64)[None, :]
    cur = t // 64
    causal = blk <= cur
    forced = ((blk == 0) | (blk >= cur - 1)) & causal
    fadd = np.where(forced, 1.0e9, np.where(causal, 0.0, -1.0e9)).astype(np.float32).reshape(NT, 128, 64)
    invf = np.exp(-math.log(500000.0) * np.arange(0, 32, 2, dtype=np.float32) / 32).astype(np.float32)
    return dict(c_ov=ov.astype(np.float32), c_efull=efull, c_cmask=cmask, c_diag=diag, c_fadd=fadd, c_invf=invf)


def _nsa_cols(gg):
    Q0 = 2048 + 3072 + 32
    q = Q0 + np.arange(8 * gg * 128, 8 * gg * 128 + 1024)
    parts = [q]
    off = Q0 + 2048
    for i in range(6):
        parts.append(off + i * 512 + np.arange(2 * gg * 128, 2 * gg * 128 + 256))
    gl = off + 6 * 512 + np.arange(8 * gg * 3, 8 * gg * 3 + 24)
    parts.append(gl)
    return np.concatenate(parts)


def _nsa_inputs(inp, xb, posb, gg, T):
    f32 = lambda a: np.ascontiguousarray(np.asarray(a), dtype=np.float32)
    m = dict(c_ident=_ident(), xb=xb, g_n=f32(inp["norm_mix"][0]), w_in=f32(np.asarray(inp["ev_w_in"][0])[:, _nsa_cols(gg)]),
             pos=np.ascontiguousarray(np.asarray(posb).astype(np.int32).reshape(T, 1)),
             qg=f32(inp["ev_q_gain"][0]), kcg=f32(inp["ev_kc_gain"][0]), ksg=f32(inp["ev_ks_gain"][0]), kwg=f32(inp["ev_kw_gain"][0]),
             pe_k=f32(inp["ev_pe_k"][0]), pe_v=f32(inp["ev_pe_v"][0]), wk1=f32(inp["ev_cmp_wk1"][0]), wv1=f32(inp["ev_cmp_wv1"][0]),
             wk2=f32(inp["ev_cmp_wk2"][0]), wv2=f32(inp["ev_cmp_wv2"][0]),
             w_o=f32(np.asarray(inp["ev_w_out"][0])[2048 + gg * 1024:2048 + (gg + 1) * 1024]))
    m.update(_nsa_consts(T))
    return m


NCORES = 8
SEQ = 4096
BATCH = 4
TSH = 2048
_cache = {}


def _run(key, builder, in_maps):
    if key not in _cache:
        _cache[key] = builder()
    res = run_bass_kernel_spmd(_cache[key], in_maps, core_ids=list(range(NCORES)))
    return res.results


def _rwkv_inputs(inp, h1b, hh):
    f32 = lambda a: np.ascontiguousarray(np.asarray(a), dtype=np.float32)
    cols = slice(hh * RC, (hh + 1) * RC)
    g = lambda n: np.asarray(inp[n][0])
    return dict(c_ident=_ident(), h1=h1b, g_n=f32(inp["norm_mix"][1]), mu=f32(g("od_mu")),
                w_r=f32(g("od_w_r")[:, cols]), w_k=f32(g("od_w_k")[:, cols]), w_v=f32(g("od_w_v")[:, cols]), w_o=f32(g("od_w_o")[cols, :]),
                w0=f32(g("od_w0")[cols]), a0=f32(g("od_a0")[cols]), k_k=f32(g("od_k_k")[cols]), k_a=f32(g("od_k_a")[cols]),
                r_k=f32(g("od_r_k").reshape(-1)[cols]), ln_w=f32(g("od_ln_w")[cols]), ln_b=f32(g("od_ln_b")[cols]),
                w1=f32(g("od_w1")), w2=f32(g("od_w2")[:, cols]), a1=f32(g("od_a1")), a2=f32(g("od_a2")[:, cols]),
                g1=f32(g("od_g1")), g2=f32(g("od_g2")[:, cols]))


def kernel(**inp):
    f32 = lambda a: np.ascontiguousarray(np.asarray(a), dtype=np.float32)
    x = f32(inp["x"])
    mem = f32(inp["mem"])
    positions = np.asarray(inp["positions"])
    ident = _ident()

    def tok_shard(arr, c):
        b, half = c // 2, c % 2
        return np.ascontiguousarray(arr[b, half * TSH:(half + 1) * TSH])

    def xattn_w(layer, c):
        return dict(mem=mem[c // 2], g_x=f32(inp["norm_xattn"][layer]), g_m=f32(inp["norm_mem"][layer]),
                    g_f=f32(inp["norm_ffn"][layer]), wq=f32(inp["xattn_wq"][layer]), wkv=f32(inp["xattn_wkv"][layer]),
                    wo=f32(inp["xattn_wo"][layer]), qg=f32(inp["xattn_q_gain"][layer]), kg=f32(inp["xattn_k_gain"][layer]))

    r_ssm = _run("ssm", lambda: build_ssm(SEQ), [_ssm_inputs(inp, x[c // 2], c % 2) for c in range(NCORES)])
    p_ssm = [r_ssm[c]["part"] for c in range(NCORES)]
    r_nsa = _run("nsa", lambda: build_nsa(SEQ), [_nsa_inputs(inp, x[c // 2], positions[c // 2], c % 2, SEQ) for c in range(NCORES)])
    p_nsa = [r_nsa[c]["part"] for c in range(NCORES)]
    maps = []
    for c in range(NCORES):
        b, half = c // 2, c % 2
        sl = slice(half * TSH, (half + 1) * TSH)
        parts = np.stack([p_ssm[2 * b][sl], p_ssm[2 * b + 1][sl], p_nsa[2 * b][sl], p_nsa[2 * b + 1][sl]], axis=0)
        m = dict(c_ident=ident, xres=tok_shard(x, c), parts=parts, w1=f32(inp["ev_ffn_w1"][0]), w3=f32(inp["ev_ffn_w3"][0]), w2=f32(inp["ev_ffn_w2"][0]))
        m.update(xattn_w(0, c))
        maps.append(m)
    r = _run("mid0", lambda: build_mid(TSH, 4, 5632, False), maps)
    h1 = [r[c]["h_out"] for c in range(NCORES)]
    maps = [_rwkv_inputs(inp, np.concatenate([h1[2 * (c // 2)], h1[2 * (c // 2) + 1]], axis=0), c % 2) for c in range(NCORES)]
    r_rw = _run("rwkv", lambda: build_rwkv(SEQ), maps)
    p_rw = [r_rw[c]["part"] for c in range(NCORES)]
    maps = []
    for c in range(NCORES):
        b, half = c // 2, c % 2
        sl = slice(half * TSH, (half + 1) * TSH)
        parts = np.stack([p_rw[2 * b][sl], p_rw[2 * b + 1][sl]], axis=0)
        m = dict(c_ident=ident, xres=h1[c], parts=parts, router=f32(inp["od_router"][0]))
        m.update(xattn_w(1, c))
        maps.append(m)
    r = _run("mid1", lambda: build_mid(TSH, 2, 0, True), maps)
    h2 = [r[c]["h_out"] for c in range(NCORES)]
    hn_all = np.concatenate([r[c]["hn_out"] for c in range(NCORES)], axis=0)
    gates = np.concatenate([r[c]["gate_out"] for c in range(NCORES)], axis=0)
    maps = []
    for e in range(NCORES):
        maps.append(dict(c_ident=ident, hn=hn_all, gate=np.ascontiguousarray(gates[:, e:e + 1]),
                         w1=f32(inp["od_moe_w1"][0, e]), w3=f32(inp["od_moe_w3"][0, e]), w2=f32(inp["od_moe_w2"][0, e])))
    r = _run("moe", lambda: build_moe(BATCH * SEQ, 7168), maps)
    maps = []
    for c in range(NCORES):
        parts = np.stack([r[e]["part"][c * TSH:(c + 1) * TSH] for e in range(NCORES)], axis=0)
        maps.append(dict(xres=h2[c], parts=parts))
    r = _run("fin", lambda: build_fin(TSH, NCORES), maps)
    out = np.stack([np.concatenate([r[2 * b]["out"], r[2 * b + 1]["out"]], axis=0) for b in range(BATCH)], axis=0)
    return out.astype(np.float32)
```
